# Optimizing a Trainium2 kernel written in Bass

```python
import jax, jax.numpy as jnp
from jax import lax
import numpy as np

D_MODEL = 2048
BATCH = 4
SEQ = 2048
DEPTH = 2
DEC_BATCH = 128
DEC_SEQ = 8
PAST_LEN = 16384
PAGE_SIZE = 128

W_A = D_MODEL
N_HEADS_A = 16
HEAD_A = W_A // N_HEADS_A
CONV_W = 4
C_GATE = 8.0
W_B = D_MODEL
N_GROUPS_B = 16
GROUP_B = W_B // N_GROUPS_B
CHUNK = 128
D_FF = 3 * D_MODEL
N_EXPERTS = 8
TOP_K = 2
D_EXPERT = D_MODEL
N_DENSE = (DEPTH + 1) // 2
N_MOE = DEPTH // 2
EPS = 1e-6
IN_COLS = 2 * W_A + 2 * W_B + 2 * D_MODEL
SPLITS = (W_A, 2 * W_A, 2 * W_A + W_B, 2 * W_A + 2 * W_B, 2 * W_A + 2 * W_B + D_MODEL)

kernel_name = 'hybrid_rglru_chunkmlp_moe_decode_step'


def rms_norm(x, g):
    xf = x.astype(jnp.float32)
    y = xf * lax.rsqrt(jnp.mean(xf * xf, axis=-1, keepdims=True) + EPS)
    return (y * g.astype(jnp.float32)).astype(x.dtype)


def causal_conv(x, buf, w, b):
    t = x.shape[1]
    xp = jnp.concatenate([buf.astype(x.dtype), x], axis=1)
    out = b
    for k in range(CONV_W):
        out = out + xp[:, k:k + t] * w[k]
    return out, xp[:, -(CONV_W - 1):]


def _lin_combine(left, right):
    a_l, b_l = left
    a_r, b_r = right
    return a_l * a_r, a_r * b_l + b_r


def rg_lru(x, h0, w_r, b_r, w_i, b_i, lam):
    bsz, t, _ = x.shape
    xf = x.astype(jnp.float32)
    xh = xf.reshape(bsz, t, N_HEADS_A, HEAD_A)
    r = jax.nn.sigmoid(jnp.einsum('bthd,hde->bthe', xh, w_r.astype(jnp.float32)).reshape(bsz, t, W_A) + b_r.astype(jnp.float32))
    i = jax.nn.sigmoid(jnp.einsum('bthd,hde->bthe', xh, w_i.astype(jnp.float32)).reshape(bsz, t, W_A) + b_i.astype(jnp.float32))
    log_a = -C_GATE * r * jax.nn.softplus(-lam.astype(jnp.float32))
    a = jnp.exp(log_a)
    b = jnp.sqrt(-jnp.expm1(2.0 * log_a)) * (i * xf)
    b = b.at[:, 0].add(a[:, 0] * h0.astype(jnp.float32))
    _, h = lax.associative_scan(_lin_combine, (a, b), axis=1)
    return h.astype(x.dtype), h[:, -1].astype(x.dtype)


def chunk_mix(u, v, w_s, b_s):
    bsz, t, _ = v.shape
    pad = (-t) % CHUNK
    vp = jnp.pad(v, ((0, 0), (0, pad), (0, 0)))
    n_c = (t + pad) // CHUNK
    vc = vp.reshape(bsz, n_c, CHUNK, N_GROUPS_B, GROUP_B)
    mask = jnp.tril(jnp.ones((CHUNK, CHUNK), dtype=w_s.dtype))
    mixed = jnp.einsum('gts,bcsgd->bctgd', w_s * mask, vc) + b_s.T[None, None, :, :, None]
    mixed = mixed.reshape(bsz, n_c * CHUNK, W_B)[:, :t]
    return u * mixed


def swiglu(x, wg, wu, wd):
    return (jax.nn.silu(x @ wg) * (x @ wu)) @ wd


def moe(x, router_w, router_b, wg, wu, wd):
    logits = x.astype(jnp.float32) @ router_w.astype(jnp.float32) + router_b.astype(jnp.float32)
    probs = jax.nn.softmax(logits, axis=-1)
    top_v, top_i = lax.top_k(probs, TOP_K)
    top_v = top_v / jnp.sum(top_v, axis=-1, keepdims=True)
    gates = jnp.sum(jax.nn.one_hot(top_i, N_EXPERTS, dtype=jnp.float32) * top_v[..., None], axis=-2)
    gates = gates.astype(x.dtype)
    y = jnp.zeros_like(x)
    for e in range(N_EXPERTS):
        y = y + gates[..., e:e + 1] * swiglu(x, wg[e], wu[e], wd[e])
    return y


def mixer(xm, h0, conv0, l, p):
    proj = xm @ p['w_in'][l]
    xa, ga, u, v, mg_a, mg_b = jnp.split(proj, SPLITS, axis=-1)
    xc, conv_new = causal_conv(xa, conv0, p['conv_w'][l], p['conv_b'][l])
    h, h_last = rg_lru(xc, h0, p['w_r'][l], p['b_r'][l], p['w_i'][l], p['b_i'][l], p['lru_lambda'][l])
    ya = jax.nn.gelu(ga) * h
    vn = rms_norm(jax.nn.gelu(v), p['g_v'][l])
    yb = chunk_mix(jax.nn.gelu(u), vn, p['w_s'][l], p['b_s'][l])
    m = jax.nn.sigmoid(mg_a) * (ya @ p['w_pa'][l]) + jax.nn.sigmoid(mg_b) * (yb @ p['w_pb'][l])
    return m @ p['w_o'][l], h_last, conv_new, vn


def trunk(x, c, h0s, conv0s, p):
    hs, convs, vs = [], [], []
    for l in range(DEPTH):
        mod = jax.nn.silu(c) @ p['w_ada'][l] + p['b_ada'][l]
        sh1, sc1, gt1, sh2, sc2, gt2 = [m[:, None, :] for m in jnp.split(mod, 6, axis=-1)]
        xn = rms_norm(x, p['g_pre_mix'][l]) * (1.0 + sc1) + sh1
        y, h_last, conv_new, vn = mixer(xn, h0s[l], conv0s[l], l, p)
        x = x + gt1 * rms_norm(y, p['g_post_mix'][l])
        xn = rms_norm(x, p['g_pre_ffn'][l]) * (1.0 + sc2) + sh2
        j = l // 2
        if l % 2 == 0:
            y = swiglu(xn, p['ffn_wg'][j], p['ffn_wu'][j], p['ffn_wd'][j])
        else:
            y = moe(xn, p['router_w'][j], p['router_b'][j], p['moe_wg'][j], p['moe_wu'][j], p['moe_wd'][j])
        x = x + gt2 * rms_norm(y, p['g_post_ffn'][l])
        hs.append(h_last)
        convs.append(conv_new)
        vs.append(vn)
    return x, jnp.stack(hs), jnp.stack(convs), jnp.stack(vs)


def setup_inputs(seed: int = 0) -> dict:
    key = jax.random.key(seed)
    ks = iter(jax.random.split(key, 40))

    def nrm(shape, s):
        return s * jax.random.normal(next(ks), shape, jnp.float32)

    u = jax.random.uniform(next(ks), (DEPTH, W_A), jnp.float32, minval=0.9, maxval=0.999)
    a_base = u ** (1.0 / C_GATE)
    lru_lambda = jnp.log(a_base) - jnp.log1p(-a_base)
    d = D_MODEL
    return {
        'x_prompt': nrm((BATCH, SEQ, d), 1.0),
        'x_sample': nrm((DEC_BATCH, DEC_SEQ, d), 1.0),
        'c_prompt': nrm((BATCH, d), 1.0),
        'c_sample': nrm((DEC_BATCH, d), 1.0),
        'state_lru_h': nrm((DEPTH, DEC_BATCH, W_A), 0.5),
        'state_lru_conv': nrm((DEPTH, DEC_BATCH, CONV_W - 1, W_A), 1.0),
        'w_ada': nrm((DEPTH, d, 6 * d), 0.5 * d ** -0.5),
        'b_ada': nrm((DEPTH, 6 * d), 0.02),
        'g_pre_mix': 1.0 + nrm((DEPTH, d), 0.05),
        'g_post_mix': 1.0 + nrm((DEPTH, d), 0.05),
        'g_pre_ffn': 1.0 + nrm((DEPTH, d), 0.05),
        'g_post_ffn': 1.0 + nrm((DEPTH, d), 0.05),
        'w_in': nrm((DEPTH, d, IN_COLS), d ** -0.5),
        'conv_w': nrm((DEPTH, CONV_W, W_A), CONV_W ** -0.5),
        'conv_b': nrm((DEPTH, W_A), 0.02),
        'w_r': nrm((DEPTH, N_HEADS_A, HEAD_A, HEAD_A), HEAD_A ** -0.5),
        'b_r': nrm((DEPTH, W_A), 0.02),
        'w_i': nrm((DEPTH, N_HEADS_A, HEAD_A, HEAD_A), HEAD_A ** -0.5),
        'b_i': nrm((DEPTH, W_A), 0.02),
        'lru_lambda': lru_lambda,
        'g_v': 1.0 + nrm((DEPTH, W_B), 0.05),
        'w_s': nrm((DEPTH, N_GROUPS_B, CHUNK, CHUNK), CHUNK ** -0.5),
        'b_s': 1.0 + nrm((DEPTH, N_GROUPS_B, CHUNK), 0.1),
        'w_pa': nrm((DEPTH, W_A, d), W_A ** -0.5),
        'w_pb': nrm((DEPTH, W_B, d), W_B ** -0.5),
        'w_o': nrm((DEPTH, d, d), d ** -0.5),
        'ffn_wg': nrm((N_DENSE, d, D_FF), d ** -0.5),
        'ffn_wu': nrm((N_DENSE, d, D_FF), d ** -0.5),
        'ffn_wd': nrm((N_DENSE, D_FF, d), D_FF ** -0.5),
        'router_w': nrm((N_MOE, d, N_EXPERTS), d ** -0.5),
        'router_b': nrm((N_MOE, N_EXPERTS), 0.01),
        'moe_wg': nrm((N_MOE, N_EXPERTS, d, D_EXPERT), d ** -0.5),
        'moe_wu': nrm((N_MOE, N_EXPERTS, d, D_EXPERT), d ** -0.5),
        'moe_wd': nrm((N_MOE, N_EXPERTS, D_EXPERT, d), D_EXPERT ** -0.5),
    }


def reference(x_prompt, x_sample, c_prompt, c_sample, state_lru_h, state_lru_conv,
              w_ada, b_ada, g_pre_mix, g_post_mix, g_pre_ffn, g_post_ffn,
              w_in, conv_w, conv_b, w_r, b_r, w_i, b_i, lru_lambda, g_v, w_s, b_s,
              w_pa, w_pb, w_o, ffn_wg, ffn_wu, ffn_wd,
              router_w, router_b, moe_wg, moe_wu, moe_wd):
    p = dict(w_ada=w_ada, b_ada=b_ada, g_pre_mix=g_pre_mix, g_post_mix=g_post_mix,
             g_pre_ffn=g_pre_ffn, g_post_ffn=g_post_ffn, w_in=w_in, conv_w=conv_w,
             conv_b=conv_b, w_r=w_r, b_r=b_r, w_i=w_i, b_i=b_i, lru_lambda=lru_lambda,
             g_v=g_v, w_s=w_s, b_s=b_s, w_pa=w_pa, w_pb=w_pb, w_o=w_o,
             ffn_wg=ffn_wg, ffn_wu=ffn_wu, ffn_wd=ffn_wd, router_w=router_w,
             router_b=router_b, moe_wg=moe_wg, moe_wu=moe_wu, moe_wd=moe_wd)
    h0_p = jnp.zeros((DEPTH, x_prompt.shape[0], W_A), x_prompt.dtype)
    conv0_p = jnp.zeros((DEPTH, x_prompt.shape[0], CONV_W - 1, W_A), x_prompt.dtype)
    y_prompt, h_p, conv_p, _ = trunk(x_prompt, c_prompt, h0_p, conv0_p, p)
    y_sample, h_s, conv_s, v_s = trunk(x_sample, c_sample, state_lru_h, state_lru_conv, p)
    return (y_prompt, y_sample, h_p, conv_p, h_s, conv_s, v_s)
```

```python
import contextlib
import numpy as np
import concourse.bass as bass
import concourse.mybir as mybir
from concourse.bass_utils import run_bass_kernel_spmd

F32 = mybir.dt.float32
BF16 = mybir.dt.bfloat16
AF = mybir.ActivationFunctionType
ALU = mybir.AluOpType
AX = mybir.AxisListType
GELU = AF.Gelu_apprx_tanh

NCORES = 4
NPASS = 2
D = 2048
KC = 16
T = 1152
TP = 1024
TS = 128
NB = 17
TU = 384
EPS = 1e-6
NVEC = 13
TW = 1216


class Ev:
    __slots__ = ("sem", "val")

    def __init__(self, sem, val):
        self.sem, self.val = sem, val


class Buf:
    def __init__(self, name):
        self.name = name
        self.wr = None
        self.rds = {}


class DSem:
    def __init__(self, key):
        self.key = key
        self.count = 0


class Prog:
    ENGS = ("pe", "act", "dve", "pool", "sp")

    def __init__(self):
        self.q = {e: [] for e in self.ENGS}
        self.cnt = {e: 0 for e in self.ENGS}
        self.dsems = []
        self.halt = False

    def new_dsem(self):
        d = DSem("d%d" % len(self.dsems))
        self.dsems.append(d)
        return d

    @staticmethod
    def _waits(reads, writes, extra):
        w = []
        for b in reads:
            if b.wr is not None:
                w.append(b.wr)
        for b in writes:
            if b.wr is not None:
                w.append(b.wr)
            for s, v in b.rds.items():
                w.append(Ev(s, v))
        for e in extra:
            if e is not None:
                w.append(e)
        return w

    def op(self, eng, fn, reads=(), writes=(), extra=(), sig=True):
        if self.halt:
            return None
        waits = self._waits(reads, writes, extra)
        ev = None
        if sig:
            self.cnt[eng] += 1
            ev = Ev(eng, self.cnt[eng])
            for b in reads:
                if b.rds.get(eng, 0) < ev.val:
                    b.rds[eng] = ev.val
            for b in writes:
                b.wr = ev
                b.rds = {}
        self.q[eng].append((fn, waits, eng if sig else None, 1))
        return ev

    def dma(self, queue, fn, dsem, reads=(), writes=(), extra=(), inc=16):
        if self.halt:
            return None
        waits = self._waits(reads, writes, extra)
        dsem.count += inc
        ev = Ev(dsem.key, dsem.count)
        for b in reads:
            if b.rds.get(dsem.key, 0) < ev.val:
                b.rds[dsem.key] = ev.val
        for b in writes:
            b.wr = ev
            b.rds = {}
        self.q[queue].append((fn, waits, dsem.key, inc))
        return ev

    def emit(self, name, eng, semh, final=()):
        seen = {}

        def wait(s, v):
            if seen.get(s, 0) < v:
                eng.wait_ge(semh[s], v)
                seen[s] = v

        for fn, waits, sem, inc in self.q[name]:
            need = {}
            for ev in waits:
                if need.get(ev.sem, 0) < ev.val:
                    need[ev.sem] = ev.val
            for s, v in need.items():
                wait(s, v)
            ins = fn(eng)
            if sem is not None:
                if inc == 1 and name == "pool":
                    ins.then_inc(semh[sem])
                else:
                    ins.then_inc(semh[sem], inc)
        for ev in final:
            wait(ev.sem, ev.val)


class _Stop(Exception):
    pass


def build(debug_stop=None):
    import os
    STOP = os.environ.get('MK_STOP', '')
    NOCC = os.environ.get('MK_NOCC', '') == '1'

    def stop(label):
        if STOP == label:
            P.halt = True
            print("STOPPED at", label)

    nc = bass.Bass("TRN2", target_bir_lowering=False)
    P = Prog()

    def din(name, shape, dt=F32):
        return nc.dram_tensor(name, list(shape), dt, kind="ExternalInput").ap()

    def dout(name, shape):
        return nc.dram_tensor(name, list(shape), F32, kind="ExternalOutput").ap()

    xtok = din("xtok", [NPASS, T, D])
    ctok = din("ctok", [NPASS, NB, D])
    st_in = din("st_in", [NPASS, 2, 64, D])
    sel_d = din("sel", [128, 8])
    vecs_d = din("vecs", [128, 2 * NVEC * KC])
    bada_d = din("bada", [128, 2 * 96])
    ident_d = din("ident", [128, 128])
    mask_d = din("mask", [128, 128])
    w_ada = din("w_ada", [2, D, 6 * D])
    w_in = din("w_in", [2, D, 6 * D])
    w_r = din("w_r", [2, 16, 128, 128])
    w_i = din("w_i", [2, 16, 128, 128])
    w_s = din("w_s", [2, 16, 128, 128])
    b_s = din("b_s", [2, 16, 128])
    w_pa = din("w_pa", [2, D, D])
    w_pb = din("w_pb", [2, D, D])
    w_o = din("w_o", [2, D, D])
    ffn_wg = din("ffn_wg", [1, D, 3 * D])
    ffn_wu = din("ffn_wu", [1, D, 3 * D])
    ffn_wd = din("ffn_wd", [1, 3 * D, D])
    router_w = din("router_w", [1, D, 8])
    router_b = din("router_b", [8, 1])
    moe_wg = din("moe_wg", [1, 8, D, D])
    moe_wu = din("moe_wu", [1, 8, D, D])
    moe_wd = din("moe_wd", [1, 8, D, D])

    yout = dout("yout", [NPASS, T, D])
    stout = dout("stout", [NPASS, 2, 68, D])
    vout = dout("vout", [NPASS, 2, 128, D])

    xres = nc.dram_tensor("xres", [KC, 128, T], F32).ap()
    modd = nc.dram_tensor("modd", [2, 128, 96 * NB], F32).ap()
    std = nc.dram_tensor("std", [2, 128, KC * 64], F32).ap()
    agx_in = nc.dram_tensor("agx_in", [128, 48], F32)
    agx_out = nc.dram_tensor("agx_out", [NCORES * 128, 48], F32)
    agh_in = nc.dram_tensor("agh_in", [128, 16], F32)
    agh_out = nc.dram_tensor("agh_out", [NCORES * 128, 16], F32)

    es = contextlib.ExitStack()
    with es:
        def sb(name, shape, dt):
            return es.enter_context(nc.sbuf_tensor(name, list(shape), dt))

        R12 = sb("R12", [128, 2 * KC * T], BF16)
        R03 = sb("R03", [128, 2 * KC * T], BF16)
        WS = sb("WS", [128, 4, 2048], BF16)
        SCR = sb("SCR", [128, 3 * TW], F32)
        IDENT = sb("IDENT", [128, 128], F32)
        MASK = sb("MASK", [128, 128], F32)
        ONESB = sb("ONESB", [128, 128], BF16)
        ONESF = sb("ONESF", [128, 128], F32)
        VECS = sb("VECS", [128, 2 * NVEC * KC], F32)
        BADA = sb("BADA", [128, 192], F32)
        SEL = sb("SEL", [128, 8], F32)
        FLAG = sb("FLAG", [128, 1], F32)
        MOD = sb("MOD", [128, 1, 3 * KC * NB], F32)
        BS8 = sb("BS8", [128, 128], F32)
        CT = sb("CT", [128, KC * NB], BF16)
        WMT = sb("WMT", [128, KC * 128], BF16)
        BD = sb("BD", [128, KC * 128], BF16)
        BSR = sb("BSR", [33, KC * 128], BF16)
        OUTS = sb("OUTS", [128, KC * 68], F32)
        SM = sb("SM", [128, 768], F32)
        XNT = sb("XNT", [128, KC * 4], BF16)
        AGL = sb("AGL", [128, 8 * 48], F32)
        RW = sb("RW", [128, KC * 8], F32)
        RB = sb("RB", [8, 1], F32)
        PS = es.enter_context(nc.psum_tensor("PS", [128, 8, 512], F32))
        print('SBUF remaining', nc.sbuf_bytes_remaining)

        R1 = R12[:, 0:KC * T]
        R2 = R12[:, KC * T:2 * KC * T]
        R0 = R03[:, 0:KC * T]
        R3 = R03[:, KC * T:2 * KC * T]
        Yf = R12[:].bitcast(F32).rearrange("p (c t) -> p c t", t=T)
        XNEW = R03[:].bitcast(F32).rearrange("p (c t) -> p c t", t=T)
        R0f = R0.bitcast(F32)
        R2f = R2.bitcast(F32)
        R3f = R3.bitcast(F32)
        R12f = R12[:].bitcast(F32)

        def v3(ap, n):
            return ap.rearrange("p (c n) -> p c n", n=n)

        XN1 = v3(R1, T)
        XN3 = v3(R3[:, 0:KC * T], T)
        YA = v3(R2, T)
        Mv = v3(R3, T)
        Qv = v3(R3[:, 0:KC * TP], TP)
        H0C = v3(R3f[:, 8192:9216], 64)
        VN = v3(R0, D)
        Hg = v3(R0, T)
        VECv = VECS[:].rearrange("p (l v c) -> p l v c", l=2, v=NVEC)
        BADAv = BADA[:].rearrange("p (l n) -> p l n", l=2)
        MODv = MOD[:].rearrange("p s (k c b) -> p s k c b", k=3, c=KC)
        CTv = v3(CT[:], NB)
        WMTv = v3(WMT[:], 128)
        BDv = v3(BD[:], 128)
        OUTSv = v3(OUTS[:], 68)
        XNTv = v3(XNT[:], 4)
        AGLv = v3(AGL[:], 48)
        RWv = v3(RW[:], 8)

        def r0t(i):
            return R0f[:, i * TW:(i + 1) * TW]

        def sct(i):
            return SCR[:, i * TW:(i + 1) * TW]

        RSV = SM[:, 0:9]
        SSV = SM[:, 512:656]
        HLL = SM[:, 96:112]
        PLL = SM[:, 112:128]
        HIN = SM[:, 128:144]
        CL = SM[:, 144:160]
        CL2 = SM[:, 160:176]
        TX = SM[:, 176:224]
        TXS = SM[:, 224:272]
        RST = SM[:, 272:275]
        LT = SM[:, 288:360]
        LE = SM[:, 360:432]
        M1 = SM[:, 432:441]
        M2 = SM[:, 448:457]
        TMPS = SM[:, 464:480]

        b_R0, b_R1, b_R2, b_R3 = Buf("R0"), Buf("R1"), Buf("R2"), Buf("R3")
        b_ws = [Buf("ws%d" % i) for i in range(4)]
        b_scr = [Buf("scr0"), Buf("scr1"), Buf("scr2")]
        b_r0t = [Buf("r0t%d" % i) for i in range(7)]
        b_ps = [Buf("ps%d" % i) for i in range(8)]
        b_const = Buf("const")
        b_mod = [Buf("mod0"), Buf("mod1")]
        b_ct = Buf("ct")
        b_wmt, b_bd, b_bsr, b_bs8 = Buf("wmt"), Buf("bd"), Buf("bsr"), Buf("bs8")
        b_outs, b_sm, b_xnt, b_agl = Buf("outs"), Buf("sm"), Buf("xnt"), Buf("agl")
        b_xres = [Buf("xres%d" % j) for j in range(KC)]
        b_modd = [Buf("modd0"), Buf("modd1")]
        b_std = [Buf("std0"), Buf("std1")]
        b_agx, b_agxo, b_agh, b_agho = Buf("agx"), Buf("agxo"), Buf("agh"), Buf("agho")
        b_h0c = Buf("h0c")

        ds_setup = P.new_dsem()
        ds_ws = [P.new_dsem() for _ in range(4)]
        ds_misc = [P.new_dsem() for _ in range(4)]
        ds_out = P.new_dsem()
        ds_x = [P.new_dsem(), P.new_dsem(), P.new_dsem()]
        ds_cc = P.new_dsem()
        out_events = []

        bank_rr = [0]

        def nb():
            b = bank_rr[0]
            bank_rr[0] = (b + 1) % 5
            return b

        def act(fn, reads=(), writes=()):
            return P.op("act", fn, reads, writes)

        def dve(fn, reads=(), writes=()):
            return P.op("dve", fn, reads, writes)

        def A_(out, in_, func, bias=None, scale=None, accum_out=None):
            kw = {}
            if bias is not None:
                kw["bias"] = bias
            if scale is not None:
                kw["scale"] = scale
            if accum_out is not None:
                kw["accum_out"] = accum_out
            return lambda e: e.activation(out=out, in_=in_, func=func, **kw)

        def TT(out, in0, in1, op):
            return lambda e: e.tensor_tensor(out=out, in0=in0, in1=in1, op=op)

        def TS_(out, in0, s1, s2, op0, op1=None):
            if op1 is None:
                return lambda e: e.tensor_scalar(out=out, in0=in0, scalar1=s1, scalar2=None, op0=op0)
            return lambda e: e.tensor_scalar(out=out, in0=in0, scalar1=s1, scalar2=s2, op0=op0, op1=op1)

        def STT(out, in0, scalar, in1, op0, op1):
            return lambda e: e.scalar_tensor_tensor(out=out, in0=in0, scalar=scalar, in1=in1, op0=op0, op1=op1)

        def CP(out, in_):
            return lambda e: e.tensor_copy(out=out, in_=in_)

        def mm_group(out, pairs, reads, writes, extra_first=()):
            n = len(pairs)
            ev = None
            for i, (l, r) in enumerate(pairs):
                fn = (lambda e, l=l, r=r, i=i: e.matmul(out, lhsT=l, rhs=r, start=(i == 0), stop=(i == n - 1)))
                last = (i == n - 1)
                if i == 0 or last:
                    ev = P.op("pe", fn, reads, writes, extra=extra_first if i == 0 else (), sig=last)
                else:
                    P.op("pe", fn, (), (), sig=False)
            return ev

        def transpose(out, in_, ident, reads, writes):
            return P.op("pe", lambda e: e.transpose(out, in_, ident), reads, writes)

        wcount = [0]

        def wq(*parts):
            i = wcount[0]
            wcount[0] += 1
            sl = i % 4
            for (off, n, inner, src) in parts:
                dst = WS[:, sl, off:off + n].rearrange("p (a b) -> p a b", b=inner)
                P.dma("pool", (lambda e, dst=dst, src=src: e.dma_start(out=dst, in_=src)),
                      ds_ws[sl], reads=(), writes=(b_ws[sl],))
            return sl

        def kblock(W2d, c0, ncols=128):
            src = W2d.rearrange("(c p) n -> p c n", p=128)[:, :, c0:c0 + ncols]
            return ((0, KC * ncols, ncols, src),)

        def wsv(sl, ncols=128):
            return WS[:, sl, 0:KC * ncols].rearrange("p (c n) -> p c n", n=ncols)

        def alias(frm, to):
            for t in to:
                for f in frm:
                    if f.wr is not None and t.rds.get(f.wr.sem, 0) < f.wr.val:
                        t.rds[f.wr.sem] = f.wr.val
                    for sk, v in f.rds.items():
                        if t.rds.get(sk, 0) < v:
                            t.rds[sk] = v

        def new_ds():
            return P.new_dsem()

        SELT = sb("SELT", [8, 128], F32)
        ONE1 = ONESF[:, 0:1]
        b_selt = Buf("selt")
        ds_std = [new_ds(), new_ds()]
        ds_modd = [new_ds(), new_ds()]
        ds_mod = [new_ds(), new_ds()]
        ds_h0c, ds_wmt, ds_bd, ds_bs, ds_ag, ds_agl = new_ds(), new_ds(), new_ds(), new_ds(), new_ds(), new_ds()
        ds_xres = new_ds()
        out_ds = []

        def out_store(fn, stage_bufs):
            d = new_ds()
            out_ds.append(d)
            return P.dma("sp", fn, d, reads=stage_bufs)

        def sload(dst, src):
            P.dma("sp", (lambda e: e.dma_start(out=dst, in_=src)), ds_setup, writes=(b_const,))

        sload(IDENT[:], ident_d)
        sload(MASK[:], mask_d)
        sload(VECS[:], vecs_d)
        sload(BADA[:], bada_d)
        sload(SEL[:], sel_d)
        sload(RW[:].rearrange("p (c e) -> p c e", e=8), router_w[0].rearrange("(c p) e -> p c e", p=128))
        sload(RB[:], router_b)
        C17 = R0f[0:NB, 0:D]
        STI = [R0f[0:64, D:2 * D], R0f[0:64, 2 * D:3 * D]]
        TXSV = sb("TXSV", [128, 2 * 48], F32)
        HSV = sb("HSV", [128, 2 * 16], F32)
        b_sv = Buf("sv")
        dve(lambda e: e.memset(ONESB[:], 1.0), writes=(b_ct,))
        dve(lambda e: e.memset(ONESF[:], 1.0), writes=(b_ct,))
        EPSC = SM[:, 500:501]
        dve(lambda e: e.memset(EPSC, EPS), writes=(b_ct,))
        dve(lambda e: e.tensor_reduce(out=FLAG[:], in_=SEL[:], axis=AX.X, op=ALU.add), reads=(b_const,), writes=(b_sm,))

        b_xt = [Buf("xt0"), Buf("xt1")]
        b_xf = [Buf("xf0"), Buf("xf1")]
        for pas in range(NPASS):
            alias([b_R0, b_R1, b_R2, b_R3] + b_r0t + b_scr, [b_const] + b_xt + b_xf)
            sload(C17, ctok[pas])
            sload(STI[0], st_in[pas][0])
            sload(STI[1], st_in[pas][1])
            bk = nb()
            for j in range(KC):
                transpose(PS[:, bk, j * NB:(j + 1) * NB], C17[:, j * 128:(j + 1) * 128], IDENT[0:NB, 0:NB],
                          reads=(b_const,), writes=(b_ps[bk],))
            act(A_(CT[:], PS[:, bk, 0:KC * NB], AF.Silu), reads=(b_ps[bk], b_ct), writes=(b_ct,))
            for l in range(2):
                H0T = R0f[:, 3 * D + l * 1024:3 * D + (l + 1) * 1024]
                for half in range(2):
                    bk = nb()
                    for jj in range(8):
                        j = half * 8 + jj
                        transpose(PS[:, bk, jj * 64:(jj + 1) * 64], STI[l][:, j * 128:(j + 1) * 128], IDENT[0:64, 0:64],
                                  reads=(b_const,), writes=(b_ps[bk],))
                    dve(CP(H0T[:, half * 512:(half + 1) * 512], PS[:, bk, :]), reads=(b_ps[bk],), writes=(b_R0,))
                P.dma("sp", (lambda e, H0T=H0T, l=l: e.dma_start(out=std[l], in_=H0T)), ds_std[l],
                      reads=(b_R0,), writes=(b_std[l],))

            stop("p0")
            for l in range(2):
                MODT = R3f[:, l * 96 * NB:(l + 1) * 96 * NB]
                MODTv = MODT.rearrange("p (n b) -> p n b", b=NB)
                for tno in range(96):
                    s = wq(*kblock(w_ada[l], tno * 128))
                    wv = wsv(s)
                    if tno % 8 == 0:
                        bk = nb()
                    n = tno % 8
                    mm_group(PS[:, bk, n * NB:(n + 1) * NB],
                             [(wv[:, k, :], CTv[:, k, :]) for k in range(KC)],
                             reads=(b_ws[s], b_ct), writes=(b_ps[bk],))
                    act(A_(MODTv[:, tno, :], PS[:, bk, n * NB:(n + 1) * NB], AF.Identity,
                           bias=BADAv[:, l, tno:tno + 1]), reads=(b_ps[bk], b_const), writes=(b_R3,))
                for (kind, vec, addone) in ((1, 0, True), (2, 1, False), (4, 2, True), (5, 3, False)):
                    sl = MODTv[:, kind * KC:(kind + 1) * KC, :]
                    gb = VECv[:, l, vec, :].unsqueeze(2).broadcast_to([128, KC, NB])
                    if addone:
                        dve(TS_(sl, sl, 1.0, None, ALU.add), reads=(b_R3,), writes=(b_R3,))
                    dve(TT(sl, sl, gb, ALU.mult), reads=(b_R3, b_const), writes=(b_R3,))
                P.dma("sp", (lambda e, MODT=MODT, l=l: e.dma_start(out=modd[l], in_=MODT)), ds_modd[l],
                      reads=(b_R3,), writes=(b_modd[l],))

            stop("p1")
            SSB = (5, 6, 7)

            def rstd_from_ss(dst_tile, dst_buf):
                for tt in range(3):
                    act(A_(dst_tile[:, tt * TU:(tt + 1) * TU], PS[:, SSB[tt], 0:TU], AF.Sqrt, bias=EPSC, scale=1.0 / D),
                        reads=(b_ps[SSB[tt]], b_ct), writes=(dst_buf,))
                dve(lambda e: e.reciprocal(out=dst_tile[:, 0:T], in_=dst_tile[:, 0:T]), reads=(dst_buf,), writes=(dst_buf,))

            SQ = sct(2).bitcast(BF16)
            xresv = xres.rearrange("c p t -> p c t")
            for i in range(9):
                XT = R12f[:, (i % 2) * D:(i % 2 + 1) * D]
                XF = R12f[:, 2 * D + (i % 2) * D:2 * D + (i % 2 + 1) * D]
                XFv = v3(XF, 128)
                P.dma("sp", (lambda e, XT=XT, i=i, pas=pas: e.dma_start(out=XT, in_=xtok[pas][i * 128:(i + 1) * 128, :])),
                      ds_x[i % 2], writes=(b_xt[i % 2],))
                for q in range(4):
                    bk = nb()
                    for jj in range(4):
                        j = q * 4 + jj
                        transpose(PS[:, bk, jj * 128:(jj + 1) * 128], XT[:, j * 128:(j + 1) * 128], IDENT[:],
                                  reads=(b_xt[i % 2], b_const), writes=(b_ps[bk],))
                    if q % 2 == 0:
                        act(A_(XF[:, q * 512:(q + 1) * 512], PS[:, bk, :], AF.Copy), reads=(b_ps[bk],), writes=(b_xf[i % 2],))
                    else:
                        dve(CP(XF[:, q * 512:(q + 1) * 512], PS[:, bk, :]), reads=(b_ps[bk],), writes=(b_xf[i % 2],))
                act(A_(SQ[:, 0:D], XF, AF.Square), reads=(b_xf[i % 2],), writes=(b_scr[2],))
                tt, off = (i * 128) // TU, (i * 128) % TU
                mm_group(PS[:, SSB[tt], off:off + 128], [(ONESB[:], SQ[:, j * 128:(j + 1) * 128]) for j in range(KC)],
                         reads=(b_scr[2], b_ct), writes=(b_ps[SSB[tt]],))
                P.dma("sp", (lambda e, XFv=XFv, i=i: e.dma_start(out=xresv[:, :, i * 128:(i + 1) * 128], in_=XFv)),
                      ds_xres, reads=(b_xf[i % 2],), writes=tuple(b_xres))
                if i == 7:
                    act(A_(v3(TXS, 3), XFv[:, :, 125:128], AF.Copy), reads=(b_xf[i % 2],), writes=(b_sm,))
            alias((b_const, b_R0), b_r0t)
            alias(b_xt + b_xf, (b_R1, b_R2))
            RS = r0t(0)
            rstd_from_ss(RS, b_r0t[0])

            stop("p2")
            def load_mod(l, kind):
                slot = 0
                P.dma("sp", (lambda e: e.dma_start(out=MOD[:, slot, :],
                                                   in_=modd[l][:, kind * 3 * KC * NB:(kind + 1) * 3 * KC * NB])),
                      ds_mod[slot], reads=(b_modd[l],), writes=(b_mod[slot],))
                return slot

            ag_count = [0]

            def allgather(src_sb, src_buf, ag_in_unused, ag_out_unused, b_in_unused, b_out_unused, dst_sb, dst_buf):
                i = ag_count[0]
                ag_count[0] += 1
                ncol = dst_sb.shape[-1]
                ag_in = nc.dram_tensor("agi%d" % i, [128, ncol], F32)
                ag_out = nc.dram_tensor("ago%d" % i, [NCORES * 128, ncol], F32)
                b_in, b_out = Buf("agi%d" % i), Buf("ago%d" % i)
                d_in, d_cc, d_ld = new_ds(), new_ds(), new_ds()
                if NOCC:
                    P.dma("pool", (lambda e: e.dma_start(out=ag_out.ap()[0:128, :], in_=src_sb)), d_in, reads=(src_buf,), writes=(b_out,))
                else:
                    P.dma("pool", (lambda e: e.dma_start(out=ag_in[:, :], in_=src_sb)), d_in, reads=(src_buf,), writes=(b_in,))
                    P.dma("pool", (lambda e: e.collective_compute("AllGather", ALU.bypass, replica_groups=[list(range(NCORES))],
                                                                  ins=[ag_in.ap().opt()], outs=[ag_out.ap().opt()])),
                          d_cc, reads=(b_in,), writes=(b_out,), inc=1)
                P.dma("pool", (lambda e: e.dma_start(out=dst_sb, in_=ag_out.ap().rearrange("(r p) n -> p r n", p=128))),
                      d_ld, reads=(b_out,), writes=(dst_buf,))

            def sel_combine(dst, ncol, dst_buf):
                G = AGLv
                dve(TS_(dst, G[:, 0, 0:ncol], SEL[:, 0:1], None, ALU.mult), reads=(b_agl, b_const), writes=(dst_buf,))
                for r in range(1, 8):
                    dve(STT(dst, G[:, r, 0:ncol], SEL[:, r:r + 1], dst, ALU.mult, ALU.add), reads=(b_agl, b_const, dst_buf),
                        writes=(dst_buf,))

            def xn_pass(l, kind, XNd, b_XNd, moe=False):
                ms = 0
                SH, SC = MODv[:, ms, 0], MODv[:, ms, 1]
                XB = [sct(0), sct(1)]
                TMP = r0t(1)
                XNF = [r0t(2), r0t(3)]
                for j in range(KC):
                    xb, bxb = XB[j % 2], b_scr[j % 2]
                    xf, bxf = XNF[j % 2], b_r0t[2 + j % 2]
                    P.dma("sp", (lambda e, xb=xb, j=j: e.dma_start(out=xb[:, 0:T], in_=xres[j])), ds_x[j % 2],
                          reads=(b_xres[j],), writes=(bxb,))
                    dve(TT(TMP[:, 0:T], xb[:, 0:T], RS[:, 0:T], ALU.mult), reads=(bxb, b_r0t[0]), writes=(b_r0t[1],))
                    act(A_(xf[:, 0:TP], TMP[:, 0:TP], AF.Identity, bias=SH[:, j, 0:1], scale=SC[:, j, 0:1]),
                        reads=(b_r0t[1], b_mod[ms]), writes=(bxf,))
                    ts3 = TMP[:, TP:T].rearrange("p (b t) -> p b t", t=8)
                    xs3 = xf[:, TP:T].rearrange("p (b t) -> p b t", t=8)
                    dve(TT(ts3, ts3, SC[:, j, 1:NB].unsqueeze(2).broadcast_to([128, 16, 8]), ALU.mult),
                        reads=(b_r0t[1], b_mod[ms]), writes=(b_r0t[1],))
                    dve(TT(xs3, ts3, SH[:, j, 1:NB].unsqueeze(2).broadcast_to([128, 16, 8]), ALU.add),
                        reads=(b_r0t[1], b_mod[ms]), writes=(bxf,))
                    dve(CP(XNd[:, j, :], xf[:, 0:T]), reads=(bxf,), writes=(b_XNd,))
                    if moe:
                        for tt in range(3):
                            P.op("pe", (lambda e, j=j, tt=tt, xf=xf: e.matmul(PS[0:8, SSB[tt], 0:TU], lhsT=RWv[:, j, :],
                                                                              rhs=xf[:, tt * TU:(tt + 1) * TU],
                                                                              start=(j == 0), stop=(j == KC - 1))),
                                 reads=(bxf, b_const), writes=(b_ps[SSB[tt]],))

            def update_pass(l, kind, final):
                ms = 0
                GT = MODv[:, ms, 2]
                XB = [sct(0), sct(1)]
                TMP = r0t(2)
                RSY = r0t(1)
                SQb = sct(2).bitcast(BF16)
                for j in range(KC):
                    xb, bxb = XB[j % 2], b_scr[j % 2]
                    P.dma("sp", (lambda e, xb=xb, j=j: e.dma_start(out=xb[:, 0:T], in_=xres[j])), ds_x[j % 2],
                          reads=(b_xres[j],), writes=(bxb,))
                    dve(TT(TMP[:, 0:T], Yf[:, j, :], RSY[:, 0:T], ALU.mult), reads=(b_R1, b_R2, b_r0t[1]), writes=(b_r0t[2],))
                    dve(STT(xb[:, 0:TP], TMP[:, 0:TP], GT[:, j, 0:1], xb[:, 0:TP], ALU.mult, ALU.add),
                        reads=(b_r0t[2], b_mod[ms], bxb), writes=(bxb,))
                    ts3 = TMP[:, TP:T].rearrange("p (b t) -> p b t", t=8)
                    xs3 = xb[:, TP:T].rearrange("p (b t) -> p b t", t=8)
                    dve(TT(ts3, ts3, GT[:, j, 1:NB].unsqueeze(2).broadcast_to([128, 16, 8]), ALU.mult),
                        reads=(b_r0t[2], b_mod[ms]), writes=(b_r0t[2],))
                    dve(TT(xs3, xs3, ts3, ALU.add), reads=(b_r0t[2], bxb), writes=(bxb,))
                    if not final:
                        P.dma("sp", (lambda e, xb=xb, j=j: e.dma_start(out=xres[j], in_=xb[:, 0:T])), ds_x[j % 2],
                              reads=(bxb,), writes=(b_xres[j],))
                        act(A_(SQb[:, 0:T], xb[:, 0:T], AF.Square), reads=(bxb,), writes=(b_scr[2],))
                        for tt in range(3):
                            P.op("pe", (lambda e, j=j, tt=tt: e.matmul(PS[:, SSB[tt], 0:TU], lhsT=ONESB[:],
                                                                       rhs=SQb[:, tt * TU:(tt + 1) * TU],
                                                                       start=(j == 0), stop=(j == KC - 1))),
                                 reads=(b_scr[2], b_ct), writes=(b_ps[SSB[tt]],))
                        if kind == 1:
                            act(A_(TXS[:, j * 3:(j + 1) * 3], xb[:, TP - 3:TP], AF.Copy), reads=(bxb,), writes=(b_sm,))
                    else:
                        act(A_(Yf[:, j, :], xb[:, 0:T], AF.Copy), reads=(bxb, b_r0t[2]), writes=(b_R1, b_R2))
                if not final:
                    rstd_from_ss(RS, b_r0t[0])

            def y_stats_and_store(j, tt, bk, first, last_group):
                ysl = Yf[:, j, tt * TU:(tt + 1) * TU]
                if first:
                    act(A_(ysl, PS[:, bk, 0:TU], AF.Copy), reads=(b_ps[bk],), writes=(b_R1, b_R2))
                else:
                    dve(TT(ysl, PS[:, bk, 0:TU], ysl, ALU.add), reads=(b_ps[bk], b_R1, b_R2), writes=(b_R1, b_R2))
                if last_group:
                    SQb = sct(2).bitcast(BF16)
                    sq = SQb[:, (j % 2) * TW + tt * TU:(j % 2) * TW + (tt + 1) * TU]
                    act(A_(sq, ysl, AF.Square), reads=(b_R1, b_R2), writes=(b_scr[2],))
                    P.op("pe", (lambda e: e.matmul(PS[:, SSB[tt], 0:TU], lhsT=ONESB[:], rhs=sq,
                                                   start=(j == 0), stop=(j == KC - 1))),
                         reads=(b_scr[2], b_ct), writes=(b_ps[SSB[tt]],))

            def proj_units(W2d, c0, rhs3, b_rhs, consume):
                s = wq(*kblock(W2d, c0))
                wv = wsv(s)
                for tt in range(3):
                    bk = nb()
                    mm_group(PS[:, bk, 0:TU], [(wv[:, k, :], rhs3[:, k, tt * TU:(tt + 1) * TU]) for k in range(KC)],
                             reads=(b_ws[s], b_rhs), writes=(b_ps[bk],))
                    consume(tt, bk)

            def pair_units(WA, cA, rhsA, b_rhsA, WB, cB, rhsB, b_rhsB, consume):
                sA = wq(*kblock(WA, cA))
                sB = wq(*kblock(WB, cB))
                wvA, wvB = wsv(sA), wsv(sB)
                for tt in range(3):
                    sl = slice(tt * TU, (tt + 1) * TU)
                    ba, bb = nb(), nb()
                    mm_group(PS[:, ba, 0:TU], [(wvA[:, k, :], rhsA[:, k, sl]) for k in range(KC)],
                             reads=(b_ws[sA], b_rhsA), writes=(b_ps[ba],))
                    mm_group(PS[:, bb, 0:TU], [(wvB[:, k, :], rhsB[:, k, sl]) for k in range(KC)],
                             reads=(b_ws[sB], b_rhsB), writes=(b_ps[bb],))
                    consume(tt, ba, bb)

            def mixer(l):
                ms = load_mod(l, 0)
                SH, SC = MODv[:, ms, 0], MODv[:, ms, 1]
                if pas == 0:
                    dve(CP(TXSV[:, l * 48:(l + 1) * 48], TXS), reads=(b_sm,), writes=(b_sv,))
                else:
                    dve(CP(TX, TXSV[:, l * 48:(l + 1) * 48]), reads=(b_sv,), writes=(b_sm,))
                TXv = v3(TX, 3)
                sqt = SQ[:, 0:48]
                act(A_(sqt, TX, AF.Square), reads=(b_sm,), writes=(b_scr[2],))
                bk = nb()
                mm_group(PS[:, bk, 0:3], [(ONESB[:], sqt[:, j * 3:(j + 1) * 3]) for j in range(KC)],
                         reads=(b_scr[2], b_ct), writes=(b_ps[bk],))
                act(A_(RST, PS[:, bk, 0:3], AF.Sqrt, bias=EPSC, scale=1.0 / D), reads=(b_ps[bk], b_ct), writes=(b_sm,))
                dve(lambda e: e.reciprocal(out=RST, in_=RST), reads=(b_sm,), writes=(b_sm,))
                dve(TT(TXv, TXv, RST.unsqueeze(1).broadcast_to([128, KC, 3]), ALU.mult), reads=(b_sm,), writes=(b_sm,))
                dve(TT(TXv, TXv, SC[:, :, 0:1].broadcast_to([128, KC, 3]), ALU.mult), reads=(b_sm, b_mod[ms]), writes=(b_sm,))
                dve(TT(TXv, TXv, SH[:, :, 0:1].broadcast_to([128, KC, 3]), ALU.add), reads=(b_sm, b_mod[ms]), writes=(b_sm,))
                if pas == 0:
                    dve(lambda e: e.memset(XNT[:], 0.0), reads=(b_sm,), writes=(b_xnt,))
                else:
                    dve(CP(XNTv[:, :, 0:3], TXv), reads=(b_sm,), writes=(b_xnt,))

                stop("m%d_tail" % l)
                xn_pass(l, 0, XN1, b_R1)
                stop("m%d_xn" % l)

                P.dma("sp", (lambda e: e.dma_start(out=H0C.rearrange("p c n -> p (c n)"), in_=std[l])), ds_h0c,
                      reads=(b_std[l],), writes=(b_h0c, b_R3))
                lam = VECv[:, l, 11, :]
                act(A_(CL, lam, AF.Exp, scale=-1.0), reads=(b_const,), writes=(b_sm,))
                act(A_(CL, CL, AF.Ln, bias=ONE1), reads=(b_sm, b_ct), writes=(b_sm,))
                dve(TS_(CL2, CL, -16.0, None, ALU.mult), reads=(b_sm,), writes=(b_sm,))
                dve(TS_(CL, CL, -8.0, None, ALU.mult), reads=(b_sm,), writes=(b_sm,))

                for h in range(KC):
                    proj_units(w_in[l], D + h * 128, XN1, b_R1,
                               lambda tt, bk, h=h: act(A_(YA[:, h, tt * TU:(tt + 1) * TU], PS[:, bk, 0:TU], GELU),
                                                       reads=(b_ps[bk],), writes=(b_R2,)))

                stop("m%d_ga" % l)
                cw = [VECv[:, l, 4 + k, :] for k in range(4)]
                cb, br, bi = VECv[:, l, 8, :], VECv[:, l, 9, :], VECv[:, l, 10, :]
                for h in range(KC):
                    sA = wq(*kblock(w_in[l], h * 128))
                    sB = wq((0, 128, 128, w_r[l][h]), (128, 128, 128, w_i[l][h]))
                    wvA = wsv(sA)
                    wR, wI = WS[:, sB, 0:128], WS[:, sB, 128:256]
                    XA, bXA = r0t(h % 2), b_r0t[h % 2]
                    XC, bXC = r0t(2), b_r0t[2]
                    Rt, bR = r0t(3), b_r0t[3]
                    It, bI = r0t(4), b_r0t[4]
                    At, bA = r0t(5), b_r0t[5]
                    St, bS = r0t(6), b_r0t[6]
                    HL, bHL = sct(0), b_scr[0]
                    Pt, bP = sct(1), b_scr[1]
                    XCb, bXCb = sct(2).bitcast(BF16), b_scr[2]
                    XAs = XA[:, 3 + TP:3 + TP + 176].rearrange("p (b t) -> p b t", t=11)
                    lw = [wvA[:, k, :] for k in range(KC)]
                    b0 = nb()
                    mm_group(PS[:, b0, TU:TU + 3], [(lw[k], XNTv[:, k, 0:3]) for k in range(KC)],
                             reads=(b_ws[sA], b_xnt), writes=(b_ps[b0],))
                    banks = [b0, None, None]
                    for tt in range(3):
                        bk = b0 if tt == 0 else nb()
                        banks[tt] = bk
                        mm_group(PS[:, bk, 0:TU], [(lw[k], XN1[:, k, tt * TU:(tt + 1) * TU]) for k in range(KC)],
                                 reads=(b_ws[sA], b_R1), writes=(b_ps[bk],))
                    act(A_(XA[:, 0:3], PS[:, b0, TU:TU + 3], AF.Copy), reads=(b_ps[b0],), writes=(bXA,))
                    act(A_(XA[:, 3:3 + TU], PS[:, b0, 0:TU], AF.Copy), reads=(b_ps[b0],), writes=(bXA,))
                    act(A_(XA[:, 3 + TU:3 + 2 * TU], PS[:, banks[1], 0:TU], AF.Copy), reads=(b_ps[banks[1]],), writes=(bXA,))
                    act(A_(XA[:, 3 + 2 * TU:3 + TP], PS[:, banks[2], 0:256], AF.Copy), reads=(b_ps[banks[2]],), writes=(bXA,))
                    dve(CP(XAs[:, :, 3:11], PS[:, banks[2], 256:TU].rearrange("p (b t) -> p b t", t=8)),
                        reads=(b_ps[banks[2]],), writes=(bXA,))
                    dve(CP(XAs[:, :, 0:3], H0C[:, h, 16:64].rearrange("p (b k) -> p b k", k=3)), reads=(b_h0c,), writes=(bXA,))
                    dve(CP(OUTSv[:, h, 17:20], XA[:, TP:TP + 3]), reads=(bXA,), writes=(b_outs,))
                    dve(CP(OUTSv[:, h, 20:68].rearrange("p (b k) -> p b k", k=3), XAs[:, :, 8:11]), reads=(bXA,), writes=(b_outs,))
                    XCs = XC[:, TP:T].rearrange("p (b t) -> p b t", t=8)
                    act(A_(XC[:, 0:TP], XA[:, 3:3 + TP], AF.Identity, bias=cb[:, h:h + 1], scale=cw[3][:, h:h + 1]),
                        reads=(bXA, b_const), writes=(bXC,))
                    act(A_(XCs, XAs[:, :, 3:11], AF.Identity, bias=cb[:, h:h + 1], scale=cw[3][:, h:h + 1]),
                        reads=(bXA, b_const), writes=(bXC,))
                    for k in range(3):
                        dve(STT(XC[:, 0:TP], XA[:, k:k + TP], cw[k][:, h:h + 1], XC[:, 0:TP], ALU.mult, ALU.add),
                            reads=(bXA, b_const, bXC), writes=(bXC,))
                        dve(STT(XCs, XAs[:, :, k:k + 8], cw[k][:, h:h + 1], XCs, ALU.mult, ALU.add),
                            reads=(bXA, b_const, bXC), writes=(bXC,))
                    dve(CP(XCb[:, 0:T], XC[:, 0:T]), reads=(bXC,), writes=(bXCb,))
                    for tt in range(3):
                        bkr, bki = nb(), nb()
                        mm_group(PS[:, bkr, 0:TU], [(wR, XCb[:, tt * TU:(tt + 1) * TU])],
                                 reads=(b_ws[sB], bXCb), writes=(b_ps[bkr],))
                        mm_group(PS[:, bki, 0:TU], [(wI, XCb[:, tt * TU:(tt + 1) * TU])],
                                 reads=(b_ws[sB], bXCb), writes=(b_ps[bki],))
                        act(A_(Rt[:, tt * TU:(tt + 1) * TU], PS[:, bkr, 0:TU], AF.Sigmoid, bias=br[:, h:h + 1]),
                            reads=(b_ps[bkr], b_const), writes=(bR,))
                        act(A_(It[:, tt * TU:(tt + 1) * TU], PS[:, bki, 0:TU], AF.Sigmoid, bias=bi[:, h:h + 1]),
                            reads=(b_ps[bki], b_const), writes=(bI,))
                    act(A_(At[:, 0:T], Rt[:, 0:T], AF.Exp, scale=CL[:, h:h + 1]), reads=(bR, b_sm), writes=(bA,))
                    act(A_(St[:, 0:T], Rt[:, 0:T], AF.Exp, scale=CL2[:, h:h + 1]), reads=(bR, b_sm), writes=(bS,))
                    dve(TS_(St[:, 0:T], St[:, 0:T], -1.0, 1.0, ALU.mult, ALU.add), reads=(bS,), writes=(bS,))
                    dve(lambda e, St=St: e.tensor_scalar_max(out=St[:, 0:T], in0=St[:, 0:T], scalar1=0.0), reads=(bS,), writes=(bS,))
                    act(A_(St[:, 0:T], St[:, 0:T], AF.Sqrt), reads=(bS,), writes=(bS,))
                    dve(TT(It[:, 0:T], It[:, 0:T], St[:, 0:T], ALU.mult), reads=(bI, bS), writes=(bI,))
                    dve(TT(It[:, 0:T], It[:, 0:T], XC[:, 0:T], ALU.mult), reads=(bI, bXC), writes=(bI,))
                    dve(lambda e, HL=HL, At=At, It=It: e.tensor_tensor_scan(out=HL[:, 0:TP], data0=At[:, 0:TP], data1=It[:, 0:TP],
                                                                             initial=0.0, op0=ALU.mult, op1=ALU.add),
                        reads=(bA, bI), writes=(bHL,))
                    dve(lambda e, Pt=Pt, At=At: e.tensor_tensor_scan(out=Pt[:, 0:TP], data0=At[:, 0:TP], data1=At[:, 0:TP],
                                                                      initial=1.0, op0=ALU.mult, op1=ALU.min),
                        reads=(bA,), writes=(bP,))
                    for b in range(16):
                        c0 = TP + b * 8
                        dve(lambda e, HL=HL, At=At, It=It, c0=c0, b=b, h=h: e.tensor_tensor_scan(
                            out=HL[:, c0:c0 + 8], data0=At[:, c0:c0 + 8], data1=It[:, c0:c0 + 8],
                            initial=H0C[:, h, b:b + 1], op0=ALU.mult, op1=ALU.add),
                            reads=(bA, bI, b_h0c), writes=(bHL,))
                    dve(CP(HLL[:, h:h + 1], HL[:, TP - 1:TP]), reads=(bHL,), writes=(b_sm,))
                    dve(CP(PLL[:, h:h + 1], Pt[:, TP - 1:TP]), reads=(bP,), writes=(b_sm,))
                    dve(CP(OUTSv[:, h, 1:17], HL[:, TP:T].rearrange("p (b t) -> p b t", t=8)[:, :, 7]), reads=(bHL,), writes=(b_outs,))
                    dve(TT(Qv[:, h, :], YA[:, h, 0:TP], Pt[:, 0:TP], ALU.mult), reads=(b_R2, bP), writes=(b_R3,))
                    dve(TT(YA[:, h, :], YA[:, h, :], HL[:, 0:T], ALU.mult), reads=(b_R2, bHL), writes=(b_R2,))

                stop("m%d_lru" % l)

                alias(b_r0t, (b_R0,))
                branch_b_v(l)

                stop("m%d_v" % l)
                if pas == 0:
                    dve(lambda e: e.memset(HIN, 0.0), writes=(b_sm,))
                    dve(CP(HSV[:, l * 16:(l + 1) * 16], HLL), reads=(b_sm,), writes=(b_sv,))
                else:
                    dve(CP(HIN, HSV[:, l * 16:(l + 1) * 16]), reads=(b_sv,), writes=(b_sm,))
                for h in range(KC):
                    dve(STT(YA[:, h, 0:TP], Qv[:, h, :], HIN[:, h:h + 1], YA[:, h, 0:TP], ALU.mult, ALU.add),
                        reads=(b_R3, b_sm, b_R2), writes=(b_R2,))
                dve(TT(TMPS, PLL, HIN, ALU.mult), reads=(b_sm,), writes=(b_sm,))
                dve(TT(OUTSv[:, :, 0], TMPS, HLL, ALU.add), reads=(b_sm,), writes=(b_outs,))
                b_so = [Buf("so0"), Buf("so1")]
                alias((b_scr[0],), b_so)
                for q in range(4):
                    bk = nb()
                    for jj in range(4):
                        hh = q * 4 + jj
                        transpose(PS[0:68, bk, jj * 128:(jj + 1) * 128], OUTSv[:, hh, :], IDENT[:],
                                  reads=(b_outs, b_const), writes=(b_ps[bk],))
                    so = SCR[0:68, (q % 2) * 512:(q % 2 + 1) * 512]
                    dve(CP(so, PS[0:68, bk, :]), reads=(b_ps[bk],), writes=(b_so[q % 2],))
                    out_store((lambda e, so=so, q=q, pas=pas: e.dma_start(out=stout[pas][l][:, q * 512:(q + 1) * 512], in_=so)), (b_so[q % 2],))
                alias(b_so, (b_scr[0],))
                stop("m%d_fix" % l)
                merge_stage(l, w_pa, 4 * D, first=True)
                stop("m%d_s5" % l)
                branch_b_u(l)
                stop("m%d_u" % l)
                merge_stage(l, w_pb, 5 * D, first=False)
                stop("m%d_s3" % l)
                for j in range(KC):
                    proj_units(w_o[l], j * 128, Mv, b_R3, lambda tt, bk, j=j: y_stats_and_store(j, tt, bk, True, True))
                stop("m%d_s6" % l)
                alias((b_R0,), b_r0t)
                rstd_from_ss(r0t(1), b_r0t[1])
                update_pass(l, 0, False)

            def merge_stage(l, Wp, mgcol, first):
                for j in range(KC):
                    def consume(tt, ba, bb, j=j):
                        sl = slice(tt * TU, (tt + 1) * TU)
                        SG = sct(1)[:, tt * TU:(tt + 1) * TU]
                        act(A_(SG, PS[:, bb, 0:TU], AF.Sigmoid), reads=(b_ps[bb],), writes=(b_scr[1],))
                        if first:
                            dve(TT(Mv[:, j, sl], SG, PS[:, ba, 0:TU], ALU.mult), reads=(b_scr[1], b_ps[ba]), writes=(b_R3,))
                        else:
                            TM = sct(2)[:, tt * TU:(tt + 1) * TU]
                            dve(TT(TM, SG, PS[:, ba, 0:TU], ALU.mult), reads=(b_scr[1], b_ps[ba]), writes=(b_scr[2],))
                            dve(TT(Mv[:, j, sl], Mv[:, j, sl], TM, ALU.add), reads=(b_scr[2], b_R3), writes=(b_R3,))
                    pair_units(Wp[l], j * 128, YA, b_R2, w_in[l], mgcol + j * 128, XN1, b_R1, consume)

            def branch_b_v(l):
                STG = SCR[:, TW:TW + D].rearrange("p (g s) -> p g s", s=128)
                stg_b = (b_scr[1], b_scr[2])

                def build_mixmat(dstv, dst_buf):
                    for g4 in range(4):
                        bk = nb()
                        for gg in range(4):
                            transpose(PS[:, bk, gg * 128:(gg + 1) * 128], STG[:, g4 * 4 + gg, :], IDENT[:], reads=stg_b + (b_const,),
                                      writes=(b_ps[bk],))
                        dve(TT(dstv[:, g4 * 4:(g4 + 1) * 4, :], PS[:, bk, :].rearrange("p (g t) -> p g t", t=128),
                               MASK[:].unsqueeze(1).broadcast_to([128, 4, 128]), ALU.mult), reads=(b_ps[bk], b_const), writes=(dst_buf,))

                P.dma("sp", (lambda e: e.dma_start(out=STG, in_=w_s[l].rearrange("g t s -> t g s"))), ds_wmt, writes=stg_b)
                build_mixmat(WMTv, b_wmt)
                dve(lambda e: e.memset(SCR[:, TW:TW + D], 0.0), writes=stg_b)
                for b in range(16):
                    P.dma("sp", (lambda e, b=b: e.dma_start(out=STG[b * 8:(b + 1) * 8, :, b * 8:(b + 1) * 8],
                                                            in_=w_s[l][:, 0:8, 0:8].rearrange("g t s -> t g s"))),
                          ds_bd, reads=(), writes=stg_b)
                build_mixmat(BDv, b_bd)
                brow = SCR[:, TW:TW + D]
                bsl = b_s[l].rearrange("g t -> (g t)").unsqueeze(0)
                P.dma("sp", (lambda e: e.dma_start(out=brow[0:1, :], in_=bsl)), ds_bs, writes=(b_scr[1], b_scr[2]))
                P.dma("sp", (lambda e: e.dma_start(out=brow[32:33, :], in_=bsl)), ds_bs, writes=(b_scr[1], b_scr[2]))
                dve(lambda e: e.memset(BSR[:], 0.0), writes=(b_bsr,))
                P.dma("sp", (lambda e: e.dma_start(out=BS8[:].rearrange("p (g t) -> p g t", t=8),
                                                   in_=b_s[l][:, 0:8].partition_broadcast(128))), ds_bs, writes=(b_bs8,))
                act(A_(BSR[0:1, :], brow[0:1, :], AF.Copy), reads=(b_scr[1], b_scr[2]), writes=(b_bsr,))
                hi32 = sct(0)[32:33, 0:1024].bitcast(BF16)
                act(A_(hi32, brow[32:33, :], AF.Copy), reads=(b_scr[1], b_scr[2]), writes=(b_scr[0],))
                dve(TT(BSR[32:33, :], brow[32:33, :], hi32, ALU.subtract), reads=(b_scr[0], b_scr[1], b_scr[2]), writes=(b_bsr,))

                DG = sct(2)[:, 0:256]
                for c in range(KC):
                    dg = DG[:, (c % 2) * 128:(c % 2 + 1) * 128]
                    dve(TS_(dg, IDENT[:], VECv[:, l, 12, c:c + 1], None, ALU.mult), reads=(b_const,), writes=(b_scr[2],))
                    bk = c // 4
                    P.op("pe", (lambda e, bk=bk, c=c, dg=dg: e.matmul(PS[:, bk, (c % 4) * 128:(c % 4 + 1) * 128], lhsT=ONESF[:], rhs=dg,
                                                                     start=True, stop=True)),
                         reads=(b_scr[2], b_ct), writes=(b_ps[bk],))
                GVS = SCR[:, TW:TW + D]
                JUNK = sct(0)[:, 1024:1152].bitcast(BF16)
                vb = (4, 5, 6, 7)
                u = 0
                for blk in range(16):
                    s = wq(*kblock(w_in[l], 3 * D + blk * 128))
                    wv = wsv(s)
                    for i in range(9):
                        bk = vb[u % 4]
                        u += 1
                        mm_group(PS[:, bk, 0:128], [(XN1[:, k, i * 128:(i + 1) * 128], wv[:, k, :]) for k in range(KC)],
                                 reads=(b_ws[s], b_R1), writes=(b_ps[bk],))
                        if i < 8:
                            GV = sct(0)[:, (u % 8) * 128:(u % 8 + 1) * 128]
                            wrs = (b_scr[0],)
                        else:
                            GV = GVS[:, blk * 128:(blk + 1) * 128]
                            wrs = (b_scr[1], b_scr[2])
                        act(A_(GV, PS[:, bk, 0:128], GELU), reads=(b_ps[bk],), writes=wrs)
                        act(A_(JUNK[:, 0:128], GV, AF.Square, accum_out=SSV[:, i * 16 + blk:i * 16 + blk + 1]),
                            reads=wrs, writes=(b_sm, b_scr[0]))
                        dve(CP(VN[:, i, blk * 128:(blk + 1) * 128], GV), reads=wrs, writes=(b_R0,))
                dve(lambda e: e.tensor_reduce(out=RSV, in_=SSV.rearrange("p (i b) -> p i b", b=16), axis=AX.X, op=ALU.add),
                    reads=(b_sm,), writes=(b_sm,))
                act(A_(RSV, RSV, AF.Sqrt, bias=EPSC, scale=1.0 / D), reads=(b_sm, b_ct), writes=(b_sm,))
                dve(lambda e: e.reciprocal(out=RSV, in_=RSV), reads=(b_sm,), writes=(b_sm,))
                GVP = PS[:, 0:4, :]
                gvr = tuple(b_ps[b] for b in range(4))
                for i in range(9):
                    vi = VN[:, i, :].rearrange("p (q n) -> p q n", n=512)
                    dve(STT(vi, vi, RSV[:, i:i + 1], GVP, ALU.mult, ALU.mult), reads=(b_R0, b_sm) + gvr, writes=(b_R0,))
                gs = GVS.rearrange("p (q n) -> p q n", n=512)
                dve(STT(gs, gs, RSV[:, 8:9], GVP, ALU.mult, ALU.mult), reads=(b_scr[1], b_scr[2], b_sm) + gvr,
                    writes=(b_scr[1], b_scr[2]))
                out_store((lambda e, pas=pas: e.dma_start(out=vout[pas][l], in_=GVS)), (b_scr[1], b_scr[2]))

            def branch_b_u(l):
                GU = sct(0)
                for g in range(KC):
                    proj_units(w_in[l], 2 * D + g * 128, XN1, b_R1,
                               lambda tt, bk: act(A_(GU[:, tt * TU:(tt + 1) * TU], PS[:, bk, 0:TU], GELU),
                                                  reads=(b_ps[bk],), writes=(b_scr[0],)))
                    for tt in range(3):
                        bk = nb()
                        for sidx in range(3):
                            i = tt * 3 + sidx
                            o2 = PS[:, bk, sidx * 128:(sidx + 1) * 128]
                            if i < 8:
                                rd = (b_R0, b_wmt, b_bsr, b_ct)
                                P.op("pe", (lambda e, o2=o2, i=i, g=g: e.matmul(o2, lhsT=VN[:, i, g * 128:(g + 1) * 128], rhs=WMTv[:, g, :],
                                                                              start=True, stop=False)),
                                     reads=rd, writes=(b_ps[bk],), sig=False)
                                P.op("pe", (lambda e, o2=o2, g=g: e.matmul(o2, lhsT=ONESB[0:33, :], rhs=BSR[0:33, g * 128:(g + 1) * 128],
                                                                         start=False, stop=True)),
                                     reads=rd, writes=(b_ps[bk],), sig=True)
                            else:
                                rd = (b_R0, b_bd)
                                P.op("pe", (lambda e, o2=o2, i=i, g=g: e.matmul(o2, lhsT=VN[:, i, g * 128:(g + 1) * 128], rhs=BDv[:, g, :],
                                                                              start=True, stop=True)),
                                     reads=rd, writes=(b_ps[bk],), sig=True)
                        if tt < 2:
                            dve(TT(YA[:, g, tt * TU:(tt + 1) * TU], PS[:, bk, 0:TU], GU[:, tt * TU:(tt + 1) * TU], ALU.mult),
                                reads=(b_ps[bk], b_scr[0]), writes=(b_R2,))
                        else:
                            dve(TT(YA[:, g, 2 * TU:2 * TU + 256], PS[:, bk, 0:256], GU[:, 2 * TU:2 * TU + 256], ALU.mult),
                                reads=(b_ps[bk], b_scr[0]), writes=(b_R2,))
                            tmpb = sct(1)[:, 0:128].rearrange("p (b t) -> p b t", t=8)
                            dve(TT(tmpb, PS[:, bk, 256:TU].rearrange("p (b t) -> p b t", t=8),
                                   BS8[:, g * 8:(g + 1) * 8].unsqueeze(1).broadcast_to([128, 16, 8]), ALU.add),
                                reads=(b_ps[bk], b_bs8), writes=(b_scr[1],))
                            dve(TT(YA[:, g, TP:T], sct(1)[:, 0:128], GU[:, TP:T], ALU.mult),
                                reads=(b_scr[1], b_scr[0]), writes=(b_R2,))

            def ffn(l, final):
                ms = load_mod(l, 1)
                moe = (l == 1)
                xn_pass(l, 1, XN3, b_R3, moe=moe)
                if moe:
                    GE = moe_gates()
                alias(b_r0t, (b_R0,))
                ngroups = 8 if moe else 3
                SGt = sct(0)
                GB = sct(1)
                for grp in range(ngroups):
                    if moe:
                        Wg2, Wu2, Wd2 = moe_wg[0][grp], moe_wu[0][grp], moe_wd[0][grp]
                        c_off = 0
                        dve(TS_(SELT[:], ONESF[0:8, :], IDENT[0:8, grp:grp + 1], None, ALU.mult), reads=(b_const, b_ct), writes=(b_selt,))
                        for tt in range(3):
                            bk = nb()
                            P.op("pe", (lambda e, bk=bk, tt=tt: e.matmul(PS[:, bk, 0:TU], lhsT=SELT[:], rhs=GE[:, tt * TU:(tt + 1) * TU],
                                                                        start=True, stop=True)),
                                 reads=(b_scr[2], b_selt), writes=(b_ps[bk],))
                            act(A_(GB[:, tt * TU:(tt + 1) * TU], PS[:, bk, 0:TU], AF.Copy), reads=(b_ps[bk],), writes=(b_scr[1],))
                    else:
                        Wg2, Wu2, Wd2 = ffn_wg[0], ffn_wu[0], ffn_wd[0][grp * D:(grp + 1) * D, :]
                        c_off = grp * D
                    for hc in range(KC):
                        def consume(tt, ba, bb, hc=hc):
                            sl = slice(tt * TU, (tt + 1) * TU)
                            SG = SGt[:, sl]
                            act(A_(SG, PS[:, ba, 0:TU], AF.Silu), reads=(b_ps[ba],), writes=(b_scr[0],))
                            if moe:
                                dve(TT(SG, SG, GB[:, sl], ALU.mult), reads=(b_scr[0], b_scr[1]), writes=(b_scr[0],))
                            dve(TT(Hg[:, hc, sl], SG, PS[:, bb, 0:TU], ALU.mult), reads=(b_scr[0], b_ps[bb]), writes=(b_R0,))
                        pair_units(Wg2, c_off + hc * 128, XN3, b_R3, Wu2, c_off + hc * 128, XN3, b_R3, consume)
                    for j in range(KC):
                        proj_units(Wd2, j * 128, Hg, b_R0,
                                   lambda tt, bk, j=j, grp=grp: y_stats_and_store(j, tt, bk, grp == 0, grp == ngroups - 1))
                alias((b_R0,), b_r0t)
                rstd_from_ss(r0t(1), b_r0t[1])
                update_pass(l, 1, final)

            def moe_gates():
                LG = sct(1)[0:8, 0:T]
                for tt in range(3):
                    act(A_(LG[:, tt * TU:(tt + 1) * TU], PS[0:8, SSB[tt], 0:TU], AF.Identity, bias=RB[:, 0:1]),
                        reads=(b_ps[SSB[tt]], b_const), writes=(b_scr[1],))
                LTv = LT.rearrange("p (i e) -> p i e", e=8)
                LEv = LE.rearrange("p (i e) -> p i e", e=8)
                bk = nb()
                for i in range(9):
                    transpose(PS[:, bk, i * 8:(i + 1) * 8], LG[:, i * 128:(i + 1) * 128], IDENT[0:8, 0:8],
                              reads=(b_scr[1], b_const), writes=(b_ps[bk],))
                dve(CP(LT, PS[:, bk, 0:72]), reads=(b_ps[bk],), writes=(b_sm,))
                dve(lambda e: e.tensor_reduce(out=M1, in_=LTv, axis=AX.X, op=ALU.max), reads=(b_sm,), writes=(b_sm,))
                dve(TT(LTv, LTv, M1.unsqueeze(2).broadcast_to([128, 9, 8]), ALU.subtract), reads=(b_sm,), writes=(b_sm,))
                act(A_(LE, LT, AF.Exp), reads=(b_sm,), writes=(b_sm,))
                dve(TS_(LT, LE, 1.0, None, ALU.is_lt), reads=(b_sm,), writes=(b_sm,))
                dve(TT(LT, LT, LE, ALU.mult), reads=(b_sm,), writes=(b_sm,))
                dve(lambda e: e.tensor_reduce(out=M2, in_=LTv, axis=AX.X, op=ALU.max), reads=(b_sm,), writes=(b_sm,))
                dve(TT(LTv, LEv, M2.unsqueeze(2).broadcast_to([128, 9, 8]), ALU.is_ge), reads=(b_sm,), writes=(b_sm,))
                dve(TT(LE, LE, LT, ALU.mult), reads=(b_sm,), writes=(b_sm,))
                dve(lambda e: e.tensor_reduce(out=M1, in_=LEv, axis=AX.X, op=ALU.add), reads=(b_sm,), writes=(b_sm,))
                dve(lambda e: e.reciprocal(out=M1, in_=M1), reads=(b_sm,), writes=(b_sm,))
                dve(TT(LEv, LEv, M1.unsqueeze(2).broadcast_to([128, 9, 8]), ALU.mult), reads=(b_sm,), writes=(b_sm,))
                GE = sct(2)[0:8, 0:T]
                for tt in range(3):
                    bk = nb()
                    for sidx in range(3):
                        i = tt * 3 + sidx
                        transpose(PS[0:8, bk, sidx * 128:(sidx + 1) * 128], LEv[:, i, :], IDENT[:], reads=(b_sm, b_const), writes=(b_ps[bk],))
                    act(A_(GE[:, tt * TU:(tt + 1) * TU], PS[0:8, bk, 0:TU], AF.Copy), reads=(b_ps[bk],), writes=(b_scr[2],))
                return GE

            for l in range(2):
                mixer(l)
                stop("m%d" % l)
                ffn(l, final=(l == 1))
                stop("f%d" % l)

            b_yt = [Buf("yt%d" % i) for i in range(3)]
            alias(b_scr, b_yt)
            for i in range(9):
                for q in range(4):
                    bk = nb()
                    for jj in range(4):
                        j = q * 4 + jj
                        transpose(PS[:, bk, jj * 128:(jj + 1) * 128], Yf[:, j, i * 128:(i + 1) * 128], IDENT[:],
                                  reads=(b_R1, b_R2, b_const), writes=(b_ps[bk],))
                    slot = (i * 4 + q) % 3
                    yt = sct(slot)[:, 0:512]
                    if q % 2 == 0:
                        act(A_(yt, PS[:, bk, :], AF.Copy), reads=(b_ps[bk],), writes=(b_yt[slot],))
                    else:
                        dve(CP(yt, PS[:, bk, :]), reads=(b_ps[bk],), writes=(b_yt[slot],))
                    if i == 0 and q < 3:
                        b_yt[slot].ds = new_ds()
                        out_ds.append(b_yt[slot].ds)
                    P.dma("sp", (lambda e, yt=yt, i=i, q=q, pas=pas: e.dma_start(out=yout[pas][i * 128:(i + 1) * 128, q * 512:(q + 1) * 512], in_=yt)),
                          b_yt[slot].ds, reads=(b_yt[slot],))

        final_ev = [Ev(d.key, d.count) for d in P.dsems if d.count > 0]

        semh = {}
        for k in ("pe", "act", "dve", "pool", "sp"):
            semh[k] = es.enter_context(nc.semaphore("s_" + k))
        for d in P.dsems:
            semh[d.key] = es.enter_context(nc.semaphore("s_" + d.key))
        block = es.enter_context(nc.Block())

        @block.tensor
        def _(e):
            P.emit("pe", e, semh)

        @block.scalar
        def _(e):
            P.emit("act", e, semh)

        @block.vector
        def _(e):
            P.emit("dve", e, semh)

        @block.gpsimd
        def _(e):
            P.emit("pool", e, semh)

        @block.sync
        def _(e):
            P.emit("sp", e, semh, final=final_ev)

    print("counts", P.cnt, {d.key: d.count for d in P.dsems}, "wblocks", wcount[0])
    return nc


_CACHE = {}


def _fm(v):
    return np.ascontiguousarray(v.reshape(KC, 128).T)


def kernel(x_prompt, x_sample, c_prompt, c_sample, state_lru_h, state_lru_conv,
           w_ada, b_ada, g_pre_mix, g_post_mix, g_pre_ffn, g_post_ffn,
           w_in, conv_w, conv_b, w_r, b_r, w_i, b_i, lru_lambda, g_v, w_s, b_s,
           w_pa, w_pb, w_o, ffn_wg, ffn_wu, ffn_wd,
           router_w, router_b, moe_wg, moe_wu, moe_wd):
    f = lambda a: np.ascontiguousarray(np.asarray(a, dtype=np.float32))
    x_prompt, x_sample, c_prompt, c_sample = f(x_prompt), f(x_sample), f(c_prompt), f(c_sample)
    state_lru_h, state_lru_conv = f(state_lru_h), f(state_lru_conv)
    if "nc" not in _CACHE:
        _CACHE["nc"] = build()
    nc = _CACHE["nc"]
    vecs = np.zeros((128, 2, NVEC, KC), np.float32)
    for l in range(2):
        lst = [g_pre_mix[l], g_post_mix[l], g_pre_ffn[l], g_post_ffn[l], conv_w[l][0], conv_w[l][1], conv_w[l][2],
               conv_w[l][3], conv_b[l], b_r[l], b_i[l], lru_lambda[l], g_v[l]]
        for vi, v in enumerate(lst):
            vecs[:, l, vi, :] = _fm(f(v))
    vecs = vecs.reshape(128, -1)
    bada = np.stack([f(b_ada)[l].reshape(96, 128).T for l in range(2)], axis=1).reshape(128, 192)
    bada = np.ascontiguousarray(bada)
    ident = np.eye(128, dtype=np.float32)
    mask = np.triu(np.ones((128, 128), np.float32))
    shared = dict(vecs=vecs, bada=bada, ident=ident, mask=mask, w_ada=f(w_ada), w_in=f(w_in), w_r=f(w_r), w_i=f(w_i),
                  w_s=f(w_s), b_s=f(b_s), w_pa=f(w_pa), w_pb=f(w_pb), w_o=f(w_o), ffn_wg=f(ffn_wg), ffn_wu=f(ffn_wu),
                  ffn_wd=f(ffn_wd), router_w=f(router_w), router_b=f(router_b).reshape(8, 1), moe_wg=f(moe_wg),
                  moe_wu=f(moe_wu), moe_wd=f(moe_wd))
    in_maps = []
    for c in range(NCORES):
        xs, cs, sts = [], [], []
        for p in range(NPASS):
            sb0 = 16 * (2 * c + p)
            xs.append(np.concatenate([x_prompt[c, p * TP:(p + 1) * TP], x_sample[sb0:sb0 + 16].reshape(TS, D)], axis=0))
            cs.append(np.concatenate([c_prompt[c:c + 1], c_sample[sb0:sb0 + 16]], axis=0))
            sts.append(np.stack([np.concatenate([state_lru_h[l, sb0:sb0 + 16],
                                                 state_lru_conv[l, sb0:sb0 + 16].reshape(48, D)], axis=0) for l in range(2)]))
        sel = np.zeros((128, 8), np.float32)
        m = dict(shared)
        m.update(xtok=np.ascontiguousarray(np.stack(xs)), ctok=np.ascontiguousarray(np.stack(cs)),
                 st_in=np.ascontiguousarray(np.stack(sts)), sel=sel)
        in_maps.append(m)
    res = run_bass_kernel_spmd(nc, in_maps, core_ids=list(range(NCORES)))
    R = res.results
    B, S = x_prompt.shape[0], x_prompt.shape[1]
    y_prompt = np.zeros((B, S, D), np.float32)
    y_sample = np.zeros((128, 8, D), np.float32)
    h_p = np.zeros((2, B, D), np.float32)
    conv_p = np.zeros((2, B, 3, D), np.float32)
    h_s = np.zeros((2, 128, D), np.float32)
    conv_s = np.zeros((2, 128, 3, D), np.float32)
    v_s = np.zeros((2, 128, 8, D), np.float32)
    for c in range(NCORES):
        for p in range(NPASS):
            sb0 = 16 * (2 * c + p)
            yo, so, vo = R[c]["yout"][p], R[c]["stout"][p], R[c]["vout"][p]
            y_prompt[c, p * TP:(p + 1) * TP] = yo[:TP]
            y_sample[sb0:sb0 + 16] = yo[TP:].reshape(16, 8, D)
            for l in range(2):
                if p == 1:
                    h_p[l, c] = so[l, 0]
                    conv_p[l, c] = so[l, 17:20]
                h_s[l, sb0:sb0 + 16] = so[l, 1:17]
                conv_s[l, sb0:sb0 + 16] = so[l, 20:68].reshape(16, 3, D)
                v_s[l, sb0:sb0 + 16] = vo[l].reshape(16, 8, D)
    return (y_prompt, y_sample, h_p, conv_p, h_s, conv_s, v_s)
```

```python
import contextlib
import numpy as np
import concourse.bass as bass
import concourse.mybir as mybir
from concourse.bass_utils import run_bass_kernel_spmd

F32 = mybir.dt.float32
BF16 = mybir.dt.bfloat16
AF = mybir.ActivationFunctionType
ALU = mybir.AluOpType
AX = mybir.AxisListType
GELU = AF.Gelu_apprx_tanh

NCORES = 4
NPASS = 2
D = 2048
KC = 16
T = 1152
TP = 1024
TS = 128
NB = 17
TU = 384
EPS = 1e-6
NVEC = 13
TW = 1216


class Ev:
    __slots__ = ("sem", "val")

    def __init__(self, sem, val):
        self.sem, self.val = sem, val


class Buf:
    def __init__(self, name):
        self.name = name
        self.wr = None
        self.rds = {}


class DSem:
    def __init__(self, key):
        self.key = key
        self.count = 0


class Prog:
    ENGS = ("pe", "act", "dve", "pool", "sp")

    def __init__(self):
        self.q = {e: [] for e in self.ENGS}
        self.cnt = {e: 0 for e in self.ENGS}
        self.dsems = []
        self.halt = False

    def new_dsem(self):
        d = DSem("d%d" % len(self.dsems))
        self.dsems.append(d)
        return d

    @staticmethod
    def _waits(reads, writes, extra):
        w = []
        for b in reads:
            if b.wr is not None:
                w.append(b.wr)
        for b in writes:
            if b.wr is not None:
                w.append(b.wr)
            for s, v in b.rds.items():
                w.append(Ev(s, v))
        for e in extra:
            if e is not None:
                w.append(e)
        return w

    def op(self, eng, fn, reads=(), writes=(), extra=(), sig=True):
        if self.halt:
            return None
        waits = self._waits(reads, writes, extra)
        ev = None
        if sig:
            self.cnt[eng] += 1
            ev = Ev(eng, self.cnt[eng])
            for b in reads:
                if b.rds.get(eng, 0) < ev.val:
                    b.rds[eng] = ev.val
            for b in writes:
                b.wr = ev
                b.rds = {}
        self.q[eng].append((fn, waits, eng if sig else None, 1))
        return ev

    def dma(self, queue, fn, dsem, reads=(), writes=(), extra=(), inc=16):
        if self.halt:
            return None
        waits = self._waits(reads, writes, extra)
        dsem.count += inc
        ev = Ev(dsem.key, dsem.count)
        for b in reads:
            if b.rds.get(dsem.key, 0) < ev.val:
                b.rds[dsem.key] = ev.val
        for b in writes:
            b.wr = ev
            b.rds = {}
        self.q[queue].append((fn, waits, dsem.key, inc))
        return ev

    def emit(self, name, eng, semh, final=()):
        seen = {}

        def wait(s, v):
            if seen.get(s, 0) < v:
                eng.wait_ge(semh[s], v)
                seen[s] = v

        for fn, waits, sem, inc in self.q[name]:
            need = {}
            for ev in waits:
                if need.get(ev.sem, 0) < ev.val:
                    need[ev.sem] = ev.val
            for s, v in need.items():
                wait(s, v)
            ins = fn(eng)
            if sem is not None:
                if inc == 1 and name == "pool":
                    ins.then_inc(semh[sem])
                else:
                    ins.then_inc(semh[sem], inc)
        for ev in final:
            wait(ev.sem, ev.val)


class _Stop(Exception):
    pass


def build(debug_stop=None):
    import os
    STOP = os.environ.get('MK_STOP', '')
    NOCC = os.environ.get('MK_NOCC', '') == '1'

    def stop(label):
        if STOP == label:
            P.halt = True
            print("STOPPED at", label)

    nc = bass.Bass("TRN2", target_bir_lowering=False)
    P = Prog()

    def din(name, shape, dt=F32):
        return nc.dram_tensor(name, list(shape), dt, kind="ExternalInput").ap()

    def dout(name, shape):
        return nc.dram_tensor(name, list(shape), F32, kind="ExternalOutput").ap()

    xtok = din("xtok", [NPASS, T, D])
    ctok = din("ctok", [NPASS, NB, D])
    st_in = din("st_in", [NPASS, 2, 64, D])
    sel_d = din("sel", [128, 8])
    vecs_d = din("vecs", [128, 2 * NVEC * KC])
    bada_d = din("bada", [128, 2 * 96])
    ident_d = din("ident", [128, 128])
    mask_d = din("mask", [128, 128])
    w_ada = din("w_ada", [2, D, 6 * D])
    w_in = din("w_in", [2, D, 6 * D])
    w_r = din("w_r", [2, 16, 128, 128])
    w_i = din("w_i", [2, 16, 128, 128])
    w_s = din("w_s", [2, 16, 128, 128])
    b_s = din("b_s", [2, 16, 128])
    w_pa = din("w_pa", [2, D, D])
    w_pb = din("w_pb", [2, D, D])
    w_o = din("w_o", [2, D, D])
    ffn_wg = din("ffn_wg", [1, D, 3 * D])
    ffn_wu = din("ffn_wu", [1, D, 3 * D])
    ffn_wd = din("ffn_wd", [1, 3 * D, D])
    router_w = din("router_w", [1, D, 8])
    router_b = din("router_b", [8, 1])
    moe_wg = din("moe_wg", [1, 8, D, D])
    moe_wu = din("moe_wu", [1, 8, D, D])
    moe_wd = din("moe_wd", [1, 8, D, D])

    yout = dout("yout", [NPASS, T, D])
    stout = dout("stout", [NPASS, 2, 68, D])
    vout = dout("vout", [NPASS, 2, 128, D])

    xres = nc.dram_tensor("xres", [KC, 128, T], F32).ap()
    modd = nc.dram_tensor("modd", [4, 128, 96 * NB], F32).ap()
    std = nc.dram_tensor("std", [2, 128, KC * 64], F32).ap()
    agx_in = nc.dram_tensor("agx_in", [128, 48], F32)
    agx_out = nc.dram_tensor("agx_out", [NCORES * 128, 48], F32)
    agh_in = nc.dram_tensor("agh_in", [128, 16], F32)
    agh_out = nc.dram_tensor("agh_out", [NCORES * 128, 16], F32)

    es = contextlib.ExitStack()
    with es:
        def sb(name, shape, dt):
            return es.enter_context(nc.sbuf_tensor(name, list(shape), dt))

        R12 = sb("R12", [128, 2 * KC * T], BF16)
        R03 = sb("R03", [128, 2 * KC * T], BF16)
        WS = sb("WS", [128, 4, 2048], BF16)
        SCR = sb("SCR", [128, 3 * TW], F32)
        IDENT = sb("IDENT", [128, 128], F32)
        MASK = sb("MASK", [128, 128], F32)
        ONESB = sb("ONESB", [128, 128], BF16)
        ONESF = sb("ONESF", [128, 128], F32)
        VECS = sb("VECS", [128, 2 * NVEC * KC], F32)
        BADA = sb("BADA", [128, 192], F32)
        SEL = sb("SEL", [128, 8], F32)
        FLAG = sb("FLAG", [128, 1], F32)
        MOD = sb("MOD", [128, 1, 3 * KC * NB], F32)
        BS8 = sb("BS8", [128, 128], F32)
        CT = sb("CT", [128, KC * 2 * NB], BF16)
        WMT = sb("WMT", [128, KC * 128], BF16)
        BD = sb("BD", [128, KC * 128], BF16)
        BSR = sb("BSR", [33, KC * 128], BF16)
        OUTS = sb("OUTS", [128, KC * 68], F32)
        SM = sb("SM", [128, 768], F32)
        XNT = sb("XNT", [128, KC * 4], BF16)
        AGL = sb("AGL", [128, 8 * 48], F32)
        RW = sb("RW", [128, KC * 8], F32)
        RB = sb("RB", [8, 1], F32)
        PS = es.enter_context(nc.psum_tensor("PS", [128, 8, 512], F32))
        print('SBUF remaining', nc.sbuf_bytes_remaining)

        R1 = R12[:, 0:KC * T]
        R2 = R12[:, KC * T:2 * KC * T]
        R0 = R03[:, 0:KC * T]
        R3 = R03[:, KC * T:2 * KC * T]
        Yf = R12[:].bitcast(F32).rearrange("p (c t) -> p c t", t=T)
        XNEW = R03[:].bitcast(F32).rearrange("p (c t) -> p c t", t=T)
        R0f = R0.bitcast(F32)
        R2f = R2.bitcast(F32)
        R3f = R3.bitcast(F32)
        R12f = R12[:].bitcast(F32)

        def v3(ap, n):
            return ap.rearrange("p (c n) -> p c n", n=n)

        XN1 = v3(R1, T)
        XN3 = v3(R3[:, 0:KC * T], T)
        YA = v3(R2, T)
        Mv = v3(R3, T)
        Qv = v3(R3[:, 0:KC * TP], TP)
        H0C = v3(R3f[:, 8192:9216], 64)
        VN = v3(R0, D)
        Hg = v3(R0, T)
        VECv = VECS[:].rearrange("p (l v c) -> p l v c", l=2, v=NVEC)
        BADAv = BADA[:].rearrange("p (l n) -> p l n", l=2)
        MODv = MOD[:].rearrange("p s (k c b) -> p s k c b", k=3, c=KC)
        CTv = v3(CT[:], 2 * NB)
        WMTv = v3(WMT[:], 128)
        BDv = v3(BD[:], 128)
        OUTSv = v3(OUTS[:], 68)
        XNTv = v3(XNT[:], 4)
        AGLv = v3(AGL[:], 48)
        RWv = v3(RW[:], 8)

        def r0t(i):
            return R0f[:, i * TW:(i + 1) * TW]

        def sct(i):
            return SCR[:, i * TW:(i + 1) * TW]

        RSV = SM[:, 0:9]
        SSV = SM[:, 512:656]
        HLL = SM[:, 96:112]
        PLL = SM[:, 112:128]
        HIN = SM[:, 128:144]
        CL = SM[:, 144:160]
        CL2 = SM[:, 160:176]
        TX = SM[:, 176:224]
        TXS = SM[:, 224:272]
        RST = SM[:, 272:275]
        LT = SM[:, 288:360]
        LE = SM[:, 360:432]
        M1 = SM[:, 432:441]
        M2 = SM[:, 448:457]
        TMPS = SM[:, 464:480]

        b_R0, b_R1, b_R2, b_R3 = Buf("R0"), Buf("R1"), Buf("R2"), Buf("R3")
        b_ws = [Buf("ws%d" % i) for i in range(4)]
        b_scr = [Buf("scr0"), Buf("scr1"), Buf("scr2")]
        b_r0t = [Buf("r0t%d" % i) for i in range(7)]
        b_ps = [Buf("ps%d" % i) for i in range(8)]
        b_const = Buf("const")
        b_mod = [Buf("mod0"), Buf("mod1")]
        b_ct = Buf("ct")
        b_wmt, b_bd, b_bsr, b_bs8 = Buf("wmt"), Buf("bd"), Buf("bsr"), Buf("bs8")
        b_outs, b_sm, b_xnt, b_agl = Buf("outs"), Buf("sm"), Buf("xnt"), Buf("agl")
        b_xres = [Buf("xres%d" % j) for j in range(KC)]
        b_modd = [Buf("modd%d" % i) for i in range(4)]
        b_std = [Buf("std0"), Buf("std1")]
        b_agx, b_agxo, b_agh, b_agho = Buf("agx"), Buf("agxo"), Buf("agh"), Buf("agho")
        b_h0c = Buf("h0c")

        ds_setup = P.new_dsem()
        ds_ws = [P.new_dsem() for _ in range(4)]
        ds_misc = [P.new_dsem() for _ in range(4)]
        ds_out = P.new_dsem()
        ds_x = [P.new_dsem(), P.new_dsem(), P.new_dsem()]
        ds_cc = P.new_dsem()
        out_events = []

        bank_rr = [0]

        def nb():
            b = bank_rr[0]
            bank_rr[0] = (b + 1) % 5
            return b

        def act(fn, reads=(), writes=()):
            return P.op("act", fn, reads, writes)

        def dve(fn, reads=(), writes=()):
            return P.op("dve", fn, reads, writes)

        def A_(out, in_, func, bias=None, scale=None, accum_out=None):
            kw = {}
            if bias is not None:
                kw["bias"] = bias
            if scale is not None:
                kw["scale"] = scale
            if accum_out is not None:
                kw["accum_out"] = accum_out
            return lambda e: e.activation(out=out, in_=in_, func=func, **kw)

        def TT(out, in0, in1, op):
            return lambda e: e.tensor_tensor(out=out, in0=in0, in1=in1, op=op)

        def TS_(out, in0, s1, s2, op0, op1=None):
            if op1 is None:
                return lambda e: e.tensor_scalar(out=out, in0=in0, scalar1=s1, scalar2=None, op0=op0)
            return lambda e: e.tensor_scalar(out=out, in0=in0, scalar1=s1, scalar2=s2, op0=op0, op1=op1)

        def STT(out, in0, scalar, in1, op0, op1):
            return lambda e: e.scalar_tensor_tensor(out=out, in0=in0, scalar=scalar, in1=in1, op0=op0, op1=op1)

        def CP(out, in_):
            return lambda e: e.tensor_copy(out=out, in_=in_)

        def mm_group(out, pairs, reads, writes, extra_first=()):
            n = len(pairs)
            ev = None
            for i, (l, r) in enumerate(pairs):
                fn = (lambda e, l=l, r=r, i=i: e.matmul(out, lhsT=l, rhs=r, start=(i == 0), stop=(i == n - 1)))
                last = (i == n - 1)
                if i == 0 or last:
                    ev = P.op("pe", fn, reads, writes, extra=extra_first if i == 0 else (), sig=last)
                else:
                    P.op("pe", fn, (), (), sig=False)
            return ev

        def transpose(out, in_, ident, reads, writes):
            return P.op("pe", lambda e: e.transpose(out, in_, ident), reads, writes)

        wcount = [0]

        def wq(*parts):
            i = wcount[0]
            wcount[0] += 1
            sl = i % 4
            for (off, n, inner, src) in parts:
                dst = WS[:, sl, off:off + n].rearrange("p (a b) -> p a b", b=inner)
                P.dma("pool", (lambda e, dst=dst, src=src: e.dma_start(out=dst, in_=src)),
                      ds_ws[sl], reads=(), writes=(b_ws[sl],))
            return sl

        def kblock(W2d, c0, ncols=128):
            src = W2d.rearrange("(c p) n -> p c n", p=128)[:, :, c0:c0 + ncols]
            return ((0, KC * ncols, ncols, src),)

        def wsv(sl, ncols=128):
            return WS[:, sl, 0:KC * ncols].rearrange("p (c n) -> p c n", n=ncols)

        def alias(frm, to):
            for t in to:
                for f in frm:
                    if f.wr is not None and t.rds.get(f.wr.sem, 0) < f.wr.val:
                        t.rds[f.wr.sem] = f.wr.val
                    for sk, v in f.rds.items():
                        if t.rds.get(sk, 0) < v:
                            t.rds[sk] = v

        def new_ds():
            return P.new_dsem()

        SELT = sb("SELT", [8, 128], F32)
        ONE1 = ONESF[:, 0:1]
        b_selt = Buf("selt")
        ds_std = [new_ds(), new_ds()]
        ds_modd = [new_ds() for _ in range(4)]
        ds_mod = [new_ds(), new_ds()]
        ds_h0c, ds_wmt, ds_bd, ds_bs, ds_ag, ds_agl = new_ds(), new_ds(), new_ds(), new_ds(), new_ds(), new_ds()
        ds_xres = new_ds()
        out_ds = []

        def out_store(fn, stage_bufs):
            d = new_ds()
            out_ds.append(d)
            return P.dma("sp", fn, d, reads=stage_bufs)

        def sload(dst, src):
            P.dma("sp", (lambda e: e.dma_start(out=dst, in_=src)), ds_setup, writes=(b_const,))

        sload(IDENT[:], ident_d)
        sload(MASK[:], mask_d)
        sload(VECS[:], vecs_d)
        sload(BADA[:], bada_d)
        sload(SEL[:], sel_d)
        sload(RW[:].rearrange("p (c e) -> p c e", e=8), router_w[0].rearrange("(c p) e -> p c e", p=128))
        sload(RB[:], router_b)
        C17 = R0f[0:NB, 0:D]
        STI = [R0f[0:64, D:2 * D], R0f[0:64, 2 * D:3 * D]]
        TXSV = sb("TXSV", [128, 2 * 48], F32)
        HSV = sb("HSV", [128, 2 * 16], F32)
        b_sv = Buf("sv")
        dve(lambda e: e.memset(ONESB[:], 1.0), writes=(b_ct,))
        dve(lambda e: e.memset(ONESF[:], 1.0), writes=(b_ct,))
        EPSC = SM[:, 500:501]
        dve(lambda e: e.memset(EPSC, EPS), writes=(b_ct,))
        dve(lambda e: e.tensor_reduce(out=FLAG[:], in_=SEL[:], axis=AX.X, op=ALU.add), reads=(b_const,), writes=(b_sm,))

        b_xt = [Buf("xt0"), Buf("xt1")]
        b_xf = [Buf("xf0"), Buf("xf1")]
        for pas in range(NPASS):
            alias([b_R0, b_R1, b_R2, b_R3] + b_r0t + b_scr, [b_const] + b_xt + b_xf)
            sload(C17, ctok[pas])
            sload(STI[0], st_in[pas][0])
            sload(STI[1], st_in[pas][1])
            if pas == 0:
                for p2 in range(NPASS):
                    if p2 > 0:
                        sload(C17, ctok[p2])
                    bk = nb()
                    for j in range(KC):
                        transpose(PS[:, bk, j * NB:(j + 1) * NB], C17[:, j * 128:(j + 1) * 128], IDENT[0:NB, 0:NB],
                                  reads=(b_const,), writes=(b_ps[bk],))
                    act(A_(CTv[:, :, p2 * NB:(p2 + 1) * NB], PS[:, bk, 0:KC * NB].rearrange("p (c b) -> p c b", b=NB), AF.Silu),
                        reads=(b_ps[bk], b_ct), writes=(b_ct,))
            for l in range(2):
                H0T = R0f[:, 3 * D + l * 1024:3 * D + (l + 1) * 1024]
                for half in range(2):
                    bk = nb()
                    for jj in range(8):
                        j = half * 8 + jj
                        transpose(PS[:, bk, jj * 64:(jj + 1) * 64], STI[l][:, j * 128:(j + 1) * 128], IDENT[0:64, 0:64],
                                  reads=(b_const,), writes=(b_ps[bk],))
                    dve(CP(H0T[:, half * 512:(half + 1) * 512], PS[:, bk, :]), reads=(b_ps[bk],), writes=(b_R0,))
                P.dma("sp", (lambda e, H0T=H0T, l=l: e.dma_start(out=std[l], in_=H0T)), ds_std[l],
                      reads=(b_R0,), writes=(b_std[l],))

            stop("p0")
            NB2 = 2 * NB
            if pas == 0:
                for l in range(2):
                    MODT = R3f[:, l * 96 * NB2:(l + 1) * 96 * NB2]
                    MODTv = MODT.rearrange("p (n b) -> p n b", b=NB2)
                    for tno in range(96):
                        s = wq(*kblock(w_ada[l], tno * 128))
                        wv = wsv(s)
                        if tno % 8 == 0:
                            bk = nb()
                        n = tno % 8
                        mm_group(PS[:, bk, n * NB2:(n + 1) * NB2],
                                 [(wv[:, k, :], CTv[:, k, :]) for k in range(KC)],
                                 reads=(b_ws[s], b_ct), writes=(b_ps[bk],))
                        act(A_(MODTv[:, tno, :], PS[:, bk, n * NB2:(n + 1) * NB2], AF.Identity,
                               bias=BADAv[:, l, tno:tno + 1]), reads=(b_ps[bk], b_const), writes=(b_R3,))
                    for (kind, vec, addone) in ((1, 0, True), (2, 1, False), (4, 2, True), (5, 3, False)):
                        sl = MODTv[:, kind * KC:(kind + 1) * KC, :]
                        gb = VECv[:, l, vec, :].unsqueeze(2).broadcast_to([128, KC, NB2])
                        if addone:
                            dve(TS_(sl, sl, 1.0, None, ALU.add), reads=(b_R3,), writes=(b_R3,))
                        dve(TT(sl, sl, gb, ALU.mult), reads=(b_R3, b_const), writes=(b_R3,))
                    for p2 in range(NPASS):
                        P.dma("sp", (lambda e, MODTv=MODTv, l=l, p2=p2: e.dma_start(
                            out=modd[p2 * 2 + l].rearrange("p (n b) -> p n b", b=NB), in_=MODTv[:, :, p2 * NB:(p2 + 1) * NB])),
                              ds_modd[p2 * 2 + l], reads=(b_R3,), writes=(b_modd[p2 * 2 + l],))

            stop("p1")
            SSB = (5, 6, 7)

            def rstd_from_ss(dst_tile, dst_buf):
                for tt in range(3):
                    act(A_(dst_tile[:, tt * TU:(tt + 1) * TU], PS[:, SSB[tt], 0:TU], AF.Sqrt, bias=EPSC, scale=1.0 / D),
                        reads=(b_ps[SSB[tt]], b_ct), writes=(dst_buf,))
                dve(lambda e: e.reciprocal(out=dst_tile[:, 0:T], in_=dst_tile[:, 0:T]), reads=(dst_buf,), writes=(dst_buf,))

            SQ = sct(2).bitcast(BF16)
            xresv = xres.rearrange("c p t -> p c t")
            for i in range(9):
                XT = R12f[:, (i % 2) * D:(i % 2 + 1) * D]
                XF = R12f[:, 2 * D + (i % 2) * D:2 * D + (i % 2 + 1) * D]
                XFv = v3(XF, 128)
                P.dma("sp", (lambda e, XT=XT, i=i, pas=pas: e.dma_start(out=XT, in_=xtok[pas][i * 128:(i + 1) * 128, :])),
                      ds_x[i % 2], writes=(b_xt[i % 2],))
                for q in range(4):
                    bk = nb()
                    for jj in range(4):
                        j = q * 4 + jj
                        transpose(PS[:, bk, jj * 128:(jj + 1) * 128], XT[:, j * 128:(j + 1) * 128], IDENT[:],
                                  reads=(b_xt[i % 2], b_const), writes=(b_ps[bk],))
                    if q % 2 == 0:
                        act(A_(XF[:, q * 512:(q + 1) * 512], PS[:, bk, :], AF.Copy), reads=(b_ps[bk],), writes=(b_xf[i % 2],))
                    else:
                        dve(CP(XF[:, q * 512:(q + 1) * 512], PS[:, bk, :]), reads=(b_ps[bk],), writes=(b_xf[i % 2],))
                act(A_(SQ[:, 0:D], XF, AF.Square), reads=(b_xf[i % 2],), writes=(b_scr[2],))
                tt, off = (i * 128) // TU, (i * 128) % TU
                mm_group(PS[:, SSB[tt], off:off + 128], [(ONESB[:], SQ[:, j * 128:(j + 1) * 128]) for j in range(KC)],
                         reads=(b_scr[2], b_ct), writes=(b_ps[SSB[tt]],))
                P.dma("sp", (lambda e, XFv=XFv, i=i: e.dma_start(out=xresv[:, :, i * 128:(i + 1) * 128], in_=XFv)),
                      ds_xres, reads=(b_xf[i % 2],), writes=tuple(b_xres))
                if i == 7:
                    act(A_(v3(TXS, 3), XFv[:, :, 125:128], AF.Copy), reads=(b_xf[i % 2],), writes=(b_sm,))
            alias((b_const, b_R0), b_r0t)
            alias(b_xt + b_xf, (b_R1, b_R2))
            RS = r0t(0)
            rstd_from_ss(RS, b_r0t[0])

            stop("p2")
            def load_mod(l, kind):
                slot = 0
                mi = pas * 2 + l
                P.dma("sp", (lambda e, mi=mi: e.dma_start(out=MOD[:, slot, :],
                                                          in_=modd[mi][:, kind * 3 * KC * NB:(kind + 1) * 3 * KC * NB])),
                      ds_mod[slot], reads=(b_modd[mi],), writes=(b_mod[slot],))
                return slot

            ag_count = [0]

            def allgather(src_sb, src_buf, ag_in_unused, ag_out_unused, b_in_unused, b_out_unused, dst_sb, dst_buf):
                i = ag_count[0]
                ag_count[0] += 1
                ncol = dst_sb.shape[-1]
                ag_in = nc.dram_tensor("agi%d" % i, [128, ncol], F32)
                ag_out = nc.dram_tensor("ago%d" % i, [NCORES * 128, ncol], F32)
                b_in, b_out = Buf("agi%d" % i), Buf("ago%d" % i)
                d_in, d_cc, d_ld = new_ds(), new_ds(), new_ds()
                if NOCC:
                    P.dma("pool", (lambda e: e.dma_start(out=ag_out.ap()[0:128, :], in_=src_sb)), d_in, reads=(src_buf,), writes=(b_out,))
                else:
                    P.dma("pool", (lambda e: e.dma_start(out=ag_in[:, :], in_=src_sb)), d_in, reads=(src_buf,), writes=(b_in,))
                    P.dma("pool", (lambda e: e.collective_compute("AllGather", ALU.bypass, replica_groups=[list(range(NCORES))],
                                                                  ins=[ag_in.ap().opt()], outs=[ag_out.ap().opt()])),
                          d_cc, reads=(b_in,), writes=(b_out,), inc=1)
                P.dma("pool", (lambda e: e.dma_start(out=dst_sb, in_=ag_out.ap().rearrange("(r p) n -> p r n", p=128))),
                      d_ld, reads=(b_out,), writes=(dst_buf,))

            def sel_combine(dst, ncol, dst_buf):
                G = AGLv
                dve(TS_(dst, G[:, 0, 0:ncol], SEL[:, 0:1], None, ALU.mult), reads=(b_agl, b_const), writes=(dst_buf,))
                for r in range(1, 8):
                    dve(STT(dst, G[:, r, 0:ncol], SEL[:, r:r + 1], dst, ALU.mult, ALU.add), reads=(b_agl, b_const, dst_buf),
                        writes=(dst_buf,))

            def xn_pass(l, kind, XNd, b_XNd, moe=False):
                ms = 0
                SH, SC = MODv[:, ms, 0], MODv[:, ms, 1]
                XB = [sct(0), sct(1)]
                TMP = r0t(1)
                XNF = [r0t(2), r0t(3)]
                for j in range(KC):
                    xb, bxb = XB[j % 2], b_scr[j % 2]
                    xf, bxf = XNF[j % 2], b_r0t[2 + j % 2]
                    P.dma("sp", (lambda e, xb=xb, j=j: e.dma_start(out=xb[:, 0:T], in_=xres[j])), ds_x[j % 2],
                          reads=(b_xres[j],), writes=(bxb,))
                    dve(TT(TMP[:, 0:T], xb[:, 0:T], RS[:, 0:T], ALU.mult), reads=(bxb, b_r0t[0]), writes=(b_r0t[1],))
                    act(A_(xf[:, 0:TP], TMP[:, 0:TP], AF.Identity, bias=SH[:, j, 0:1], scale=SC[:, j, 0:1]),
                        reads=(b_r0t[1], b_mod[ms]), writes=(bxf,))
                    ts3 = TMP[:, TP:T].rearrange("p (b t) -> p b t", t=8)
                    xs3 = xf[:, TP:T].rearrange("p (b t) -> p b t", t=8)
                    dve(TT(ts3, ts3, SC[:, j, 1:NB].unsqueeze(2).broadcast_to([128, 16, 8]), ALU.mult),
                        reads=(b_r0t[1], b_mod[ms]), writes=(b_r0t[1],))
                    dve(TT(xs3, ts3, SH[:, j, 1:NB].unsqueeze(2).broadcast_to([128, 16, 8]), ALU.add),
                        reads=(b_r0t[1], b_mod[ms]), writes=(bxf,))
                    dve(CP(XNd[:, j, :], xf[:, 0:T]), reads=(bxf,), writes=(b_XNd,))
                    if moe:
                        for tt in range(3):
                            P.op("pe", (lambda e, j=j, tt=tt, xf=xf: e.matmul(PS[0:8, SSB[tt], 0:TU], lhsT=RWv[:, j, :],
                                                                              rhs=xf[:, tt * TU:(tt + 1) * TU],
                                                                              start=(j == 0), stop=(j == KC - 1))),
                                 reads=(bxf, b_const), writes=(b_ps[SSB[tt]],))

            def update_pass(l, kind, final):
                ms = 0
                GT = MODv[:, ms, 2]
                XB = [sct(0), sct(1)]
                TMP = r0t(2)
                RSY = r0t(1)
                SQb = sct(2).bitcast(BF16)
                for j in range(KC):
                    xb, bxb = XB[j % 2], b_scr[j % 2]
                    P.dma("sp", (lambda e, xb=xb, j=j: e.dma_start(out=xb[:, 0:T], in_=xres[j])), ds_x[j % 2],
                          reads=(b_xres[j],), writes=(bxb,))
                    dve(TT(TMP[:, 0:T], Yf[:, j, :], RSY[:, 0:T], ALU.mult), reads=(b_R1, b_R2, b_r0t[1]), writes=(b_r0t[2],))
                    dve(STT(xb[:, 0:TP], TMP[:, 0:TP], GT[:, j, 0:1], xb[:, 0:TP], ALU.mult, ALU.add),
                        reads=(b_r0t[2], b_mod[ms], bxb), writes=(bxb,))
                    ts3 = TMP[:, TP:T].rearrange("p (b t) -> p b t", t=8)
                    xs3 = xb[:, TP:T].rearrange("p (b t) -> p b t", t=8)
                    dve(TT(ts3, ts3, GT[:, j, 1:NB].unsqueeze(2).broadcast_to([128, 16, 8]), ALU.mult),
                        reads=(b_r0t[2], b_mod[ms]), writes=(b_r0t[2],))
                    dve(TT(xs3, xs3, ts3, ALU.add), reads=(b_r0t[2], bxb), writes=(bxb,))
                    if not final:
                        P.dma("sp", (lambda e, xb=xb, j=j: e.dma_start(out=xres[j], in_=xb[:, 0:T])), ds_x[j % 2],
                              reads=(bxb,), writes=(b_xres[j],))
                        act(A_(SQb[:, 0:T], xb[:, 0:T], AF.Square), reads=(bxb,), writes=(b_scr[2],))
                        for tt in range(3):
                            P.op("pe", (lambda e, j=j, tt=tt: e.matmul(PS[:, SSB[tt], 0:TU], lhsT=ONESB[:],
                                                                       rhs=SQb[:, tt * TU:(tt + 1) * TU],
                                                                       start=(j == 0), stop=(j == KC - 1))),
                                 reads=(b_scr[2], b_ct), writes=(b_ps[SSB[tt]],))
                        if kind == 1:
                            act(A_(TXS[:, j * 3:(j + 1) * 3], xb[:, TP - 3:TP], AF.Copy), reads=(bxb,), writes=(b_sm,))
                    else:
                        act(A_(Yf[:, j, :], xb[:, 0:T], AF.Copy), reads=(bxb, b_r0t[2]), writes=(b_R1, b_R2))
                if not final:
                    rstd_from_ss(RS, b_r0t[0])

            def y_stats_and_store(j, tt, bk, first, last_group):
                ysl = Yf[:, j, tt * TU:(tt + 1) * TU]
                if first:
                    act(A_(ysl, PS[:, bk, 0:TU], AF.Copy), reads=(b_ps[bk],), writes=(b_R1, b_R2))
                else:
                    dve(TT(ysl, PS[:, bk, 0:TU], ysl, ALU.add), reads=(b_ps[bk], b_R1, b_R2), writes=(b_R1, b_R2))
                if last_group:
                    SQb = sct(2).bitcast(BF16)
                    sq = SQb[:, (j % 2) * TW + tt * TU:(j % 2) * TW + (tt + 1) * TU]
                    act(A_(sq, ysl, AF.Square), reads=(b_R1, b_R2), writes=(b_scr[2],))
                    P.op("pe", (lambda e: e.matmul(PS[:, SSB[tt], 0:TU], lhsT=ONESB[:], rhs=sq,
                                                   start=(j == 0), stop=(j == KC - 1))),
                         reads=(b_scr[2], b_ct), writes=(b_ps[SSB[tt]],))

            def proj_units(W2d, c0, rhs3, b_rhs, consume):
                s = wq(*kblock(W2d, c0))
                wv = wsv(s)
                for tt in range(3):
                    bk = nb()
                    mm_group(PS[:, bk, 0:TU], [(wv[:, k, :], rhs3[:, k, tt * TU:(tt + 1) * TU]) for k in range(KC)],
                             reads=(b_ws[s], b_rhs), writes=(b_ps[bk],))
                    consume(tt, bk)

            def pair_units(WA, cA, rhsA, b_rhsA, WB, cB, rhsB, b_rhsB, consume):
                sA = wq(*kblock(WA, cA))
                sB = wq(*kblock(WB, cB))
                wvA, wvB = wsv(sA), wsv(sB)
                for tt in range(3):
                    sl = slice(tt * TU, (tt + 1) * TU)
                    ba, bb = nb(), nb()
                    mm_group(PS[:, ba, 0:TU], [(wvA[:, k, :], rhsA[:, k, sl]) for k in range(KC)],
                             reads=(b_ws[sA], b_rhsA), writes=(b_ps[ba],))
                    mm_group(PS[:, bb, 0:TU], [(wvB[:, k, :], rhsB[:, k, sl]) for k in range(KC)],
                             reads=(b_ws[sB], b_rhsB), writes=(b_ps[bb],))
                    consume(tt, ba, bb)

            def mixer(l):
                ms = load_mod(l, 0)
                SH, SC = MODv[:, ms, 0], MODv[:, ms, 1]
                if pas == 0:
                    dve(CP(TXSV[:, l * 48:(l + 1) * 48], TXS), reads=(b_sm,), writes=(b_sv,))
                else:
                    dve(CP(TX, TXSV[:, l * 48:(l + 1) * 48]), reads=(b_sv,), writes=(b_sm,))
                TXv = v3(TX, 3)
                sqt = SQ[:, 0:48]
                act(A_(sqt, TX, AF.Square), reads=(b_sm,), writes=(b_scr[2],))
                bk = nb()
                mm_group(PS[:, bk, 0:3], [(ONESB[:], sqt[:, j * 3:(j + 1) * 3]) for j in range(KC)],
                         reads=(b_scr[2], b_ct), writes=(b_ps[bk],))
                act(A_(RST, PS[:, bk, 0:3], AF.Sqrt, bias=EPSC, scale=1.0 / D), reads=(b_ps[bk], b_ct), writes=(b_sm,))
                dve(lambda e: e.reciprocal(out=RST, in_=RST), reads=(b_sm,), writes=(b_sm,))
                dve(TT(TXv, TXv, RST.unsqueeze(1).broadcast_to([128, KC, 3]), ALU.mult), reads=(b_sm,), writes=(b_sm,))
                dve(TT(TXv, TXv, SC[:, :, 0:1].broadcast_to([128, KC, 3]), ALU.mult), reads=(b_sm, b_mod[ms]), writes=(b_sm,))
                dve(TT(TXv, TXv, SH[:, :, 0:1].broadcast_to([128, KC, 3]), ALU.add), reads=(b_sm, b_mod[ms]), writes=(b_sm,))
                if pas == 0:
                    dve(lambda e: e.memset(XNT[:], 0.0), reads=(b_sm,), writes=(b_xnt,))
                else:
                    dve(CP(XNTv[:, :, 0:3], TXv), reads=(b_sm,), writes=(b_xnt,))

                stop("m%d_tail" % l)
                xn_pass(l, 0, XN1, b_R1)
                stop("m%d_xn" % l)

                P.dma("sp", (lambda e: e.dma_start(out=H0C.rearrange("p c n -> p (c n)"), in_=std[l])), ds_h0c,
                      reads=(b_std[l],), writes=(b_h0c, b_R3))
                lam = VECv[:, l, 11, :]
                act(A_(CL, lam, AF.Exp, scale=-1.0), reads=(b_const,), writes=(b_sm,))
                act(A_(CL, CL, AF.Ln, bias=ONE1), reads=(b_sm, b_ct), writes=(b_sm,))
                dve(TS_(CL2, CL, -16.0, None, ALU.mult), reads=(b_sm,), writes=(b_sm,))
                dve(TS_(CL, CL, -8.0, None, ALU.mult), reads=(b_sm,), writes=(b_sm,))

                for h in range(KC):
                    proj_units(w_in[l], D + h * 128, XN1, b_R1,
                               lambda tt, bk, h=h: act(A_(YA[:, h, tt * TU:(tt + 1) * TU], PS[:, bk, 0:TU], GELU),
                                                       reads=(b_ps[bk],), writes=(b_R2,)))

                stop("m%d_ga" % l)
                cw = [VECv[:, l, 4 + k, :] for k in range(4)]
                cb, br, bi = VECv[:, l, 8, :], VECv[:, l, 9, :], VECv[:, l, 10, :]
                for h in range(KC):
                    sA = wq(*kblock(w_in[l], h * 128))
                    sB = wq((0, 128, 128, w_r[l][h]), (128, 128, 128, w_i[l][h]))
                    wvA = wsv(sA)
                    wR, wI = WS[:, sB, 0:128], WS[:, sB, 128:256]
                    XA, bXA = r0t(h % 2), b_r0t[h % 2]
                    XC, bXC = r0t(2), b_r0t[2]
                    Rt, bR = r0t(3), b_r0t[3]
                    It, bI = r0t(4), b_r0t[4]
                    At, bA = r0t(5), b_r0t[5]
                    St, bS = r0t(6), b_r0t[6]
                    HL, bHL = sct(0), b_scr[0]
                    Pt, bP = sct(1), b_scr[1]
                    XCb, bXCb = sct(2).bitcast(BF16), b_scr[2]
                    XAs = XA[:, 3 + TP:3 + TP + 176].rearrange("p (b t) -> p b t", t=11)
                    lw = [wvA[:, k, :] for k in range(KC)]
                    b0 = nb()
                    mm_group(PS[:, b0, TU:TU + 3], [(lw[k], XNTv[:, k, 0:3]) for k in range(KC)],
                             reads=(b_ws[sA], b_xnt), writes=(b_ps[b0],))
                    banks = [b0, None, None]
                    for tt in range(3):
                        bk = b0 if tt == 0 else nb()
                        banks[tt] = bk
                        mm_group(PS[:, bk, 0:TU], [(lw[k], XN1[:, k, tt * TU:(tt + 1) * TU]) for k in range(KC)],
                                 reads=(b_ws[sA], b_R1), writes=(b_ps[bk],))
                    act(A_(XA[:, 0:3], PS[:, b0, TU:TU + 3], AF.Copy), reads=(b_ps[b0],), writes=(bXA,))
                    act(A_(XA[:, 3:3 + TU], PS[:, b0, 0:TU], AF.Copy), reads=(b_ps[b0],), writes=(bXA,))
                    act(A_(XA[:, 3 + TU:3 + 2 * TU], PS[:, banks[1], 0:TU], AF.Copy), reads=(b_ps[banks[1]],), writes=(bXA,))
                    act(A_(XA[:, 3 + 2 * TU:3 + TP], PS[:, banks[2], 0:256], AF.Copy), reads=(b_ps[banks[2]],), writes=(bXA,))
                    dve(CP(XAs[:, :, 3:11], PS[:, banks[2], 256:TU].rearrange("p (b t) -> p b t", t=8)),
                        reads=(b_ps[banks[2]],), writes=(bXA,))
                    dve(CP(XAs[:, :, 0:3], H0C[:, h, 16:64].rearrange("p (b k) -> p b k", k=3)), reads=(b_h0c,), writes=(bXA,))
                    dve(CP(OUTSv[:, h, 17:20], XA[:, TP:TP + 3]), reads=(bXA,), writes=(b_outs,))
                    dve(CP(OUTSv[:, h, 20:68].rearrange("p (b k) -> p b k", k=3), XAs[:, :, 8:11]), reads=(bXA,), writes=(b_outs,))
                    XCs = XC[:, TP:T].rearrange("p (b t) -> p b t", t=8)
                    act(A_(XC[:, 0:TP], XA[:, 3:3 + TP], AF.Identity, bias=cb[:, h:h + 1], scale=cw[3][:, h:h + 1]),
                        reads=(bXA, b_const), writes=(bXC,))
                    act(A_(XCs, XAs[:, :, 3:11], AF.Identity, bias=cb[:, h:h + 1], scale=cw[3][:, h:h + 1]),
                        reads=(bXA, b_const), writes=(bXC,))
                    for k in range(3):
                        dve(STT(XC[:, 0:TP], XA[:, k:k + TP], cw[k][:, h:h + 1], XC[:, 0:TP], ALU.mult, ALU.add),
                            reads=(bXA, b_const, bXC), writes=(bXC,))
                        dve(STT(XCs, XAs[:, :, k:k + 8], cw[k][:, h:h + 1], XCs, ALU.mult, ALU.add),
                            reads=(bXA, b_const, bXC), writes=(bXC,))
                    dve(CP(XCb[:, 0:T], XC[:, 0:T]), reads=(bXC,), writes=(bXCb,))
                    for tt in range(3):
                        bkr, bki = nb(), nb()
                        mm_group(PS[:, bkr, 0:TU], [(wR, XCb[:, tt * TU:(tt + 1) * TU])],
                                 reads=(b_ws[sB], bXCb), writes=(b_ps[bkr],))
                        mm_group(PS[:, bki, 0:TU], [(wI, XCb[:, tt * TU:(tt + 1) * TU])],
                                 reads=(b_ws[sB], bXCb), writes=(b_ps[bki],))
                        act(A_(Rt[:, tt * TU:(tt + 1) * TU], PS[:, bkr, 0:TU], AF.Sigmoid, bias=br[:, h:h + 1]),
                            reads=(b_ps[bkr], b_const), writes=(bR,))
                        act(A_(It[:, tt * TU:(tt + 1) * TU], PS[:, bki, 0:TU], AF.Sigmoid, bias=bi[:, h:h + 1]),
                            reads=(b_ps[bki], b_const), writes=(bI,))
                    act(A_(At[:, 0:T], Rt[:, 0:T], AF.Exp, scale=CL[:, h:h + 1]), reads=(bR, b_sm), writes=(bA,))
                    act(A_(St[:, 0:T], Rt[:, 0:T], AF.Exp, scale=CL2[:, h:h + 1]), reads=(bR, b_sm), writes=(bS,))
                    dve(TS_(St[:, 0:T], St[:, 0:T], -1.0, 1.0, ALU.mult, ALU.add), reads=(bS,), writes=(bS,))
                    dve(lambda e, St=St: e.tensor_scalar_max(out=St[:, 0:T], in0=St[:, 0:T], scalar1=0.0), reads=(bS,), writes=(bS,))
                    act(A_(St[:, 0:T], St[:, 0:T], AF.Sqrt), reads=(bS,), writes=(bS,))
                    dve(TT(It[:, 0:T], It[:, 0:T], St[:, 0:T], ALU.mult), reads=(bI, bS), writes=(bI,))
                    dve(TT(It[:, 0:T], It[:, 0:T], XC[:, 0:T], ALU.mult), reads=(bI, bXC), writes=(bI,))
                    dve(lambda e, HL=HL, At=At, It=It: e.tensor_tensor_scan(out=HL[:, 0:TP], data0=At[:, 0:TP], data1=It[:, 0:TP],
                                                                             initial=0.0, op0=ALU.mult, op1=ALU.add),
                        reads=(bA, bI), writes=(bHL,))
                    dve(lambda e, Pt=Pt, At=At: e.tensor_tensor_scan(out=Pt[:, 0:TP], data0=At[:, 0:TP], data1=At[:, 0:TP],
                                                                      initial=1.0, op0=ALU.mult, op1=ALU.min),
                        reads=(bA,), writes=(bP,))
                    for b in range(16):
                        c0 = TP + b * 8
                        dve(lambda e, HL=HL, At=At, It=It, c0=c0, b=b, h=h: e.tensor_tensor_scan(
                            out=HL[:, c0:c0 + 8], data0=At[:, c0:c0 + 8], data1=It[:, c0:c0 + 8],
                            initial=H0C[:, h, b:b + 1], op0=ALU.mult, op1=ALU.add),
                            reads=(bA, bI, b_h0c), writes=(bHL,))
                    dve(CP(HLL[:, h:h + 1], HL[:, TP - 1:TP]), reads=(bHL,), writes=(b_sm,))
                    dve(CP(PLL[:, h:h + 1], Pt[:, TP - 1:TP]), reads=(bP,), writes=(b_sm,))
                    dve(CP(OUTSv[:, h, 1:17], HL[:, TP:T].rearrange("p (b t) -> p b t", t=8)[:, :, 7]), reads=(bHL,), writes=(b_outs,))
                    dve(TT(Qv[:, h, :], YA[:, h, 0:TP], Pt[:, 0:TP], ALU.mult), reads=(b_R2, bP), writes=(b_R3,))
                    dve(TT(YA[:, h, :], YA[:, h, :], HL[:, 0:T], ALU.mult), reads=(b_R2, bHL), writes=(b_R2,))

                stop("m%d_lru" % l)

                alias(b_r0t, (b_R0,))
                branch_b_v(l)

                stop("m%d_v" % l)
                if pas == 0:
                    dve(lambda e: e.memset(HIN, 0.0), writes=(b_sm,))
                    dve(CP(HSV[:, l * 16:(l + 1) * 16], HLL), reads=(b_sm,), writes=(b_sv,))
                else:
                    dve(CP(HIN, HSV[:, l * 16:(l + 1) * 16]), reads=(b_sv,), writes=(b_sm,))
                for h in range(KC):
                    dve(STT(YA[:, h, 0:TP], Qv[:, h, :], HIN[:, h:h + 1], YA[:, h, 0:TP], ALU.mult, ALU.add),
                        reads=(b_R3, b_sm, b_R2), writes=(b_R2,))
                dve(TT(TMPS, PLL, HIN, ALU.mult), reads=(b_sm,), writes=(b_sm,))
                dve(TT(OUTSv[:, :, 0], TMPS, HLL, ALU.add), reads=(b_sm,), writes=(b_outs,))
                b_so = [Buf("so0"), Buf("so1")]
                alias((b_scr[0],), b_so)
                for q in range(4):
                    bk = nb()
                    for jj in range(4):
                        hh = q * 4 + jj
                        transpose(PS[0:68, bk, jj * 128:(jj + 1) * 128], OUTSv[:, hh, :], IDENT[:],
                                  reads=(b_outs, b_const), writes=(b_ps[bk],))
                    so = SCR[0:68, (q % 2) * 512:(q % 2 + 1) * 512]
                    dve(CP(so, PS[0:68, bk, :]), reads=(b_ps[bk],), writes=(b_so[q % 2],))
                    out_store((lambda e, so=so, q=q, pas=pas: e.dma_start(out=stout[pas][l][:, q * 512:(q + 1) * 512], in_=so)), (b_so[q % 2],))
                alias(b_so, (b_scr[0],))
                stop("m%d_fix" % l)
                merge_stage(l, w_pa, 4 * D, first=True)
                stop("m%d_s5" % l)
                branch_b_u(l)
                stop("m%d_u" % l)
                merge_stage(l, w_pb, 5 * D, first=False)
                stop("m%d_s3" % l)
                for j in range(KC):
                    proj_units(w_o[l], j * 128, Mv, b_R3, lambda tt, bk, j=j: y_stats_and_store(j, tt, bk, True, True))
                stop("m%d_s6" % l)
                alias((b_R0,), b_r0t)
                rstd_from_ss(r0t(1), b_r0t[1])
                update_pass(l, 0, False)

            def merge_stage(l, Wp, mgcol, first):
                for j in range(KC):
                    def consume(tt, ba, bb, j=j):
                        sl = slice(tt * TU, (tt + 1) * TU)
                        SG = sct(1)[:, tt * TU:(tt + 1) * TU]
                        act(A_(SG, PS[:, bb, 0:TU], AF.Sigmoid), reads=(b_ps[bb],), writes=(b_scr[1],))
                        if first:
                            dve(TT(Mv[:, j, sl], SG, PS[:, ba, 0:TU], ALU.mult), reads=(b_scr[1], b_ps[ba]), writes=(b_R3,))
                        else:
                            TM = sct(2)[:, tt * TU:(tt + 1) * TU]
                            dve(TT(TM, SG, PS[:, ba, 0:TU], ALU.mult), reads=(b_scr[1], b_ps[ba]), writes=(b_scr[2],))
                            dve(TT(Mv[:, j, sl], Mv[:, j, sl], TM, ALU.add), reads=(b_scr[2], b_R3), writes=(b_R3,))
                    pair_units(Wp[l], j * 128, YA, b_R2, w_in[l], mgcol + j * 128, XN1, b_R1, consume)

            def branch_b_v(l):
                STG = SCR[:, TW:TW + D].rearrange("p (g s) -> p g s", s=128)
                stg_b = (b_scr[1], b_scr[2])

                def build_mixmat(dstv, dst_buf):
                    for g4 in range(4):
                        bk = nb()
                        for gg in range(4):
                            transpose(PS[:, bk, gg * 128:(gg + 1) * 128], STG[:, g4 * 4 + gg, :], IDENT[:], reads=stg_b + (b_const,),
                                      writes=(b_ps[bk],))
                        dve(TT(dstv[:, g4 * 4:(g4 + 1) * 4, :], PS[:, bk, :].rearrange("p (g t) -> p g t", t=128),
                               MASK[:].unsqueeze(1).broadcast_to([128, 4, 128]), ALU.mult), reads=(b_ps[bk], b_const), writes=(dst_buf,))

                P.dma("sp", (lambda e: e.dma_start(out=STG, in_=w_s[l].rearrange("g t s -> t g s"))), ds_wmt, writes=stg_b)
                build_mixmat(WMTv, b_wmt)
                dve(lambda e: e.memset(SCR[:, TW:TW + D], 0.0), writes=stg_b)
                for b in range(16):
                    P.dma("sp", (lambda e, b=b: e.dma_start(out=STG[b * 8:(b + 1) * 8, :, b * 8:(b + 1) * 8],
                                                            in_=w_s[l][:, 0:8, 0:8].rearrange("g t s -> t g s"))),
                          ds_bd, reads=(), writes=stg_b)
                build_mixmat(BDv, b_bd)
                brow = SCR[:, TW:TW + D]
                bsl = b_s[l].rearrange("g t -> (g t)").unsqueeze(0)
                P.dma("sp", (lambda e: e.dma_start(out=brow[0:1, :], in_=bsl)), ds_bs, writes=(b_scr[1], b_scr[2]))
                P.dma("sp", (lambda e: e.dma_start(out=brow[32:33, :], in_=bsl)), ds_bs, writes=(b_scr[1], b_scr[2]))
                dve(lambda e: e.memset(BSR[:], 0.0), writes=(b_bsr,))
                P.dma("sp", (lambda e: e.dma_start(out=BS8[:].rearrange("p (g t) -> p g t", t=8),
                                                   in_=b_s[l][:, 0:8].partition_broadcast(128))), ds_bs, writes=(b_bs8,))
                act(A_(BSR[0:1, :], brow[0:1, :], AF.Copy), reads=(b_scr[1], b_scr[2]), writes=(b_bsr,))
                hi32 = sct(0)[32:33, 0:1024].bitcast(BF16)
                act(A_(hi32, brow[32:33, :], AF.Copy), reads=(b_scr[1], b_scr[2]), writes=(b_scr[0],))
                dve(TT(BSR[32:33, :], brow[32:33, :], hi32, ALU.subtract), reads=(b_scr[0], b_scr[1], b_scr[2]), writes=(b_bsr,))

                DG = sct(2)[:, 0:256]
                for c in range(KC):
                    dg = DG[:, (c % 2) * 128:(c % 2 + 1) * 128]
                    dve(TS_(dg, IDENT[:], VECv[:, l, 12, c:c + 1], None, ALU.mult), reads=(b_const,), writes=(b_scr[2],))
                    bk = c // 4
                    P.op("pe", (lambda e, bk=bk, c=c, dg=dg: e.matmul(PS[:, bk, (c % 4) * 128:(c % 4 + 1) * 128], lhsT=ONESF[:], rhs=dg,
                                                                     start=True, stop=True)),
                         reads=(b_scr[2], b_ct), writes=(b_ps[bk],))
                GVS = SCR[:, TW:TW + D]
                JUNK = sct(0)[:, 1024:1152].bitcast(BF16)
                vb = (4, 5, 6, 7)
                u = 0
                for blk in range(16):
                    s = wq(*kblock(w_in[l], 3 * D + blk * 128))
                    wv = wsv(s)
                    for i in range(9):
                        bk = vb[u % 4]
                        u += 1
                        mm_group(PS[:, bk, 0:128], [(XN1[:, k, i * 128:(i + 1) * 128], wv[:, k, :]) for k in range(KC)],
                                 reads=(b_ws[s], b_R1), writes=(b_ps[bk],))
                        if i < 8:
                            GV = sct(0)[:, (u % 8) * 128:(u % 8 + 1) * 128]
                            wrs = (b_scr[0],)
                        else:
                            GV = GVS[:, blk * 128:(blk + 1) * 128]
                            wrs = (b_scr[1], b_scr[2])
                        act(A_(GV, PS[:, bk, 0:128], GELU), reads=(b_ps[bk],), writes=wrs)
                        act(A_(JUNK[:, 0:128], GV, AF.Square, accum_out=SSV[:, i * 16 + blk:i * 16 + blk + 1]),
                            reads=wrs, writes=(b_sm, b_scr[0]))
                        dve(CP(VN[:, i, blk * 128:(blk + 1) * 128], GV), reads=wrs, writes=(b_R0,))
                dve(lambda e: e.tensor_reduce(out=RSV, in_=SSV.rearrange("p (i b) -> p i b", b=16), axis=AX.X, op=ALU.add),
                    reads=(b_sm,), writes=(b_sm,))
                act(A_(RSV, RSV, AF.Sqrt, bias=EPSC, scale=1.0 / D), reads=(b_sm, b_ct), writes=(b_sm,))
                dve(lambda e: e.reciprocal(out=RSV, in_=RSV), reads=(b_sm,), writes=(b_sm,))
                GVP = PS[:, 0:4, :]
                gvr = tuple(b_ps[b] for b in range(4))
                for i in range(9):
                    vi = VN[:, i, :].rearrange("p (q n) -> p q n", n=512)
                    dve(STT(vi, vi, RSV[:, i:i + 1], GVP, ALU.mult, ALU.mult), reads=(b_R0, b_sm) + gvr, writes=(b_R0,))
                gs = GVS.rearrange("p (q n) -> p q n", n=512)
                dve(STT(gs, gs, RSV[:, 8:9], GVP, ALU.mult, ALU.mult), reads=(b_scr[1], b_scr[2], b_sm) + gvr,
                    writes=(b_scr[1], b_scr[2]))
                out_store((lambda e, pas=pas: e.dma_start(out=vout[pas][l], in_=GVS)), (b_scr[1], b_scr[2]))

            def branch_b_u(l):
                GU = sct(0)
                for g in range(KC):
                    proj_units(w_in[l], 2 * D + g * 128, XN1, b_R1,
                               lambda tt, bk: act(A_(GU[:, tt * TU:(tt + 1) * TU], PS[:, bk, 0:TU], GELU),
                                                  reads=(b_ps[bk],), writes=(b_scr[0],)))
                    for tt in range(3):
                        bk = nb()
                        for sidx in range(3):
                            i = tt * 3 + sidx
                            o2 = PS[:, bk, sidx * 128:(sidx + 1) * 128]
                            if i < 8:
                                rd = (b_R0, b_wmt, b_bsr, b_ct)
                                P.op("pe", (lambda e, o2=o2, i=i, g=g: e.matmul(o2, lhsT=VN[:, i, g * 128:(g + 1) * 128], rhs=WMTv[:, g, :],
                                                                              start=True, stop=False)),
                                     reads=rd, writes=(b_ps[bk],), sig=False)
                                P.op("pe", (lambda e, o2=o2, g=g: e.matmul(o2, lhsT=ONESB[0:33, :], rhs=BSR[0:33, g * 128:(g + 1) * 128],
                                                                         start=False, stop=True)),
                                     reads=rd, writes=(b_ps[bk],), sig=True)
                            else:
                                rd = (b_R0, b_bd)
                                P.op("pe", (lambda e, o2=o2, i=i, g=g: e.matmul(o2, lhsT=VN[:, i, g * 128:(g + 1) * 128], rhs=BDv[:, g, :],
                                                                              start=True, stop=True)),
                                     reads=rd, writes=(b_ps[bk],), sig=True)
                        if tt < 2:
                            dve(TT(YA[:, g, tt * TU:(tt + 1) * TU], PS[:, bk, 0:TU], GU[:, tt * TU:(tt + 1) * TU], ALU.mult),
                                reads=(b_ps[bk], b_scr[0]), writes=(b_R2,))
                        else:
                            dve(TT(YA[:, g, 2 * TU:2 * TU + 256], PS[:, bk, 0:256], GU[:, 2 * TU:2 * TU + 256], ALU.mult),
                                reads=(b_ps[bk], b_scr[0]), writes=(b_R2,))
                            tmpb = sct(1)[:, 0:128].rearrange("p (b t) -> p b t", t=8)
                            dve(TT(tmpb, PS[:, bk, 256:TU].rearrange("p (b t) -> p b t", t=8),
                                   BS8[:, g * 8:(g + 1) * 8].unsqueeze(1).broadcast_to([128, 16, 8]), ALU.add),
                                reads=(b_ps[bk], b_bs8), writes=(b_scr[1],))
                            dve(TT(YA[:, g, TP:T], sct(1)[:, 0:128], GU[:, TP:T], ALU.mult),
                                reads=(b_scr[1], b_scr[0]), writes=(b_R2,))

            def ffn(l, final):
                ms = load_mod(l, 1)
                moe = (l == 1)
                xn_pass(l, 1, XN3, b_R3, moe=moe)
                if moe:
                    GE = moe_gates()
                alias(b_r0t, (b_R0,))
                ngroups = 8 if moe else 3
                SGt = sct(0)
                GB = sct(1)
                for grp in range(ngroups):
                    if moe:
                        Wg2, Wu2, Wd2 = moe_wg[0][grp], moe_wu[0][grp], moe_wd[0][grp]
                        c_off = 0
                        dve(TS_(SELT[:], ONESF[0:8, :], IDENT[0:8, grp:grp + 1], None, ALU.mult), reads=(b_const, b_ct), writes=(b_selt,))
                        for tt in range(3):
                            bk = nb()
                            P.op("pe", (lambda e, bk=bk, tt=tt: e.matmul(PS[:, bk, 0:TU], lhsT=SELT[:], rhs=GE[:, tt * TU:(tt + 1) * TU],
                                                                        start=True, stop=True)),
                                 reads=(b_scr[2], b_selt), writes=(b_ps[bk],))
                            act(A_(GB[:, tt * TU:(tt + 1) * TU], PS[:, bk, 0:TU], AF.Copy), reads=(b_ps[bk],), writes=(b_scr[1],))
                    else:
                        Wg2, Wu2, Wd2 = ffn_wg[0], ffn_wu[0], ffn_wd[0][grp * D:(grp + 1) * D, :]
                        c_off = grp * D
                    for hc in range(KC):
                        def consume(tt, ba, bb, hc=hc):
                            sl = slice(tt * TU, (tt + 1) * TU)
                            SG = SGt[:, sl]
                            act(A_(SG, PS[:, ba, 0:TU], AF.Silu), reads=(b_ps[ba],), writes=(b_scr[0],))
                            if moe:
                                dve(TT(SG, SG, GB[:, sl], ALU.mult), reads=(b_scr[0], b_scr[1]), writes=(b_scr[0],))
                            dve(TT(Hg[:, hc, sl], SG, PS[:, bb, 0:TU], ALU.mult), reads=(b_scr[0], b_ps[bb]), writes=(b_R0,))
                        pair_units(Wg2, c_off + hc * 128, XN3, b_R3, Wu2, c_off + hc * 128, XN3, b_R3, consume)
                    for j in range(KC):
                        proj_units(Wd2, j * 128, Hg, b_R0,
                                   lambda tt, bk, j=j, grp=grp: y_stats_and_store(j, tt, bk, grp == 0, grp == ngroups - 1))
                alias((b_R0,), b_r0t)
                rstd_from_ss(r0t(1), b_r0t[1])
                update_pass(l, 1, final)

            def moe_gates():
                LG = sct(1)[0:8, 0:T]
                for tt in range(3):
                    act(A_(LG[:, tt * TU:(tt + 1) * TU], PS[0:8, SSB[tt], 0:TU], AF.Identity, bias=RB[:, 0:1]),
                        reads=(b_ps[SSB[tt]], b_const), writes=(b_scr[1],))
                LTv = LT.rearrange("p (i e) -> p i e", e=8)
                LEv = LE.rearrange("p (i e) -> p i e", e=8)
                bk = nb()
                for i in range(9):
                    transpose(PS[:, bk, i * 8:(i + 1) * 8], LG[:, i * 128:(i + 1) * 128], IDENT[0:8, 0:8],
                              reads=(b_scr[1], b_const), writes=(b_ps[bk],))
                dve(CP(LT, PS[:, bk, 0:72]), reads=(b_ps[bk],), writes=(b_sm,))
                dve(lambda e: e.tensor_reduce(out=M1, in_=LTv, axis=AX.X, op=ALU.max), reads=(b_sm,), writes=(b_sm,))
                dve(TT(LTv, LTv, M1.unsqueeze(2).broadcast_to([128, 9, 8]), ALU.subtract), reads=(b_sm,), writes=(b_sm,))
                act(A_(LE, LT, AF.Exp), reads=(b_sm,), writes=(b_sm,))
                dve(TS_(LT, LE, 1.0, None, ALU.is_lt), reads=(b_sm,), writes=(b_sm,))
                dve(TT(LT, LT, LE, ALU.mult), reads=(b_sm,), writes=(b_sm,))
                dve(lambda e: e.tensor_reduce(out=M2, in_=LTv, axis=AX.X, op=ALU.max), reads=(b_sm,), writes=(b_sm,))
                dve(TT(LTv, LEv, M2.unsqueeze(2).broadcast_to([128, 9, 8]), ALU.is_ge), reads=(b_sm,), writes=(b_sm,))
                dve(TT(LE, LE, LT, ALU.mult), reads=(b_sm,), writes=(b_sm,))
                dve(lambda e: e.tensor_reduce(out=M1, in_=LEv, axis=AX.X, op=ALU.add), reads=(b_sm,), writes=(b_sm,))
                dve(lambda e: e.reciprocal(out=M1, in_=M1), reads=(b_sm,), writes=(b_sm,))
                dve(TT(LEv, LEv, M1.unsqueeze(2).broadcast_to([128, 9, 8]), ALU.mult), reads=(b_sm,), writes=(b_sm,))
                GE = sct(2)[0:8, 0:T]
                for tt in range(3):
                    bk = nb()
                    for sidx in range(3):
                        i = tt * 3 + sidx
                        transpose(PS[0:8, bk, sidx * 128:(sidx + 1) * 128], LEv[:, i, :], IDENT[:], reads=(b_sm, b_const), writes=(b_ps[bk],))
                    act(A_(GE[:, tt * TU:(tt + 1) * TU], PS[0:8, bk, 0:TU], AF.Copy), reads=(b_ps[bk],), writes=(b_scr[2],))
                return GE

            for l in range(2):
                mixer(l)
                stop("m%d" % l)
                ffn(l, final=(l == 1))
                stop("f%d" % l)

            b_yt = [Buf("yt%d" % i) for i in range(3)]
            alias(b_scr, b_yt)
            for i in range(9):
                for q in range(4):
                    bk = nb()
                    for jj in range(4):
                        j = q * 4 + jj
                        transpose(PS[:, bk, jj * 128:(jj + 1) * 128], Yf[:, j, i * 128:(i + 1) * 128], IDENT[:],
                                  reads=(b_R1, b_R2, b_const), writes=(b_ps[bk],))
                    slot = (i * 4 + q) % 3
                    yt = sct(slot)[:, 0:512]
                    if q % 2 == 0:
                        act(A_(yt, PS[:, bk, :], AF.Copy), reads=(b_ps[bk],), writes=(b_yt[slot],))
                    else:
                        dve(CP(yt, PS[:, bk, :]), reads=(b_ps[bk],), writes=(b_yt[slot],))
                    if i == 0 and q < 3:
                        b_yt[slot].ds = new_ds()
                        out_ds.append(b_yt[slot].ds)
                    P.dma("sp", (lambda e, yt=yt, i=i, q=q, pas=pas: e.dma_start(out=yout[pas][i * 128:(i + 1) * 128, q * 512:(q + 1) * 512], in_=yt)),
                          b_yt[slot].ds, reads=(b_yt[slot],))

        final_ev = [Ev(d.key, d.count) for d in P.dsems if d.count > 0]

        semh = {}
        for k in ("pe", "act", "dve", "pool", "sp"):
            semh[k] = es.enter_context(nc.semaphore("s_" + k))
        for d in P.dsems:
            semh[d.key] = es.enter_context(nc.semaphore("s_" + d.key))
        block = es.enter_context(nc.Block())

        @block.tensor
        def _(e):
            P.emit("pe", e, semh)

        @block.scalar
        def _(e):
            P.emit("act", e, semh)

        @block.vector
        def _(e):
            P.emit("dve", e, semh)

        @block.gpsimd
        def _(e):
            P.emit("pool", e, semh)

        @block.sync
        def _(e):
            P.emit("sp", e, semh, final=final_ev)

    print("counts", P.cnt, {d.key: d.count for d in P.dsems}, "wblocks", wcount[0])
    return nc


_CACHE = {}


def _fm(v):
    return np.ascontiguousarray(v.reshape(KC, 128).T)


def kernel(x_prompt, x_sample, c_prompt, c_sample, state_lru_h, state_lru_conv,
           w_ada, b_ada, g_pre_mix, g_post_mix, g_pre_ffn, g_post_ffn,
           w_in, conv_w, conv_b, w_r, b_r, w_i, b_i, lru_lambda, g_v, w_s, b_s,
           w_pa, w_pb, w_o, ffn_wg, ffn_wu, ffn_wd,
           router_w, router_b, moe_wg, moe_wu, moe_wd):
    f = lambda a: np.ascontiguousarray(np.asarray(a, dtype=np.float32))
    x_prompt, x_sample, c_prompt, c_sample = f(x_prompt), f(x_sample), f(c_prompt), f(c_sample)
    state_lru_h, state_lru_conv = f(state_lru_h), f(state_lru_conv)
    if "nc" not in _CACHE:
        _CACHE["nc"] = build()
    nc = _CACHE["nc"]
    vecs = np.zeros((128, 2, NVEC, KC), np.float32)
    for l in range(2):
        lst = [g_pre_mix[l], g_post_mix[l], g_pre_ffn[l], g_post_ffn[l], conv_w[l][0], conv_w[l][1], conv_w[l][2],
               conv_w[l][3], conv_b[l], b_r[l], b_i[l], lru_lambda[l], g_v[l]]
        for vi, v in enumerate(lst):
            vecs[:, l, vi, :] = _fm(f(v))
    vecs = vecs.reshape(128, -1)
    bada = np.stack([f(b_ada)[l].reshape(96, 128).T for l in range(2)], axis=1).reshape(128, 192)
    bada = np.ascontiguousarray(bada)
    ident = np.eye(128, dtype=np.float32)
    mask = np.triu(np.ones((128, 128), np.float32))
    shared = dict(vecs=vecs, bada=bada, ident=ident, mask=mask, w_ada=f(w_ada), w_in=f(w_in), w_r=f(w_r), w_i=f(w_i),
                  w_s=f(w_s), b_s=f(b_s), w_pa=f(w_pa), w_pb=f(w_pb), w_o=f(w_o), ffn_wg=f(ffn_wg), ffn_wu=f(ffn_wu),
                  ffn_wd=f(ffn_wd), router_w=f(router_w), router_b=f(router_b).reshape(8, 1), moe_wg=f(moe_wg),
                  moe_wu=f(moe_wu), moe_wd=f(moe_wd))
    in_maps = []
    for c in range(NCORES):
        xs, cs, sts = [], [], []
        for p in range(NPASS):
            sb0 = 16 * (2 * c + p)
            xs.append(np.concatenate([x_prompt[c, p * TP:(p + 1) * TP], x_sample[sb0:sb0 + 16].reshape(TS, D)], axis=0))
            cs.append(np.concatenate([c_prompt[c:c + 1], c_sample[sb0:sb0 + 16]], axis=0))
            sts.append(np.stack([np.concatenate([state_lru_h[l, sb0:sb0 + 16],
                                                 state_lru_conv[l, sb0:sb0 + 16].reshape(48, D)], axis=0) for l in range(2)]))
        sel = np.zeros((128, 8), np.float32)
        m = dict(shared)
        m.update(xtok=np.ascontiguousarray(np.stack(xs)), ctok=np.ascontiguousarray(np.stack(cs)),
                 st_in=np.ascontiguousarray(np.stack(sts)), sel=sel)
        in_maps.append(m)
    res = run_bass_kernel_spmd(nc, in_maps, core_ids=list(range(NCORES)))
    R = res.results
    B, S = x_prompt.shape[0], x_prompt.shape[1]
    y_prompt = np.zeros((B, S, D), np.float32)
    y_sample = np.zeros((128, 8, D), np.float32)
    h_p = np.zeros((2, B, D), np.float32)
    conv_p = np.zeros((2, B, 3, D), np.float32)
    h_s = np.zeros((2, 128, D), np.float32)
    conv_s = np.zeros((2, 128, 3, D), np.float32)
    v_s = np.zeros((2, 128, 8, D), np.float32)
    for c in range(NCORES):
        for p in range(NPASS):
            sb0 = 16 * (2 * c + p)
            yo, so, vo = R[c]["yout"][p], R[c]["stout"][p], R[c]["vout"][p]
            y_prompt[c, p * TP:(p + 1) * TP] = yo[:TP]
            y_sample[sb0:sb0 + 16] = yo[TP:].reshape(16, 8, D)
            for l in range(2):
                if p == 1:
                    h_p[l, c] = so[l, 0]
                    conv_p[l, c] = so[l, 17:20]
                h_s[l, sb0:sb0 + 16] = so[l, 1:17]
                conv_s[l, sb0:sb0 + 16] = so[l, 20:68].reshape(16, 3, D)
                v_s[l, sb0:sb0 + 16] = vo[l].reshape(16, 8, D)
    return (y_prompt, y_sample, h_p, conv_p, h_s, conv_s, v_s)
```

```python
import contextlib
import numpy as np
import concourse.bass as bass
import concourse.mybir as mybir
from concourse.bass_utils import run_bass_kernel_spmd

F32 = mybir.dt.float32
BF16 = mybir.dt.bfloat16
AF = mybir.ActivationFunctionType
ALU = mybir.AluOpType
AX = mybir.AxisListType
GELU = AF.Gelu_apprx_tanh

NCORES = 4
NPASS = 2
D = 2048
KC = 16
T = 1152
TP = 1024
TS = 128
NB = 17
TU = 384
EPS = 1e-6
NVEC = 13
TW = 1216


class Ev:
    __slots__ = ("sem", "val")

    def __init__(self, sem, val):
        self.sem, self.val = sem, val


class Buf:
    def __init__(self, name):
        self.name = name
        self.wr = None
        self.rds = {}


class DSem:
    def __init__(self, key):
        self.key = key
        self.count = 0


class Prog:
    ENGS = ("pe", "act", "dve", "pool", "sp")

    def __init__(self):
        self.q = {e: [] for e in self.ENGS}
        self.cnt = {e: 0 for e in self.ENGS}
        self.dsems = []
        self.halt = False

    def new_dsem(self):
        d = DSem("d%d" % len(self.dsems))
        self.dsems.append(d)
        return d

    @staticmethod
    def _waits(reads, writes, extra):
        w = []
        for b in reads:
            if b.wr is not None:
                w.append(b.wr)
        for b in writes:
            if b.wr is not None:
                w.append(b.wr)
            for s, v in b.rds.items():
                w.append(Ev(s, v))
        for e in extra:
            if e is not None:
                w.append(e)
        return w

    def op(self, eng, fn, reads=(), writes=(), extra=(), sig=True):
        if self.halt:
            return None
        waits = self._waits(reads, writes, extra)
        ev = None
        if sig:
            self.cnt[eng] += 1
            ev = Ev(eng, self.cnt[eng])
            for b in reads:
                if b.rds.get(eng, 0) < ev.val:
                    b.rds[eng] = ev.val
            for b in writes:
                b.wr = ev
                b.rds = {}
        self.q[eng].append((fn, waits, eng if sig else None, 1))
        return ev

    def dma(self, queue, fn, dsem, reads=(), writes=(), extra=(), inc=16):
        if self.halt:
            return None
        waits = self._waits(reads, writes, extra)
        dsem.count += inc
        ev = Ev(dsem.key, dsem.count)
        for b in reads:
            if b.rds.get(dsem.key, 0) < ev.val:
                b.rds[dsem.key] = ev.val
        for b in writes:
            b.wr = ev
            b.rds = {}
        self.q[queue].append((fn, waits, dsem.key, inc))
        return ev

    def emit(self, name, eng, semh, final=()):
        seen = {}

        def wait(s, v):
            if seen.get(s, 0) < v:
                eng.wait_ge(semh[s], v)
                seen[s] = v

        for fn, waits, sem, inc in self.q[name]:
            need = {}
            for ev in waits:
                if need.get(ev.sem, 0) < ev.val:
                    need[ev.sem] = ev.val
            for s, v in need.items():
                wait(s, v)
            ins = fn(eng)
            if sem is not None:
                if inc == 1 and name == "pool":
                    ins.then_inc(semh[sem])
                else:
                    ins.then_inc(semh[sem], inc)
        for ev in final:
            wait(ev.sem, ev.val)


class _Stop(Exception):
    pass


def build(debug_stop=None):
    import os
    STOP = os.environ.get('MK_STOP', '')
    NOCC = os.environ.get('MK_NOCC', '') == '1'

    def stop(label):
        if STOP == label:
            P.halt = True
            print("STOPPED at", label)

    nc = bass.Bass("TRN2", target_bir_lowering=False)
    P = Prog()

    def din(name, shape, dt=F32):
        return nc.dram_tensor(name, list(shape), dt, kind="ExternalInput").ap()

    def dout(name, shape):
        return nc.dram_tensor(name, list(shape), F32, kind="ExternalOutput").ap()

    xtok = din("xtok", [NPASS, T, D])
    ctok = din("ctok", [NPASS, NB, D])
    st_in = din("st_in", [NPASS, 2, 64, D])
    sel_d = din("sel", [128, 8])
    vecs_d = din("vecs", [128, 2 * NVEC * KC])
    bada_d = din("bada", [128, 2 * 96])
    ident_d = din("ident", [128, 128])
    mask_d = din("mask", [128, 128])
    w_ada = din("w_ada", [2, D, 6 * D])
    w_in = din("w_in", [2, D, 6 * D])
    w_r = din("w_r", [2, 16, 128, 128])
    w_i = din("w_i", [2, 16, 128, 128])
    w_s = din("w_s", [2, 16, 128, 128])
    b_s = din("b_s", [2, 16, 128])
    w_pa = din("w_pa", [2, D, D])
    w_pb = din("w_pb", [2, D, D])
    w_o = din("w_o", [2, D, D])
    ffn_wg = din("ffn_wg", [1, D, 3 * D])
    ffn_wu = din("ffn_wu", [1, D, 3 * D])
    ffn_wd = din("ffn_wd", [1, 3 * D, D])
    router_w = din("router_w", [1, D, 8])
    router_b = din("router_b", [8, 1])
    moe_wg = din("moe_wg", [1, 8, D, D])
    moe_wu = din("moe_wu", [1, 8, D, D])
    moe_wd = din("moe_wd", [1, 8, D, D])

    yout = dout("yout", [NPASS, T, D])
    stout = dout("stout", [NPASS, 2, 68, D])
    vout = dout("vout", [NPASS, 2, 128, D])

    xres = nc.dram_tensor("xres", [KC, 128, T], F32).ap()
    modd = nc.dram_tensor("modd", [4, 128, 96 * NB], F32).ap()
    std = nc.dram_tensor("std", [2, 128, KC * 64], F32).ap()
    agx_in = nc.dram_tensor("agx_in", [128, 48], F32)
    agx_out = nc.dram_tensor("agx_out", [NCORES * 128, 48], F32)
    agh_in = nc.dram_tensor("agh_in", [128, 16], F32)
    agh_out = nc.dram_tensor("agh_out", [NCORES * 128, 16], F32)

    es = contextlib.ExitStack()
    with es:
        def sb(name, shape, dt):
            return es.enter_context(nc.sbuf_tensor(name, list(shape), dt))

        R12 = sb("R12", [128, 2 * KC * T], BF16)
        R03 = sb("R03", [128, 2 * KC * T], BF16)
        WS = sb("WS", [128, 4, 2048], BF16)
        SCR = sb("SCR", [128, 3 * TW], F32)
        IDENT = sb("IDENT", [128, 128], F32)
        MASK = sb("MASK", [128, 128], F32)
        ONESB = sb("ONESB", [128, 128], BF16)
        ONESF = sb("ONESF", [128, 128], F32)
        VECS = sb("VECS", [128, 2 * NVEC * KC], F32)
        BADA = sb("BADA", [128, 192], F32)
        SEL = sb("SEL", [128, 8], F32)
        FLAG = sb("FLAG", [128, 1], F32)
        MOD = sb("MOD", [128, 1, 3 * KC * NB], F32)
        BS8 = sb("BS8", [128, 128], F32)
        CT = sb("CT", [128, KC * 2 * NB], BF16)
        WMT = sb("WMT", [128, KC * 128], BF16)
        BD = sb("BD", [128, KC * 128], BF16)
        BSR = sb("BSR", [33, KC * 128], BF16)
        OUTS = sb("OUTS", [128, KC * 68], F32)
        SM = sb("SM", [128, 768], F32)
        XNT = sb("XNT", [128, KC * 4], BF16)
        AGL = sb("AGL", [128, 8 * 48], F32)
        RW = sb("RW", [128, KC * 8], F32)
        RB = sb("RB", [8, 1], F32)
        PS = es.enter_context(nc.psum_tensor("PS", [128, 8, 512], F32))
        print('SBUF remaining', nc.sbuf_bytes_remaining)

        R1 = R12[:, 0:KC * T]
        R2 = R12[:, KC * T:2 * KC * T]
        R0 = R03[:, 0:KC * T]
        R3 = R03[:, KC * T:2 * KC * T]
        Yf = R12[:].bitcast(F32).rearrange("p (c t) -> p c t", t=T)
        XNEW = R03[:].bitcast(F32).rearrange("p (c t) -> p c t", t=T)
        R0f = R0.bitcast(F32)
        R2f = R2.bitcast(F32)
        R3f = R3.bitcast(F32)
        R12f = R12[:].bitcast(F32)

        def v3(ap, n):
            return ap.rearrange("p (c n) -> p c n", n=n)

        XN1 = v3(R1, T)
        XN3 = v3(R3[:, 0:KC * T], T)
        YA = v3(R2, T)
        Mv = v3(R3, T)
        Qv = v3(R3[:, 0:KC * TP], TP)
        H0C = v3(R3f[:, 8192:9216], 64)
        VN = v3(R0, D)
        Hg = v3(R0, T)
        VECv = VECS[:].rearrange("p (l v c) -> p l v c", l=2, v=NVEC)
        BADAv = BADA[:].rearrange("p (l n) -> p l n", l=2)
        MODv = MOD[:].rearrange("p s (k c b) -> p s k c b", k=3, c=KC)
        CTv = v3(CT[:], 2 * NB)
        WMTv = v3(WMT[:], 128)
        BDv = v3(BD[:], 128)
        OUTSv = v3(OUTS[:], 68)
        XNTv = v3(XNT[:], 4)
        AGLv = v3(AGL[:], 48)
        RWv = v3(RW[:], 8)

        def r0t(i):
            return R0f[:, i * TW:(i + 1) * TW]

        def sct(i):
            return SCR[:, i * TW:(i + 1) * TW]

        RSV = SM[:, 0:9]
        SSV = SM[:, 512:656]
        HLL = SM[:, 96:112]
        PLL = SM[:, 112:128]
        HIN = SM[:, 128:144]
        CL = SM[:, 144:160]
        CL2 = SM[:, 160:176]
        TX = SM[:, 176:224]
        TXS = SM[:, 224:272]
        RST = SM[:, 272:275]
        LT = SM[:, 288:360]
        LE = SM[:, 360:432]
        M1 = SM[:, 432:441]
        M2 = SM[:, 448:457]
        TMPS = SM[:, 464:480]

        b_R0, b_R1, b_R2, b_R3 = Buf("R0"), Buf("R1"), Buf("R2"), Buf("R3")
        b_ws = [Buf("ws%d" % i) for i in range(4)]
        b_scr = [Buf("scr0"), Buf("scr1"), Buf("scr2")]
        b_r0t = [Buf("r0t%d" % i) for i in range(7)]
        b_ps = [Buf("ps%d" % i) for i in range(8)]
        b_const = Buf("const")
        b_mod = [Buf("mod0"), Buf("mod1")]
        b_ct = Buf("ct")
        b_wmt, b_bd, b_bsr, b_bs8 = Buf("wmt"), Buf("bd"), Buf("bsr"), Buf("bs8")
        b_outs, b_sm, b_xnt, b_agl = Buf("outs"), Buf("sm"), Buf("xnt"), Buf("agl")
        b_xres = [Buf("xres%d" % j) for j in range(KC)]
        b_modd = [Buf("modd%d" % i) for i in range(4)]
        b_std = [Buf("std0"), Buf("std1")]
        b_agx, b_agxo, b_agh, b_agho = Buf("agx"), Buf("agxo"), Buf("agh"), Buf("agho")
        b_h0c = Buf("h0c")

        ds_setup = P.new_dsem()
        ds_ws = [P.new_dsem() for _ in range(4)]
        ds_misc = [P.new_dsem() for _ in range(4)]
        ds_out = P.new_dsem()
        ds_x = [P.new_dsem(), P.new_dsem(), P.new_dsem()]
        ds_cc = P.new_dsem()
        out_events = []

        bank_rr = [0]

        def nb():
            b = bank_rr[0]
            bank_rr[0] = (b + 1) % 5
            return b

        def act(fn, reads=(), writes=()):
            return P.op("act", fn, reads, writes)

        def dve(fn, reads=(), writes=()):
            return P.op("dve", fn, reads, writes)

        def A_(out, in_, func, bias=None, scale=None, accum_out=None):
            kw = {}
            if bias is not None:
                kw["bias"] = bias
            if scale is not None:
                kw["scale"] = scale
            if accum_out is not None:
                kw["accum_out"] = accum_out
            return lambda e: e.activation(out=out, in_=in_, func=func, **kw)

        def TT(out, in0, in1, op):
            return lambda e: e.tensor_tensor(out=out, in0=in0, in1=in1, op=op)

        def TS_(out, in0, s1, s2, op0, op1=None):
            if op1 is None:
                return lambda e: e.tensor_scalar(out=out, in0=in0, scalar1=s1, scalar2=None, op0=op0)
            return lambda e: e.tensor_scalar(out=out, in0=in0, scalar1=s1, scalar2=s2, op0=op0, op1=op1)

        def STT(out, in0, scalar, in1, op0, op1):
            return lambda e: e.scalar_tensor_tensor(out=out, in0=in0, scalar=scalar, in1=in1, op0=op0, op1=op1)

        def CP(out, in_):
            return lambda e: e.tensor_copy(out=out, in_=in_)

        def mm_group(out, pairs, reads, writes, extra_first=()):
            n = len(pairs)
            ev = None
            for i, (l, r) in enumerate(pairs):
                fn = (lambda e, l=l, r=r, i=i: e.matmul(out, lhsT=l, rhs=r, start=(i == 0), stop=(i == n - 1)))
                last = (i == n - 1)
                if i == 0 or last:
                    ev = P.op("pe", fn, reads, writes, extra=extra_first if i == 0 else (), sig=last)
                else:
                    P.op("pe", fn, (), (), sig=False)
            return ev

        def transpose(out, in_, ident, reads, writes):
            return P.op("pe", lambda e: e.transpose(out, in_, ident), reads, writes)

        wcount = [0]

        def wq(*parts):
            i = wcount[0]
            wcount[0] += 1
            sl = i % 4
            for (off, n, inner, src) in parts:
                dst = WS[:, sl, off:off + n].rearrange("p (a b) -> p a b", b=inner)
                P.dma("pool", (lambda e, dst=dst, src=src: e.dma_start(out=dst, in_=src)),
                      ds_ws[sl], reads=(), writes=(b_ws[sl],))
            return sl

        def kblock(W2d, c0, ncols=128):
            src = W2d.rearrange("(c p) n -> p c n", p=128)[:, :, c0:c0 + ncols]
            return ((0, KC * ncols, ncols, src),)

        def wsv(sl, ncols=128):
            return WS[:, sl, 0:KC * ncols].rearrange("p (c n) -> p c n", n=ncols)

        def alias(frm, to):
            for t in to:
                for f in frm:
                    if f.wr is not None and t.rds.get(f.wr.sem, 0) < f.wr.val:
                        t.rds[f.wr.sem] = f.wr.val
                    for sk, v in f.rds.items():
                        if t.rds.get(sk, 0) < v:
                            t.rds[sk] = v

        def new_ds():
            return P.new_dsem()

        SELT = sb("SELT", [8, 128], F32)
        ONE1 = ONESF[:, 0:1]
        b_selt = Buf("selt")
        ds_std = [new_ds(), new_ds()]
        ds_modd = [new_ds() for _ in range(4)]
        ds_mod = [new_ds(), new_ds()]
        ds_h0c, ds_wmt, ds_bd, ds_bs, ds_ag, ds_agl = new_ds(), new_ds(), new_ds(), new_ds(), new_ds(), new_ds()
        ds_xres = new_ds()
        ds_bs2, ds_bs8 = new_ds(), new_ds()
        out_ds = []

        def out_store(fn, stage_bufs):
            d = new_ds()
            out_ds.append(d)
            return P.dma("sp", fn, d, reads=stage_bufs)

        def sload(dst, src):
            P.dma("sp", (lambda e: e.dma_start(out=dst, in_=src)), ds_setup, writes=(b_const,))

        sload(IDENT[:], ident_d)
        sload(MASK[:], mask_d)
        sload(VECS[:], vecs_d)
        sload(BADA[:], bada_d)
        sload(SEL[:], sel_d)
        sload(RW[:].rearrange("p (c e) -> p c e", e=8), router_w[0].rearrange("(c p) e -> p c e", p=128))
        sload(RB[:], router_b)
        C17 = R0f[0:NB, 0:D]
        STI = [R0f[0:64, D:2 * D], R0f[0:64, 2 * D:3 * D]]
        TXSV = sb("TXSV", [128, 2 * 48], F32)
        HSV = sb("HSV", [128, 2 * 16], F32)
        b_sv = Buf("sv")
        dve(lambda e: e.memset(ONESB[:], 1.0), writes=(b_ct,))
        dve(lambda e: e.memset(ONESF[:], 1.0), writes=(b_ct,))
        EPSC = SM[:, 500:501]
        dve(lambda e: e.memset(EPSC, EPS), writes=(b_ct,))
        dve(lambda e: e.tensor_reduce(out=FLAG[:], in_=SEL[:], axis=AX.X, op=ALU.add), reads=(b_const,), writes=(b_sm,))

        b_xt = [Buf("xt0"), Buf("xt1")]
        b_xf = [Buf("xf0"), Buf("xf1")]
        for pas in range(NPASS):
            alias([b_R0, b_R1, b_R2, b_R3] + b_r0t + b_scr, [b_const] + b_xt + b_xf)
            sload(C17, ctok[pas])
            sload(STI[0], st_in[pas][0])
            sload(STI[1], st_in[pas][1])
            if pas == 0:
                for p2 in range(NPASS):
                    if p2 > 0:
                        sload(C17, ctok[p2])
                    bk = nb()
                    for j in range(KC):
                        transpose(PS[:, bk, j * NB:(j + 1) * NB], C17[:, j * 128:(j + 1) * 128], IDENT[0:NB, 0:NB],
                                  reads=(b_const,), writes=(b_ps[bk],))
                    act(A_(CTv[:, :, p2 * NB:(p2 + 1) * NB], PS[:, bk, 0:KC * NB].rearrange("p (c b) -> p c b", b=NB), AF.Silu),
                        reads=(b_ps[bk], b_ct), writes=(b_ct,))
            for l in range(2):
                H0T = R0f[:, 3 * D + l * 1024:3 * D + (l + 1) * 1024]
                for half in range(2):
                    bk = nb()
                    for jj in range(8):
                        j = half * 8 + jj
                        transpose(PS[:, bk, jj * 64:(jj + 1) * 64], STI[l][:, j * 128:(j + 1) * 128], IDENT[0:64, 0:64],
                                  reads=(b_const,), writes=(b_ps[bk],))
                    dve(CP(H0T[:, half * 512:(half + 1) * 512], PS[:, bk, :]), reads=(b_ps[bk],), writes=(b_R0,))
                P.dma("sp", (lambda e, H0T=H0T, l=l: e.dma_start(out=std[l], in_=H0T)), ds_std[l],
                      reads=(b_R0,), writes=(b_std[l],))

            stop("p0")
            NB2 = 2 * NB
            if pas == 0:
                for l in range(2):
                    MODT = R3f[:, l * 96 * NB2:(l + 1) * 96 * NB2]
                    MODTv = MODT.rearrange("p (n b) -> p n b", b=NB2)
                    for tno in range(96):
                        s = wq(*kblock(w_ada[l], tno * 128))
                        wv = wsv(s)
                        if tno % 8 == 0:
                            bk = nb()
                        n = tno % 8
                        mm_group(PS[:, bk, n * NB2:(n + 1) * NB2],
                                 [(wv[:, k, :], CTv[:, k, :]) for k in range(KC)],
                                 reads=(b_ws[s], b_ct), writes=(b_ps[bk],))
                        act(A_(MODTv[:, tno, :], PS[:, bk, n * NB2:(n + 1) * NB2], AF.Identity,
                               bias=BADAv[:, l, tno:tno + 1]), reads=(b_ps[bk], b_const), writes=(b_R3,))
                    for (kind, vec, addone) in ((1, 0, True), (2, 1, False), (4, 2, True), (5, 3, False)):
                        sl = MODTv[:, kind * KC:(kind + 1) * KC, :]
                        gb = VECv[:, l, vec, :].unsqueeze(2).broadcast_to([128, KC, NB2])
                        if addone:
                            dve(TS_(sl, sl, 1.0, None, ALU.add), reads=(b_R3,), writes=(b_R3,))
                        dve(TT(sl, sl, gb, ALU.mult), reads=(b_R3, b_const), writes=(b_R3,))
                    for p2 in range(NPASS):
                        P.dma("sp", (lambda e, MODTv=MODTv, l=l, p2=p2: e.dma_start(
                            out=modd[p2 * 2 + l].rearrange("p (n b) -> p n b", b=NB), in_=MODTv[:, :, p2 * NB:(p2 + 1) * NB])),
                              ds_modd[p2 * 2 + l], reads=(b_R3,), writes=(b_modd[p2 * 2 + l],))

            stop("p1")
            SSB = (5, 6, 7)

            def rstd_from_ss(dst_tile, dst_buf):
                for tt in range(3):
                    act(A_(dst_tile[:, tt * TU:(tt + 1) * TU], PS[:, SSB[tt], 0:TU], AF.Sqrt, bias=EPSC, scale=1.0 / D),
                        reads=(b_ps[SSB[tt]], b_ct), writes=(dst_buf,))
                dve(lambda e: e.reciprocal(out=dst_tile[:, 0:T], in_=dst_tile[:, 0:T]), reads=(dst_buf,), writes=(dst_buf,))

            SQ = sct(2).bitcast(BF16)
            xresv = xres.rearrange("c p t -> p c t")
            for i in range(9):
                XT = R12f[:, (i % 2) * D:(i % 2 + 1) * D]
                XF = R12f[:, 2 * D + (i % 2) * D:2 * D + (i % 2 + 1) * D]
                XFv = v3(XF, 128)
                P.dma("sp", (lambda e, XT=XT, i=i, pas=pas: e.dma_start(out=XT, in_=xtok[pas][i * 128:(i + 1) * 128, :])),
                      ds_x[i % 2], writes=(b_xt[i % 2],))
                for q in range(4):
                    bk = nb()
                    for jj in range(4):
                        j = q * 4 + jj
                        transpose(PS[:, bk, jj * 128:(jj + 1) * 128], XT[:, j * 128:(j + 1) * 128], IDENT[:],
                                  reads=(b_xt[i % 2], b_const), writes=(b_ps[bk],))
                    if q % 2 == 0:
                        act(A_(XF[:, q * 512:(q + 1) * 512], PS[:, bk, :], AF.Copy), reads=(b_ps[bk],), writes=(b_xf[i % 2],))
                    else:
                        dve(CP(XF[:, q * 512:(q + 1) * 512], PS[:, bk, :]), reads=(b_ps[bk],), writes=(b_xf[i % 2],))
                act(A_(SQ[:, 0:D], XF, AF.Square), reads=(b_xf[i % 2],), writes=(b_scr[2],))
                tt, off = (i * 128) // TU, (i * 128) % TU
                mm_group(PS[:, SSB[tt], off:off + 128], [(ONESB[:], SQ[:, j * 128:(j + 1) * 128]) for j in range(KC)],
                         reads=(b_scr[2], b_ct), writes=(b_ps[SSB[tt]],))
                P.dma("sp", (lambda e, XFv=XFv, i=i: e.dma_start(out=xresv[:, :, i * 128:(i + 1) * 128], in_=XFv)),
                      ds_xres, reads=(b_xf[i % 2],), writes=tuple(b_xres))
                if i == 7:
                    act(A_(v3(TXS, 3), XFv[:, :, 125:128], AF.Copy), reads=(b_xf[i % 2],), writes=(b_sm,))
            alias((b_const, b_R0), b_r0t)
            alias(b_xt + b_xf, (b_R1, b_R2))
            RS = r0t(0)
            rstd_from_ss(RS, b_r0t[0])

            stop("p2")
            def load_mod(l, kind):
                slot = 0
                mi = pas * 2 + l
                P.dma("sp", (lambda e, mi=mi: e.dma_start(out=MOD[:, slot, :],
                                                          in_=modd[mi][:, kind * 3 * KC * NB:(kind + 1) * 3 * KC * NB])),
                      ds_mod[slot], reads=(b_modd[mi],), writes=(b_mod[slot],))
                return slot

            ag_count = [0]

            def allgather(src_sb, src_buf, ag_in_unused, ag_out_unused, b_in_unused, b_out_unused, dst_sb, dst_buf):
                i = ag_count[0]
                ag_count[0] += 1
                ncol = dst_sb.shape[-1]
                ag_in = nc.dram_tensor("agi%d" % i, [128, ncol], F32)
                ag_out = nc.dram_tensor("ago%d" % i, [NCORES * 128, ncol], F32)
                b_in, b_out = Buf("agi%d" % i), Buf("ago%d" % i)
                d_in, d_cc, d_ld = new_ds(), new_ds(), new_ds()
                if NOCC:
                    P.dma("pool", (lambda e: e.dma_start(out=ag_out.ap()[0:128, :], in_=src_sb)), d_in, reads=(src_buf,), writes=(b_out,))
                else:
                    P.dma("pool", (lambda e: e.dma_start(out=ag_in[:, :], in_=src_sb)), d_in, reads=(src_buf,), writes=(b_in,))
                    P.dma("pool", (lambda e: e.collective_compute("AllGather", ALU.bypass, replica_groups=[list(range(NCORES))],
                                                                  ins=[ag_in.ap().opt()], outs=[ag_out.ap().opt()])),
                          d_cc, reads=(b_in,), writes=(b_out,), inc=1)
                P.dma("pool", (lambda e: e.dma_start(out=dst_sb, in_=ag_out.ap().rearrange("(r p) n -> p r n", p=128))),
                      d_ld, reads=(b_out,), writes=(dst_buf,))

            def sel_combine(dst, ncol, dst_buf):
                G = AGLv
                dve(TS_(dst, G[:, 0, 0:ncol], SEL[:, 0:1], None, ALU.mult), reads=(b_agl, b_const), writes=(dst_buf,))
                for r in range(1, 8):
                    dve(STT(dst, G[:, r, 0:ncol], SEL[:, r:r + 1], dst, ALU.mult, ALU.add), reads=(b_agl, b_const, dst_buf),
                        writes=(dst_buf,))

            def xn_pass(l, kind, XNd, b_XNd, moe=False):
                ms = 0
                SH, SC = MODv[:, ms, 0], MODv[:, ms, 1]
                XB = [sct(0), sct(1)]
                TMP = r0t(1)
                XNF = [r0t(2), r0t(3)]
                for j in range(KC):
                    xb, bxb = XB[j % 2], b_scr[j % 2]
                    xf, bxf = XNF[j % 2], b_r0t[2 + j % 2]
                    P.dma("sp", (lambda e, xb=xb, j=j: e.dma_start(out=xb[:, 0:T], in_=xres[j])), ds_x[j % 2],
                          reads=(b_xres[j],), writes=(bxb,))
                    dve(TT(TMP[:, 0:T], xb[:, 0:T], RS[:, 0:T], ALU.mult), reads=(bxb, b_r0t[0]), writes=(b_r0t[1],))
                    act(A_(xf[:, 0:TP], TMP[:, 0:TP], AF.Identity, bias=SH[:, j, 0:1], scale=SC[:, j, 0:1]),
                        reads=(b_r0t[1], b_mod[ms]), writes=(bxf,))
                    ts3 = TMP[:, TP:T].rearrange("p (b t) -> p b t", t=8)
                    xs3 = xf[:, TP:T].rearrange("p (b t) -> p b t", t=8)
                    dve(TT(ts3, ts3, SC[:, j, 1:NB].unsqueeze(2).broadcast_to([128, 16, 8]), ALU.mult),
                        reads=(b_r0t[1], b_mod[ms]), writes=(b_r0t[1],))
                    dve(TT(xs3, ts3, SH[:, j, 1:NB].unsqueeze(2).broadcast_to([128, 16, 8]), ALU.add),
                        reads=(b_r0t[1], b_mod[ms]), writes=(bxf,))
                    dve(CP(XNd[:, j, :], xf[:, 0:T]), reads=(bxf,), writes=(b_XNd,))
                    if moe:
                        for tt in range(3):
                            P.op("pe", (lambda e, j=j, tt=tt, xf=xf: e.matmul(PS[0:8, SSB[tt], 0:TU], lhsT=RWv[:, j, :],
                                                                              rhs=xf[:, tt * TU:(tt + 1) * TU],
                                                                              start=(j == 0), stop=(j == KC - 1))),
                                 reads=(bxf, b_const), writes=(b_ps[SSB[tt]],))

            def update_pass(l, kind, final):
                ms = 0
                GT = MODv[:, ms, 2]
                XB = [sct(0), sct(1)]
                TMP = r0t(2)
                RSY = r0t(1)
                SQb = sct(2).bitcast(BF16)
                for j in range(KC):
                    xb, bxb = XB[j % 2], b_scr[j % 2]
                    P.dma("sp", (lambda e, xb=xb, j=j: e.dma_start(out=xb[:, 0:T], in_=xres[j])), ds_x[j % 2],
                          reads=(b_xres[j],), writes=(bxb,))
                    dve(TT(TMP[:, 0:T], Yf[:, j, :], RSY[:, 0:T], ALU.mult), reads=(b_R1, b_R2, b_r0t[1]), writes=(b_r0t[2],))
                    dve(STT(xb[:, 0:TP], TMP[:, 0:TP], GT[:, j, 0:1], xb[:, 0:TP], ALU.mult, ALU.add),
                        reads=(b_r0t[2], b_mod[ms], bxb), writes=(bxb,))
                    ts3 = TMP[:, TP:T].rearrange("p (b t) -> p b t", t=8)
                    xs3 = xb[:, TP:T].rearrange("p (b t) -> p b t", t=8)
                    dve(TT(ts3, ts3, GT[:, j, 1:NB].unsqueeze(2).broadcast_to([128, 16, 8]), ALU.mult),
                        reads=(b_r0t[2], b_mod[ms]), writes=(b_r0t[2],))
                    dve(TT(xs3, xs3, ts3, ALU.add), reads=(b_r0t[2], bxb), writes=(bxb,))
                    if not final:
                        P.dma("sp", (lambda e, xb=xb, j=j: e.dma_start(out=xres[j], in_=xb[:, 0:T])), ds_x[j % 2],
                              reads=(bxb,), writes=(b_xres[j],))
                        act(A_(SQb[:, 0:T], xb[:, 0:T], AF.Square), reads=(bxb,), writes=(b_scr[2],))
                        for tt in range(3):
                            P.op("pe", (lambda e, j=j, tt=tt: e.matmul(PS[:, SSB[tt], 0:TU], lhsT=ONESB[:],
                                                                       rhs=SQb[:, tt * TU:(tt + 1) * TU],
                                                                       start=(j == 0), stop=(j == KC - 1))),
                                 reads=(b_scr[2], b_ct), writes=(b_ps[SSB[tt]],))
                        if kind == 1:
                            act(A_(TXS[:, j * 3:(j + 1) * 3], xb[:, TP - 3:TP], AF.Copy), reads=(bxb,), writes=(b_sm,))
                    else:
                        act(A_(Yf[:, j, :], xb[:, 0:T], AF.Copy), reads=(bxb, b_r0t[2]), writes=(b_R1, b_R2))
                if not final:
                    rstd_from_ss(RS, b_r0t[0])

            def y_stats_and_store(j, tt, bk, first, last_group):
                ysl = Yf[:, j, tt * TU:(tt + 1) * TU]
                if first:
                    act(A_(ysl, PS[:, bk, 0:TU], AF.Copy), reads=(b_ps[bk],), writes=(b_R1, b_R2))
                else:
                    dve(TT(ysl, PS[:, bk, 0:TU], ysl, ALU.add), reads=(b_ps[bk], b_R1, b_R2), writes=(b_R1, b_R2))
                if last_group:
                    SQb = sct(2).bitcast(BF16)
                    sq = SQb[:, (j % 2) * TW + tt * TU:(j % 2) * TW + (tt + 1) * TU]
                    act(A_(sq, ysl, AF.Square), reads=(b_R1, b_R2), writes=(b_scr[2],))
                    P.op("pe", (lambda e: e.matmul(PS[:, SSB[tt], 0:TU], lhsT=ONESB[:], rhs=sq,
                                                   start=(j == 0), stop=(j == KC - 1))),
                         reads=(b_scr[2], b_ct), writes=(b_ps[SSB[tt]],))

            def proj_units(W2d, c0, rhs3, b_rhs, consume):
                s = wq(*kblock(W2d, c0))
                wv = wsv(s)
                for tt in range(3):
                    bk = nb()
                    mm_group(PS[:, bk, 0:TU], [(wv[:, k, :], rhs3[:, k, tt * TU:(tt + 1) * TU]) for k in range(KC)],
                             reads=(b_ws[s], b_rhs), writes=(b_ps[bk],))
                    consume(tt, bk)

            def pair_units(WA, cA, rhsA, b_rhsA, WB, cB, rhsB, b_rhsB, consume):
                sA = wq(*kblock(WA, cA))
                sB = wq(*kblock(WB, cB))
                wvA, wvB = wsv(sA), wsv(sB)
                for tt in range(3):
                    sl = slice(tt * TU, (tt + 1) * TU)
                    ba, bb = nb(), nb()
                    mm_group(PS[:, ba, 0:TU], [(wvA[:, k, :], rhsA[:, k, sl]) for k in range(KC)],
                             reads=(b_ws[sA], b_rhsA), writes=(b_ps[ba],))
                    mm_group(PS[:, bb, 0:TU], [(wvB[:, k, :], rhsB[:, k, sl]) for k in range(KC)],
                             reads=(b_ws[sB], b_rhsB), writes=(b_ps[bb],))
                    consume(tt, ba, bb)

            def mixer(l):
                ms = load_mod(l, 0)
                SH, SC = MODv[:, ms, 0], MODv[:, ms, 1]
                if pas == 0:
                    dve(CP(TXSV[:, l * 48:(l + 1) * 48], TXS), reads=(b_sm,), writes=(b_sv,))
                else:
                    dve(CP(TX, TXSV[:, l * 48:(l + 1) * 48]), reads=(b_sv,), writes=(b_sm,))
                TXv = v3(TX, 3)
                sqt = SQ[:, 0:48]
                act(A_(sqt, TX, AF.Square), reads=(b_sm,), writes=(b_scr[2],))
                bk = nb()
                mm_group(PS[:, bk, 0:3], [(ONESB[:], sqt[:, j * 3:(j + 1) * 3]) for j in range(KC)],
                         reads=(b_scr[2], b_ct), writes=(b_ps[bk],))
                act(A_(RST, PS[:, bk, 0:3], AF.Sqrt, bias=EPSC, scale=1.0 / D), reads=(b_ps[bk], b_ct), writes=(b_sm,))
                dve(lambda e: e.reciprocal(out=RST, in_=RST), reads=(b_sm,), writes=(b_sm,))
                dve(TT(TXv, TXv, RST.unsqueeze(1).broadcast_to([128, KC, 3]), ALU.mult), reads=(b_sm,), writes=(b_sm,))
                dve(TT(TXv, TXv, SC[:, :, 0:1].broadcast_to([128, KC, 3]), ALU.mult), reads=(b_sm, b_mod[ms]), writes=(b_sm,))
                dve(TT(TXv, TXv, SH[:, :, 0:1].broadcast_to([128, KC, 3]), ALU.add), reads=(b_sm, b_mod[ms]), writes=(b_sm,))
                if pas == 0:
                    dve(lambda e: e.memset(XNT[:], 0.0), reads=(b_sm,), writes=(b_xnt,))
                else:
                    dve(CP(XNTv[:, :, 0:3], TXv), reads=(b_sm,), writes=(b_xnt,))

                stop("m%d_tail" % l)
                xn_pass(l, 0, XN1, b_R1)
                stop("m%d_xn" % l)

                P.dma("sp", (lambda e: e.dma_start(out=H0C.rearrange("p c n -> p (c n)"), in_=std[l])), ds_h0c,
                      reads=(b_std[l],), writes=(b_h0c, b_R3))
                lam = VECv[:, l, 11, :]
                act(A_(CL, lam, AF.Exp, scale=-1.0), reads=(b_const,), writes=(b_sm,))
                act(A_(CL, CL, AF.Ln, bias=ONE1), reads=(b_sm, b_ct), writes=(b_sm,))
                dve(TS_(CL2, CL, -16.0, None, ALU.mult), reads=(b_sm,), writes=(b_sm,))
                dve(TS_(CL, CL, -8.0, None, ALU.mult), reads=(b_sm,), writes=(b_sm,))

                for h in range(KC):
                    proj_units(w_in[l], D + h * 128, XN1, b_R1,
                               lambda tt, bk, h=h: act(A_(YA[:, h, tt * TU:(tt + 1) * TU], PS[:, bk, 0:TU], GELU),
                                                       reads=(b_ps[bk],), writes=(b_R2,)))

                stop("m%d_ga" % l)
                cw = [VECv[:, l, 4 + k, :] for k in range(4)]
                cb, br, bi = VECv[:, l, 8, :], VECv[:, l, 9, :], VECv[:, l, 10, :]
                for h in range(KC):
                    sA = wq(*kblock(w_in[l], h * 128))
                    sB = wq((0, 128, 128, w_r[l][h]), (128, 128, 128, w_i[l][h]))
                    wvA = wsv(sA)
                    wR, wI = WS[:, sB, 0:128], WS[:, sB, 128:256]
                    XA, bXA = r0t(h % 2), b_r0t[h % 2]
                    XC, bXC = r0t(2), b_r0t[2]
                    Rt, bR = r0t(3), b_r0t[3]
                    It, bI = r0t(4), b_r0t[4]
                    At, bA = r0t(5), b_r0t[5]
                    St, bS = r0t(6), b_r0t[6]
                    HL, bHL = sct(0), b_scr[0]
                    Pt, bP = sct(1), b_scr[1]
                    XCb, bXCb = sct(2).bitcast(BF16), b_scr[2]
                    XAs = XA[:, 3 + TP:3 + TP + 176].rearrange("p (b t) -> p b t", t=11)
                    lw = [wvA[:, k, :] for k in range(KC)]
                    b0 = nb()
                    mm_group(PS[:, b0, TU:TU + 3], [(lw[k], XNTv[:, k, 0:3]) for k in range(KC)],
                             reads=(b_ws[sA], b_xnt), writes=(b_ps[b0],))
                    banks = [b0, None, None]
                    for tt in range(3):
                        bk = b0 if tt == 0 else nb()
                        banks[tt] = bk
                        mm_group(PS[:, bk, 0:TU], [(lw[k], XN1[:, k, tt * TU:(tt + 1) * TU]) for k in range(KC)],
                                 reads=(b_ws[sA], b_R1), writes=(b_ps[bk],))
                    act(A_(XA[:, 0:3], PS[:, b0, TU:TU + 3], AF.Copy), reads=(b_ps[b0],), writes=(bXA,))
                    act(A_(XA[:, 3:3 + TU], PS[:, b0, 0:TU], AF.Copy), reads=(b_ps[b0],), writes=(bXA,))
                    act(A_(XA[:, 3 + TU:3 + 2 * TU], PS[:, banks[1], 0:TU], AF.Copy), reads=(b_ps[banks[1]],), writes=(bXA,))
                    act(A_(XA[:, 3 + 2 * TU:3 + TP], PS[:, banks[2], 0:256], AF.Copy), reads=(b_ps[banks[2]],), writes=(bXA,))
                    dve(CP(XAs[:, :, 3:11], PS[:, banks[2], 256:TU].rearrange("p (b t) -> p b t", t=8)),
                        reads=(b_ps[banks[2]],), writes=(bXA,))
                    dve(CP(XAs[:, :, 0:3], H0C[:, h, 16:64].rearrange("p (b k) -> p b k", k=3)), reads=(b_h0c,), writes=(bXA,))
                    dve(CP(OUTSv[:, h, 17:20], XA[:, TP:TP + 3]), reads=(bXA,), writes=(b_outs,))
                    dve(CP(OUTSv[:, h, 20:68].rearrange("p (b k) -> p b k", k=3), XAs[:, :, 8:11]), reads=(bXA,), writes=(b_outs,))
                    XCs = XC[:, TP:T].rearrange("p (b t) -> p b t", t=8)
                    act(A_(XC[:, 0:TP], XA[:, 3:3 + TP], AF.Identity, bias=cb[:, h:h + 1], scale=cw[3][:, h:h + 1]),
                        reads=(bXA, b_const), writes=(bXC,))
                    act(A_(XCs, XAs[:, :, 3:11], AF.Identity, bias=cb[:, h:h + 1], scale=cw[3][:, h:h + 1]),
                        reads=(bXA, b_const), writes=(bXC,))
                    for k in range(3):
                        dve(STT(XC[:, 0:TP], XA[:, k:k + TP], cw[k][:, h:h + 1], XC[:, 0:TP], ALU.mult, ALU.add),
                            reads=(bXA, b_const, bXC), writes=(bXC,))
                        dve(STT(XCs, XAs[:, :, k:k + 8], cw[k][:, h:h + 1], XCs, ALU.mult, ALU.add),
                            reads=(bXA, b_const, bXC), writes=(bXC,))
                    dve(CP(XCb[:, 0:T], XC[:, 0:T]), reads=(bXC,), writes=(bXCb,))
                    for tt in range(3):
                        bkr, bki = nb(), nb()
                        mm_group(PS[:, bkr, 0:TU], [(wR, XCb[:, tt * TU:(tt + 1) * TU])],
                                 reads=(b_ws[sB], bXCb), writes=(b_ps[bkr],))
                        mm_group(PS[:, bki, 0:TU], [(wI, XCb[:, tt * TU:(tt + 1) * TU])],
                                 reads=(b_ws[sB], bXCb), writes=(b_ps[bki],))
                        act(A_(Rt[:, tt * TU:(tt + 1) * TU], PS[:, bkr, 0:TU], AF.Sigmoid, bias=br[:, h:h + 1]),
                            reads=(b_ps[bkr], b_const), writes=(bR,))
                        act(A_(It[:, tt * TU:(tt + 1) * TU], PS[:, bki, 0:TU], AF.Sigmoid, bias=bi[:, h:h + 1]),
                            reads=(b_ps[bki], b_const), writes=(bI,))
                    act(A_(At[:, 0:T], Rt[:, 0:T], AF.Exp, scale=CL[:, h:h + 1]), reads=(bR, b_sm), writes=(bA,))
                    act(A_(St[:, 0:T], Rt[:, 0:T], AF.Exp, scale=CL2[:, h:h + 1]), reads=(bR, b_sm), writes=(bS,))
                    dve(TS_(St[:, 0:T], St[:, 0:T], -1.0, 1.0, ALU.mult, ALU.add), reads=(bS,), writes=(bS,))
                    dve(lambda e, St=St: e.tensor_scalar_max(out=St[:, 0:T], in0=St[:, 0:T], scalar1=0.0), reads=(bS,), writes=(bS,))
                    act(A_(St[:, 0:T], St[:, 0:T], AF.Sqrt), reads=(bS,), writes=(bS,))
                    dve(TT(It[:, 0:T], It[:, 0:T], St[:, 0:T], ALU.mult), reads=(bI, bS), writes=(bI,))
                    dve(TT(It[:, 0:T], It[:, 0:T], XC[:, 0:T], ALU.mult), reads=(bI, bXC), writes=(bI,))
                    dve(lambda e, HL=HL, At=At, It=It: e.tensor_tensor_scan(out=HL[:, 0:TP], data0=At[:, 0:TP], data1=It[:, 0:TP],
                                                                             initial=0.0, op0=ALU.mult, op1=ALU.add),
                        reads=(bA, bI), writes=(bHL,))
                    dve(lambda e, Pt=Pt, At=At: e.tensor_tensor_scan(out=Pt[:, 0:TP], data0=At[:, 0:TP], data1=At[:, 0:TP],
                                                                      initial=1.0, op0=ALU.mult, op1=ALU.min),
                        reads=(bA,), writes=(bP,))
                    for b in range(16):
                        c0 = TP + b * 8
                        dve(lambda e, HL=HL, At=At, It=It, c0=c0, b=b, h=h: e.tensor_tensor_scan(
                            out=HL[:, c0:c0 + 8], data0=At[:, c0:c0 + 8], data1=It[:, c0:c0 + 8],
                            initial=H0C[:, h, b:b + 1], op0=ALU.mult, op1=ALU.add),
                            reads=(bA, bI, b_h0c), writes=(bHL,))
                    dve(CP(HLL[:, h:h + 1], HL[:, TP - 1:TP]), reads=(bHL,), writes=(b_sm,))
                    dve(CP(PLL[:, h:h + 1], Pt[:, TP - 1:TP]), reads=(bP,), writes=(b_sm,))
                    dve(CP(OUTSv[:, h, 1:17], HL[:, TP:T].rearrange("p (b t) -> p b t", t=8)[:, :, 7]), reads=(bHL,), writes=(b_outs,))
                    dve(TT(Qv[:, h, :], YA[:, h, 0:TP], Pt[:, 0:TP], ALU.mult), reads=(b_R2, bP), writes=(b_R3,))
                    dve(TT(YA[:, h, :], YA[:, h, :], HL[:, 0:T], ALU.mult), reads=(b_R2, bHL), writes=(b_R2,))

                stop("m%d_lru" % l)

                alias(b_r0t, (b_R0,))
                branch_b_v(l)

                stop("m%d_v" % l)
                if pas == 0:
                    dve(lambda e: e.memset(HIN, 0.0), writes=(b_sm,))
                    dve(CP(HSV[:, l * 16:(l + 1) * 16], HLL), reads=(b_sm,), writes=(b_sv,))
                else:
                    dve(CP(HIN, HSV[:, l * 16:(l + 1) * 16]), reads=(b_sv,), writes=(b_sm,))
                for h in range(KC):
                    dve(STT(YA[:, h, 0:TP], Qv[:, h, :], HIN[:, h:h + 1], YA[:, h, 0:TP], ALU.mult, ALU.add),
                        reads=(b_R3, b_sm, b_R2), writes=(b_R2,))
                dve(TT(TMPS, PLL, HIN, ALU.mult), reads=(b_sm,), writes=(b_sm,))
                dve(TT(OUTSv[:, :, 0], TMPS, HLL, ALU.add), reads=(b_sm,), writes=(b_outs,))
                b_so = [Buf("so0"), Buf("so1")]
                alias((b_scr[0],), b_so)
                for q in range(4):
                    bk = nb()
                    for jj in range(4):
                        hh = q * 4 + jj
                        transpose(PS[0:68, bk, jj * 128:(jj + 1) * 128], OUTSv[:, hh, :], IDENT[:],
                                  reads=(b_outs, b_const), writes=(b_ps[bk],))
                    so = SCR[0:68, (q % 2) * 512:(q % 2 + 1) * 512]
                    dve(CP(so, PS[0:68, bk, :]), reads=(b_ps[bk],), writes=(b_so[q % 2],))
                    out_store((lambda e, so=so, q=q, pas=pas: e.dma_start(out=stout[pas][l][:, q * 512:(q + 1) * 512], in_=so)), (b_so[q % 2],))
                alias(b_so, (b_scr[0],))
                stop("m%d_fix" % l)
                merge_stage(l, w_pa, 4 * D, first=True)
                stop("m%d_s5" % l)
                branch_b_u(l)
                stop("m%d_u" % l)
                merge_stage(l, w_pb, 5 * D, first=False)
                stop("m%d_s3" % l)
                for j in range(KC):
                    proj_units(w_o[l], j * 128, Mv, b_R3, lambda tt, bk, j=j: y_stats_and_store(j, tt, bk, True, True))
                stop("m%d_s6" % l)
                alias((b_R0,), b_r0t)
                rstd_from_ss(r0t(1), b_r0t[1])
                update_pass(l, 0, False)

            def merge_stage(l, Wp, mgcol, first):
                for j in range(KC):
                    def consume(tt, ba, bb, j=j):
                        sl = slice(tt * TU, (tt + 1) * TU)
                        SG = sct(1)[:, tt * TU:(tt + 1) * TU]
                        act(A_(SG, PS[:, bb, 0:TU], AF.Sigmoid), reads=(b_ps[bb],), writes=(b_scr[1],))
                        if first:
                            dve(TT(Mv[:, j, sl], SG, PS[:, ba, 0:TU], ALU.mult), reads=(b_scr[1], b_ps[ba]), writes=(b_R3,))
                        else:
                            TM = sct(2)[:, tt * TU:(tt + 1) * TU]
                            dve(TT(TM, SG, PS[:, ba, 0:TU], ALU.mult), reads=(b_scr[1], b_ps[ba]), writes=(b_scr[2],))
                            dve(TT(Mv[:, j, sl], Mv[:, j, sl], TM, ALU.add), reads=(b_scr[2], b_R3), writes=(b_R3,))
                    pair_units(Wp[l], j * 128, YA, b_R2, w_in[l], mgcol + j * 128, XN1, b_R1, consume)

            def branch_b_v(l):
                STG = SCR[:, TW:TW + D].rearrange("p (g s) -> p g s", s=128)
                stg_b = (b_scr[1], b_scr[2])

                def build_mixmat(dstv, dst_buf):
                    for g4 in range(4):
                        bk = nb()
                        for gg in range(4):
                            transpose(PS[:, bk, gg * 128:(gg + 1) * 128], STG[:, g4 * 4 + gg, :], IDENT[:], reads=stg_b + (b_const,),
                                      writes=(b_ps[bk],))
                        dve(TT(dstv[:, g4 * 4:(g4 + 1) * 4, :], PS[:, bk, :].rearrange("p (g t) -> p g t", t=128),
                               MASK[:].unsqueeze(1).broadcast_to([128, 4, 128]), ALU.mult), reads=(b_ps[bk], b_const), writes=(dst_buf,))

                P.dma("sp", (lambda e: e.dma_start(out=STG, in_=w_s[l].rearrange("g t s -> t g s"))), ds_wmt, writes=stg_b)
                build_mixmat(WMTv, b_wmt)
                dve(lambda e: e.memset(SCR[:, TW:TW + D], 0.0), writes=stg_b)
                for b in range(16):
                    P.dma("sp", (lambda e, b=b: e.dma_start(out=STG[b * 8:(b + 1) * 8, :, b * 8:(b + 1) * 8],
                                                            in_=w_s[l][:, 0:8, 0:8].rearrange("g t s -> t g s"))),
                          ds_bd, reads=(), writes=stg_b)
                build_mixmat(BDv, b_bd)
                brow = SCR[:, TW:TW + D]
                bsl = b_s[l].rearrange("g t -> (g t)").unsqueeze(0)
                P.dma("sp", (lambda e: e.dma_start(out=brow[0:1, :], in_=bsl)), ds_bs, writes=(b_scr[1], b_scr[2]))
                P.dma("sp", (lambda e: e.dma_start(out=brow[32:33, :], in_=bsl)), ds_bs2, writes=(b_scr[1], b_scr[2]))
                dve(lambda e: e.memset(BSR[:], 0.0), writes=(b_bsr,))
                P.dma("sp", (lambda e: e.dma_start(out=BS8[:].rearrange("p (g t) -> p g t", t=8),
                                                   in_=b_s[l][:, 0:8].partition_broadcast(128))), ds_bs8, writes=(b_bs8,))
                act(A_(BSR[0:1, :], brow[0:1, :], AF.Copy), reads=(b_scr[1], b_scr[2]), writes=(b_bsr,))
                hi32 = sct(0)[32:33, 0:1024].bitcast(BF16)
                act(A_(hi32, brow[32:33, :], AF.Copy), reads=(b_scr[1], b_scr[2]), writes=(b_scr[0],))
                dve(TT(BSR[32:33, :], brow[32:33, :], hi32, ALU.subtract), reads=(b_scr[0], b_scr[1], b_scr[2]), writes=(b_bsr,))

                DG = sct(2)[:, 0:256]
                for c in range(KC):
                    dg = DG[:, (c % 2) * 128:(c % 2 + 1) * 128]
                    dve(TS_(dg, IDENT[:], VECv[:, l, 12, c:c + 1], None, ALU.mult), reads=(b_const,), writes=(b_scr[2],))
                    bk = c // 4
                    P.op("pe", (lambda e, bk=bk, c=c, dg=dg: e.matmul(PS[:, bk, (c % 4) * 128:(c % 4 + 1) * 128], lhsT=ONESF[:], rhs=dg,
                                                                     start=True, stop=True)),
                         reads=(b_scr[2], b_ct), writes=(b_ps[bk],))
                GVS = SCR[:, TW:TW + D]
                JUNK = sct(0)[:, 1024:1152].bitcast(BF16)
                vb = (4, 5, 6, 7)
                u = 0
                for blk in range(16):
                    s = wq(*kblock(w_in[l], 3 * D + blk * 128))
                    wv = wsv(s)
                    for i in range(9):
                        bk = vb[u % 4]
                        u += 1
                        mm_group(PS[:, bk, 0:128], [(XN1[:, k, i * 128:(i + 1) * 128], wv[:, k, :]) for k in range(KC)],
                                 reads=(b_ws[s], b_R1), writes=(b_ps[bk],))
                        if i < 8:
                            GV = sct(0)[:, (u % 8) * 128:(u % 8 + 1) * 128]
                            wrs = (b_scr[0],)
                        else:
                            GV = GVS[:, blk * 128:(blk + 1) * 128]
                            wrs = (b_scr[1], b_scr[2])
                        act(A_(GV, PS[:, bk, 0:128], GELU), reads=(b_ps[bk],), writes=wrs)
                        act(A_(JUNK[:, 0:128], GV, AF.Square, accum_out=SSV[:, i * 16 + blk:i * 16 + blk + 1]),
                            reads=wrs, writes=(b_sm, b_scr[0]))
                        dve(CP(VN[:, i, blk * 128:(blk + 1) * 128], GV), reads=wrs, writes=(b_R0,))
                dve(lambda e: e.tensor_reduce(out=RSV, in_=SSV.rearrange("p (i b) -> p i b", b=16), axis=AX.X, op=ALU.add),
                    reads=(b_sm,), writes=(b_sm,))
                act(A_(RSV, RSV, AF.Sqrt, bias=EPSC, scale=1.0 / D), reads=(b_sm, b_ct), writes=(b_sm,))
                dve(lambda e: e.reciprocal(out=RSV, in_=RSV), reads=(b_sm,), writes=(b_sm,))
                GVP = PS[:, 0:4, :]
                gvr = tuple(b_ps[b] for b in range(4))
                for i in range(9):
                    vi = VN[:, i, :].rearrange("p (q n) -> p q n", n=512)
                    dve(STT(vi, vi, RSV[:, i:i + 1], GVP, ALU.mult, ALU.mult), reads=(b_R0, b_sm) + gvr, writes=(b_R0,))
                gs = GVS.rearrange("p (q n) -> p q n", n=512)
                dve(STT(gs, gs, RSV[:, 8:9], GVP, ALU.mult, ALU.mult), reads=(b_scr[1], b_scr[2], b_sm) + gvr,
                    writes=(b_scr[1], b_scr[2]))
                out_store((lambda e, pas=pas: e.dma_start(out=vout[pas][l], in_=GVS)), (b_scr[1], b_scr[2]))

            def branch_b_u(l):
                GU = sct(0)
                for g in range(KC):
                    proj_units(w_in[l], 2 * D + g * 128, XN1, b_R1,
                               lambda tt, bk: act(A_(GU[:, tt * TU:(tt + 1) * TU], PS[:, bk, 0:TU], GELU),
                                                  reads=(b_ps[bk],), writes=(b_scr[0],)))
                    for tt in range(3):
                        bk = nb()
                        for sidx in range(3):
                            i = tt * 3 + sidx
                            o2 = PS[:, bk, sidx * 128:(sidx + 1) * 128]
                            if i < 8:
                                rd = (b_R0, b_wmt, b_bsr, b_ct)
                                P.op("pe", (lambda e, o2=o2, i=i, g=g: e.matmul(o2, lhsT=VN[:, i, g * 128:(g + 1) * 128], rhs=WMTv[:, g, :],
                                                                              start=True, stop=False)),
                                     reads=rd, writes=(b_ps[bk],), sig=False)
                                P.op("pe", (lambda e, o2=o2, g=g: e.matmul(o2, lhsT=ONESB[0:33, :], rhs=BSR[0:33, g * 128:(g + 1) * 128],
                                                                         start=False, stop=True)),
                                     reads=rd, writes=(b_ps[bk],), sig=True)
                            else:
                                rd = (b_R0, b_bd)
                                P.op("pe", (lambda e, o2=o2, i=i, g=g: e.matmul(o2, lhsT=VN[:, i, g * 128:(g + 1) * 128], rhs=BDv[:, g, :],
                                                                              start=True, stop=True)),
                                     reads=rd, writes=(b_ps[bk],), sig=True)
                        if tt < 2:
                            dve(TT(YA[:, g, tt * TU:(tt + 1) * TU], PS[:, bk, 0:TU], GU[:, tt * TU:(tt + 1) * TU], ALU.mult),
                                reads=(b_ps[bk], b_scr[0]), writes=(b_R2,))
                        else:
                            dve(TT(YA[:, g, 2 * TU:2 * TU + 256], PS[:, bk, 0:256], GU[:, 2 * TU:2 * TU + 256], ALU.mult),
                                reads=(b_ps[bk], b_scr[0]), writes=(b_R2,))
                            tmpb = sct(1)[:, 0:128].rearrange("p (b t) -> p b t", t=8)
                            dve(TT(tmpb, PS[:, bk, 256:TU].rearrange("p (b t) -> p b t", t=8),
                                   BS8[:, g * 8:(g + 1) * 8].unsqueeze(1).broadcast_to([128, 16, 8]), ALU.add),
                                reads=(b_ps[bk], b_bs8), writes=(b_scr[1],))
                            dve(TT(YA[:, g, TP:T], sct(1)[:, 0:128], GU[:, TP:T], ALU.mult),
                                reads=(b_scr[1], b_scr[0]), writes=(b_R2,))

            def ffn(l, final):
                ms = load_mod(l, 1)
                moe = (l == 1)
                xn_pass(l, 1, XN3, b_R3, moe=moe)
                if moe:
                    GE = moe_gates()
                alias(b_r0t, (b_R0,))
                ngroups = 8 if moe else 3
                SGt = sct(0)
                GB = sct(1)
                for grp in range(ngroups):
                    if moe:
                        Wg2, Wu2, Wd2 = moe_wg[0][grp], moe_wu[0][grp], moe_wd[0][grp]
                        c_off = 0
                        dve(TS_(SELT[:], ONESF[0:8, :], IDENT[0:8, grp:grp + 1], None, ALU.mult), reads=(b_const, b_ct), writes=(b_selt,))
                        for tt in range(3):
                            bk = nb()
                            P.op("pe", (lambda e, bk=bk, tt=tt: e.matmul(PS[:, bk, 0:TU], lhsT=SELT[:], rhs=GE[:, tt * TU:(tt + 1) * TU],
                                                                        start=True, stop=True)),
                                 reads=(b_scr[2], b_selt), writes=(b_ps[bk],))
                            act(A_(GB[:, tt * TU:(tt + 1) * TU], PS[:, bk, 0:TU], AF.Copy), reads=(b_ps[bk],), writes=(b_scr[1],))
                    else:
                        Wg2, Wu2, Wd2 = ffn_wg[0], ffn_wu[0], ffn_wd[0][grp * D:(grp + 1) * D, :]
                        c_off = grp * D
                    for hc in range(KC):
                        def consume(tt, ba, bb, hc=hc):
                            sl = slice(tt * TU, (tt + 1) * TU)
                            SG = SGt[:, sl]
                            act(A_(SG, PS[:, ba, 0:TU], AF.Silu), reads=(b_ps[ba],), writes=(b_scr[0],))
                            if moe:
                                dve(TT(SG, SG, GB[:, sl], ALU.mult), reads=(b_scr[0], b_scr[1]), writes=(b_scr[0],))
                            dve(TT(Hg[:, hc, sl], SG, PS[:, bb, 0:TU], ALU.mult), reads=(b_scr[0], b_ps[bb]), writes=(b_R0,))
                        pair_units(Wg2, c_off + hc * 128, XN3, b_R3, Wu2, c_off + hc * 128, XN3, b_R3, consume)
                    for j in range(KC):
                        proj_units(Wd2, j * 128, Hg, b_R0,
                                   lambda tt, bk, j=j, grp=grp: y_stats_and_store(j, tt, bk, grp == 0, grp == ngroups - 1))
                alias((b_R0,), b_r0t)
                rstd_from_ss(r0t(1), b_r0t[1])
                update_pass(l, 1, final)

            def moe_gates():
                LG = sct(1)[0:8, 0:T]
                for tt in range(3):
                    act(A_(LG[:, tt * TU:(tt + 1) * TU], PS[0:8, SSB[tt], 0:TU], AF.Identity, bias=RB[:, 0:1]),
                        reads=(b_ps[SSB[tt]], b_const), writes=(b_scr[1],))
                LTv = LT.rearrange("p (i e) -> p i e", e=8)
                LEv = LE.rearrange("p (i e) -> p i e", e=8)
                bk = nb()
                for i in range(9):
                    transpose(PS[:, bk, i * 8:(i + 1) * 8], LG[:, i * 128:(i + 1) * 128], IDENT[0:8, 0:8],
                              reads=(b_scr[1], b_const), writes=(b_ps[bk],))
                dve(CP(LT, PS[:, bk, 0:72]), reads=(b_ps[bk],), writes=(b_sm,))
                dve(lambda e: e.tensor_reduce(out=M1, in_=LTv, axis=AX.X, op=ALU.max), reads=(b_sm,), writes=(b_sm,))
                dve(TT(LTv, LTv, M1.unsqueeze(2).broadcast_to([128, 9, 8]), ALU.subtract), reads=(b_sm,), writes=(b_sm,))
                act(A_(LE, LT, AF.Exp), reads=(b_sm,), writes=(b_sm,))
                dve(TS_(LT, LE, 1.0, None, ALU.is_lt), reads=(b_sm,), writes=(b_sm,))
                dve(TT(LT, LT, LE, ALU.mult), reads=(b_sm,), writes=(b_sm,))
                dve(lambda e: e.tensor_reduce(out=M2, in_=LTv, axis=AX.X, op=ALU.max), reads=(b_sm,), writes=(b_sm,))
                dve(TT(LTv, LEv, M2.unsqueeze(2).broadcast_to([128, 9, 8]), ALU.is_ge), reads=(b_sm,), writes=(b_sm,))
                dve(TT(LE, LE, LT, ALU.mult), reads=(b_sm,), writes=(b_sm,))
                dve(lambda e: e.tensor_reduce(out=M1, in_=LEv, axis=AX.X, op=ALU.add), reads=(b_sm,), writes=(b_sm,))
                dve(lambda e: e.reciprocal(out=M1, in_=M1), reads=(b_sm,), writes=(b_sm,))
                dve(TT(LEv, LEv, M1.unsqueeze(2).broadcast_to([128, 9, 8]), ALU.mult), reads=(b_sm,), writes=(b_sm,))
                GE = sct(2)[0:8, 0:T]
                for tt in range(3):
                    bk = nb()
                    for sidx in range(3):
                        i = tt * 3 + sidx
                        transpose(PS[0:8, bk, sidx * 128:(sidx + 1) * 128], LEv[:, i, :], IDENT[:], reads=(b_sm, b_const), writes=(b_ps[bk],))
                    act(A_(GE[:, tt * TU:(tt + 1) * TU], PS[0:8, bk, 0:TU], AF.Copy), reads=(b_ps[bk],), writes=(b_scr[2],))
                return GE

            for l in range(2):
                mixer(l)
                stop("m%d" % l)
                ffn(l, final=(l == 1))
                stop("f%d" % l)

            b_yt = [Buf("yt%d" % i) for i in range(3)]
            alias(b_scr, b_yt)
            for i in range(9):
                for q in range(4):
                    bk = nb()
                    for jj in range(4):
                        j = q * 4 + jj
                        transpose(PS[:, bk, jj * 128:(jj + 1) * 128], Yf[:, j, i * 128:(i + 1) * 128], IDENT[:],
                                  reads=(b_R1, b_R2, b_const), writes=(b_ps[bk],))
                    slot = (i * 4 + q) % 3
                    yt = sct(slot)[:, 0:512]
                    if q % 2 == 0:
                        act(A_(yt, PS[:, bk, :], AF.Copy), reads=(b_ps[bk],), writes=(b_yt[slot],))
                    else:
                        dve(CP(yt, PS[:, bk, :]), reads=(b_ps[bk],), writes=(b_yt[slot],))
                    if i == 0 and q < 3:
                        b_yt[slot].ds = new_ds()
                        out_ds.append(b_yt[slot].ds)
                    P.dma("sp", (lambda e, yt=yt, i=i, q=q, pas=pas: e.dma_start(out=yout[pas][i * 128:(i + 1) * 128, q * 512:(q + 1) * 512], in_=yt)),
                          b_yt[slot].ds, reads=(b_yt[slot],))

        final_ev = [Ev(d.key, d.count) for d in P.dsems if d.count > 0]

        semh = {}
        for k in ("pe", "act", "dve", "pool", "sp"):
            semh[k] = es.enter_context(nc.semaphore("s_" + k))
        for d in P.dsems:
            semh[d.key] = es.enter_context(nc.semaphore("s_" + d.key))
        block = es.enter_context(nc.Block())

        @block.tensor
        def _(e):
            P.emit("pe", e, semh)

        @block.scalar
        def _(e):
            P.emit("act", e, semh)

        @block.vector
        def _(e):
            P.emit("dve", e, semh)

        @block.gpsimd
        def _(e):
            P.emit("pool", e, semh)

        @block.sync
        def _(e):
            P.emit("sp", e, semh, final=final_ev)

    print("counts", P.cnt, {d.key: d.count for d in P.dsems}, "wblocks", wcount[0])
    return nc


_CACHE = {}


def _fm(v):
    return np.ascontiguousarray(v.reshape(KC, 128).T)


def kernel(x_prompt, x_sample, c_prompt, c_sample, state_lru_h, state_lru_conv,
           w_ada, b_ada, g_pre_mix, g_post_mix, g_pre_ffn, g_post_ffn,
           w_in, conv_w, conv_b, w_r, b_r, w_i, b_i, lru_lambda, g_v, w_s, b_s,
           w_pa, w_pb, w_o, ffn_wg, ffn_wu, ffn_wd,
           router_w, router_b, moe_wg, moe_wu, moe_wd):
    f = lambda a: np.ascontiguousarray(np.asarray(a, dtype=np.float32))
    x_prompt, x_sample, c_prompt, c_sample = f(x_prompt), f(x_sample), f(c_prompt), f(c_sample)
    state_lru_h, state_lru_conv = f(state_lru_h), f(state_lru_conv)
    if "nc" not in _CACHE:
        _CACHE["nc"] = build()
    nc = _CACHE["nc"]
    vecs = np.zeros((128, 2, NVEC, KC), np.float32)
    for l in range(2):
        lst = [g_pre_mix[l], g_post_mix[l], g_pre_ffn[l], g_post_ffn[l], conv_w[l][0], conv_w[l][1], conv_w[l][2],
               conv_w[l][3], conv_b[l], b_r[l], b_i[l], lru_lambda[l], g_v[l]]
        for vi, v in enumerate(lst):
            vecs[:, l, vi, :] = _fm(f(v))
    vecs = vecs.reshape(128, -1)
    bada = np.stack([f(b_ada)[l].reshape(96, 128).T for l in range(2)], axis=1).reshape(128, 192)
    bada = np.ascontiguousarray(bada)
    ident = np.eye(128, dtype=np.float32)
    mask = np.triu(np.ones((128, 128), np.float32))
    shared = dict(vecs=vecs, bada=bada, ident=ident, mask=mask, w_ada=f(w_ada), w_in=f(w_in), w_r=f(w_r), w_i=f(w_i),
                  w_s=f(w_s), b_s=f(b_s), w_pa=f(w_pa), w_pb=f(w_pb), w_o=f(w_o), ffn_wg=f(ffn_wg), ffn_wu=f(ffn_wu),
                  ffn_wd=f(ffn_wd), router_w=f(router_w), router_b=f(router_b).reshape(8, 1), moe_wg=f(moe_wg),
                  moe_wu=f(moe_wu), moe_wd=f(moe_wd))
    in_maps = []
    for c in range(NCORES):
        xs, cs, sts = [], [], []
        for p in range(NPASS):
            sb0 = 16 * (2 * c + p)
            xs.append(np.concatenate([x_prompt[c, p * TP:(p + 1) * TP], x_sample[sb0:sb0 + 16].reshape(TS, D)], axis=0))
            cs.append(np.concatenate([c_prompt[c:c + 1], c_sample[sb0:sb0 + 16]], axis=0))
            sts.append(np.stack([np.concatenate([state_lru_h[l, sb0:sb0 + 16],
                                                 state_lru_conv[l, sb0:sb0 + 16].reshape(48, D)], axis=0) for l in range(2)]))
        sel = np.zeros((128, 8), np.float32)
        m = dict(shared)
        m.update(xtok=np.ascontiguousarray(np.stack(xs)), ctok=np.ascontiguousarray(np.stack(cs)),
                 st_in=np.ascontiguousarray(np.stack(sts)), sel=sel)
        in_maps.append(m)
    res = run_bass_kernel_spmd(nc, in_maps, core_ids=list(range(NCORES)))
    R = res.results
    B, S = x_prompt.shape[0], x_prompt.shape[1]
    y_prompt = np.zeros((B, S, D), np.float32)
    y_sample = np.zeros((128, 8, D), np.float32)
    h_p = np.zeros((2, B, D), np.float32)
    conv_p = np.zeros((2, B, 3, D), np.float32)
    h_s = np.zeros((2, 128, D), np.float32)
    conv_s = np.zeros((2, 128, 3, D), np.float32)
    v_s = np.zeros((2, 128, 8, D), np.float32)
    for c in range(NCORES):
        for p in range(NPASS):
            sb0 = 16 * (2 * c + p)
            yo, so, vo = R[c]["yout"][p], R[c]["stout"][p], R[c]["vout"][p]
            y_prompt[c, p * TP:(p + 1) * TP] = yo[:TP]
            y_sample[sb0:sb0 + 16] = yo[TP:].reshape(16, 8, D)
            for l in range(2):
                if p == 1:
                    h_p[l, c] = so[l, 0]
                    conv_p[l, c] = so[l, 17:20]
                h_s[l, sb0:sb0 + 16] = so[l, 1:17]
                conv_s[l, sb0:sb0 + 16] = so[l, 20:68].reshape(16, 3, D)
                v_s[l, sb0:sb0 + 16] = vo[l].reshape(16, 8, D)
    return (y_prompt, y_sample, h_p, conv_p, h_s, conv_s, v_s)
```
